# Optimizing a Trainium2 kernel written in Bass

```python
import jax, jax.numpy as jnp
from jax import lax
import numpy as np


D_MODEL = 2048
BATCH = 1
SEQ = 8192
DEPTH = 1

N_META = 16
EPS = 1e-6

N_HEADS = 16
QK_NOPE_DIM = 128
QK_ROPE_DIM = 64
QK_HEAD_DIM = QK_NOPE_DIM + QK_ROPE_DIM
V_HEAD_DIM = 128
Q_LORA_RANK = 512
KV_LORA_RANK = 512
ROPE_THETA = 10000.0
Q_BLOCK = 128
MLA_WIDTH = N_HEADS * V_HEAD_DIM

POOL_WINDOWS = (2, 4, 8, 16)
POOL_GROUPS = len(POOL_WINDOWS)
POOL_WIDTH = D_MODEL // 2
POOL_GROUP_DIM = POOL_WIDTH // POOL_GROUPS

IN_COLS = Q_LORA_RANK + KV_LORA_RANK + QK_ROPE_DIM + POOL_WIDTH + 2 * D_MODEL
SPLIT_POINTS = (Q_LORA_RANK,
                Q_LORA_RANK + KV_LORA_RANK,
                Q_LORA_RANK + KV_LORA_RANK + QK_ROPE_DIM,
                Q_LORA_RANK + KV_LORA_RANK + QK_ROPE_DIM + POOL_WIDTH,
                Q_LORA_RANK + KV_LORA_RANK + QK_ROPE_DIM + POOL_WIDTH + D_MODEL)

N_EXPERT_GROUPS = 8
EXPERTS_PER_GROUP = 8
N_EXPERTS = N_EXPERT_GROUPS * EXPERTS_PER_GROUP
TOP_K_IN_GROUP = 2
EXPERT_FF = D_MODEL // 4
EXPERT_BLOCK = 128

kernel_name = 'hybrid_mla_pool_hmoe'


def rms_norm(x, w):
    xf = x.astype(jnp.float32)
    y = xf * lax.rsqrt(jnp.mean(xf * xf, axis=-1, keepdims=True) + EPS)
    return (y * w.astype(jnp.float32)).astype(x.dtype)


def rope_tables(length):
    inv = 1.0 / (ROPE_THETA ** (jnp.arange(0, QK_ROPE_DIM, 2, dtype=jnp.float32) / QK_ROPE_DIM))
    ang = jnp.arange(length, dtype=jnp.float32)[:, None] * inv[None, :]
    return jnp.cos(ang), jnp.sin(ang)


def apply_rope(x, cos, sin):
    half = QK_ROPE_DIM // 2
    xf = x.astype(jnp.float32)
    x1, x2 = xf[..., :half], xf[..., half:]
    return jnp.concatenate([x1 * cos - x2 * sin, x1 * sin + x2 * cos], axis=-1).astype(x.dtype)


def mla_attention(c_q, c_kv, k_rope, q_norm_w, w_uq, kv_norm_w, w_ukv, cos, sin):
    B, L, _ = c_q.shape
    q = (rms_norm(c_q, q_norm_w) @ w_uq).reshape(B, L, N_HEADS, QK_HEAD_DIM)
    q_nope, q_pe = q[..., :QK_NOPE_DIM], q[..., QK_NOPE_DIM:]
    q_pe = apply_rope(q_pe, cos[None, :, None, :], sin[None, :, None, :])
    kv = (rms_norm(c_kv, kv_norm_w) @ w_ukv).reshape(B, L, N_HEADS, QK_NOPE_DIM + V_HEAD_DIM)
    k_nope, v = kv[..., :QK_NOPE_DIM], kv[..., QK_NOPE_DIM:]
    k_pe = apply_rope(k_rope, cos[None], sin[None])
    q = jnp.concatenate([q_nope, q_pe], axis=-1)
    k = jnp.concatenate([k_nope, jnp.broadcast_to(k_pe[:, :, None, :], (B, L, N_HEADS, QK_ROPE_DIM))], axis=-1)
    Lp = -(-L // Q_BLOCK) * Q_BLOCK
    pad = ((0, 0), (0, Lp - L), (0, 0), (0, 0))
    q, k, v = jnp.pad(q, pad), jnp.pad(k, pad), jnp.pad(v, pad)
    n_blocks = Lp // Q_BLOCK
    q_blocks = q.reshape(B, n_blocks, Q_BLOCK, N_HEADS, QK_HEAD_DIM).transpose(1, 0, 2, 3, 4)
    kpos = jnp.arange(Lp)
    scale = QK_HEAD_DIM ** -0.5

    def attend_block(args):
        q_blk, blk = args
        s = jnp.einsum('bqhd,bkhd->bhqk', q_blk, k).astype(jnp.float32) * scale
        qpos = blk * Q_BLOCK + jnp.arange(Q_BLOCK)
        causal = kpos[None, :] <= qpos[:, None]
        s = jnp.where(causal[None, None], s, jnp.finfo(jnp.float32).min)
        p = jax.nn.softmax(s, axis=-1).astype(v.dtype)
        return jnp.einsum('bhqk,bkhd->bqhd', p, v)

    o = lax.map(attend_block, (q_blocks, jnp.arange(n_blocks)))
    return o.transpose(1, 0, 2, 3, 4).reshape(B, Lp, MLA_WIDTH)[:, :L]


def multiscale_pool(u, pool_w, pool_scale):
    B, L, C = u.shape
    uf = u.astype(jnp.float32)
    csum = jnp.concatenate([jnp.zeros((B, 1, C), jnp.float32), lax.cumsum(uf, axis=1)], axis=1)
    t = jnp.arange(L)
    outs = []
    for g, w in enumerate(POOL_WINDOWS):
        lo, hi = g * POOL_GROUP_DIM, (g + 1) * POOL_GROUP_DIM
        start = jnp.maximum(t + 1 - w, 0)
        total = csum[:, 1:, lo:hi] - csum[:, start, lo:hi]
        count = (t + 1 - start).astype(jnp.float32)
        outs.append(total / count[None, :, None] - uf[:, :, lo:hi])
    pooled = jnp.stack(outs, axis=2).astype(u.dtype)
    mixed = jnp.einsum('blgc,gcd->blgd', pooled, pool_w).reshape(B, L, POOL_WIDTH)
    return mixed * pool_scale


def hybrid_mixer(a, w_in, q_norm_w, w_uq, kv_norm_w, w_ukv, w_o_mla, pool_w, pool_scale,
                 w_pool_out, w_out, cos, sin):
    proj = a @ w_in
    c_q, c_kv, k_rope, u_pool, gate_mla, gate_pool = jnp.split(proj, SPLIT_POINTS, axis=-1)
    y_mla = mla_attention(c_q, c_kv, k_rope, q_norm_w, w_uq, kv_norm_w, w_ukv, cos, sin) @ w_o_mla
    y_pool = multiscale_pool(u_pool, pool_w, pool_scale) @ w_pool_out
    merged = jax.nn.sigmoid(gate_mla) * y_mla + jax.nn.sigmoid(gate_pool) * y_pool
    return merged @ w_out


def routed_experts(xt, expert_ids, weights, w_gate, w_up, w_down):
    T, D = xt.shape
    A = T * TOP_K_IN_GROUP
    n_blocks = -(-(A + N_EXPERTS * (EXPERT_BLOCK - 1)) // EXPERT_BLOCK)
    cap = n_blocks * EXPERT_BLOCK
    flat_e = expert_ids.reshape(A)
    order = jnp.argsort(flat_e)
    sorted_e = flat_e[order]
    counts = jnp.bincount(flat_e, length=N_EXPERTS)
    padded = (counts + EXPERT_BLOCK - 1) // EXPERT_BLOCK * EXPERT_BLOCK
    seg_start = jnp.cumsum(counts) - counts
    pad_end = jnp.cumsum(padded)
    pad_start = pad_end - padded
    dest = pad_start[sorted_e] + (jnp.arange(A) - seg_start[sorted_e])
    slot_token = jnp.full((cap,), T, jnp.int32).at[dest].set((order // TOP_K_IN_GROUP).astype(jnp.int32))
    slot_weight = jnp.zeros((cap,), xt.dtype).at[dest].set(weights.reshape(A)[order].astype(xt.dtype))
    block_expert = jnp.minimum(jnp.searchsorted(pad_end, jnp.arange(n_blocks) * EXPERT_BLOCK, side='right'),
                               N_EXPERTS - 1)
    x_pad = jnp.concatenate([xt, jnp.zeros((1, D), xt.dtype)], axis=0)
    xb = x_pad[slot_token].reshape(n_blocks, EXPERT_BLOCK, D)

    def expert_block(args):
        x_blk, e = args
        hdn = jax.nn.silu(x_blk @ w_gate[e]) * (x_blk @ w_up[e])
        return hdn @ w_down[e]

    yb = lax.map(expert_block, (xb, block_expert)).reshape(cap, D)
    out = jnp.zeros((T + 1, D), xt.dtype).at[slot_token].add(yb * slot_weight[:, None])
    return out[:T]


def hierarchical_moe(b, w_router_group, b_router_group, w_router_expert, b_router_expert,
                     w_exp_gate, w_exp_up, w_exp_down):
    B, L, D = b.shape
    T = B * L
    xt = b.reshape(T, D)
    g_logits = (xt @ w_router_group).astype(jnp.float32) + b_router_group.astype(jnp.float32)
    p_group = jax.nn.softmax(g_logits, axis=-1)
    _, g_idx = lax.top_k(g_logits, 1)
    p_g = jnp.take_along_axis(p_group, g_idx, axis=-1)
    e_logits = ((xt @ w_router_expert).astype(jnp.float32) + b_router_expert.astype(jnp.float32)).reshape(
        T, N_EXPERT_GROUPS, EXPERTS_PER_GROUP)
    e_in_group = jnp.take_along_axis(e_logits, g_idx[:, :, None], axis=1)[:, 0]
    e_val, e_idx = lax.top_k(e_in_group, TOP_K_IN_GROUP)
    weights = p_g * jax.nn.softmax(e_val, axis=-1)
    expert_ids = g_idx * EXPERTS_PER_GROUP + e_idx
    y = routed_experts(xt, expert_ids, weights, w_exp_gate, w_exp_up, w_exp_down)
    return y.reshape(B, L, D)


def setup_inputs(seed: int = 0) -> dict:
    key = jax.random.key(seed)
    ks = jax.random.split(key, 24)
    f32 = jnp.float32

    def dense(k, shape, fan_in):
        return jax.random.normal(k, shape, f32) * fan_in ** -0.5

    def gain(k, shape, noise=0.05):
        return 1.0 + noise * jax.random.normal(k, shape, f32)

    return {
        'x': jax.random.normal(ks[0], (BATCH, SEQ, D_MODEL), f32),
        'meta_tokens': jax.random.normal(ks[1], (N_META, D_MODEL), f32),
        'norm_mix_w': gain(ks[2], (DEPTH, D_MODEL)),
        'w_in': dense(ks[3], (DEPTH, D_MODEL, IN_COLS), D_MODEL),
        'q_norm_w': gain(ks[4], (DEPTH, Q_LORA_RANK)),
        'w_uq': dense(ks[5], (DEPTH, Q_LORA_RANK, N_HEADS * QK_HEAD_DIM), Q_LORA_RANK),
        'kv_norm_w': gain(ks[6], (DEPTH, KV_LORA_RANK)),
        'w_ukv': dense(ks[7], (DEPTH, KV_LORA_RANK, N_HEADS * (QK_NOPE_DIM + V_HEAD_DIM)), KV_LORA_RANK),
        'w_o_mla': dense(ks[8], (DEPTH, MLA_WIDTH, D_MODEL), MLA_WIDTH),
        'pool_w': dense(ks[9], (DEPTH, POOL_GROUPS, POOL_GROUP_DIM, POOL_GROUP_DIM), POOL_GROUP_DIM),
        'pool_scale': gain(ks[10], (DEPTH, POOL_WIDTH), 0.1),
        'w_pool_out': dense(ks[11], (DEPTH, POOL_WIDTH, D_MODEL), POOL_WIDTH),
        'w_out': dense(ks[12], (DEPTH, D_MODEL, D_MODEL), D_MODEL),
        'norm_ffn_w': gain(ks[13], (DEPTH, D_MODEL)),
        'w_router_group': dense(ks[14], (DEPTH, D_MODEL, N_EXPERT_GROUPS), D_MODEL),
        'b_router_group': 0.01 * jax.random.normal(ks[15], (DEPTH, N_EXPERT_GROUPS), f32),
        'w_router_expert': dense(ks[16], (DEPTH, D_MODEL, N_EXPERTS), D_MODEL),
        'b_router_expert': 0.01 * jax.random.normal(ks[17], (DEPTH, N_EXPERTS), f32),
        'w_exp_gate': dense(ks[18], (DEPTH, N_EXPERTS, D_MODEL, EXPERT_FF), D_MODEL),
        'w_exp_up': dense(ks[19], (DEPTH, N_EXPERTS, D_MODEL, EXPERT_FF), D_MODEL),
        'w_exp_down': dense(ks[20], (DEPTH, N_EXPERTS, EXPERT_FF, D_MODEL), EXPERT_FF),
        'final_norm_w': gain(ks[21], (D_MODEL,)),
    }


def reference(x, meta_tokens, norm_mix_w, w_in, q_norm_w, w_uq, kv_norm_w, w_ukv, w_o_mla,
              pool_w, pool_scale, w_pool_out, w_out, norm_ffn_w, w_router_group, b_router_group,
              w_router_expert, b_router_expert, w_exp_gate, w_exp_up, w_exp_down, final_norm_w):
    B = x.shape[0]
    meta = jnp.broadcast_to(meta_tokens[None].astype(x.dtype), (B, N_META, D_MODEL))
    h = jnp.concatenate([meta, x], axis=1)
    cos, sin = rope_tables(h.shape[1])
    for layer in range(DEPTH):
        a = rms_norm(h, norm_mix_w[layer])
        h = h + hybrid_mixer(a, w_in[layer], q_norm_w[layer], w_uq[layer], kv_norm_w[layer],
                             w_ukv[layer], w_o_mla[layer], pool_w[layer], pool_scale[layer],
                             w_pool_out[layer], w_out[layer], cos, sin)
        bn = rms_norm(h, norm_ffn_w[layer])
        h = h + hierarchical_moe(bn, w_router_group[layer], b_router_group[layer],
                                 w_router_expert[layer], b_router_expert[layer],
                                 w_exp_gate[layer], w_exp_up[layer], w_exp_down[layer])
    h = rms_norm(h, final_norm_w)
    return h[:, N_META:]
```

```python
import numpy as np
import ml_dtypes
import concourse.bass as bass
import concourse.mybir as mybir
from concourse.bass_utils import run_bass_kernel_spmd

F32 = mybir.dt.float32
BF16 = mybir.dt.bfloat16
I32 = mybir.dt.int32
ALU = mybir.AluOpType
ACTF = mybir.ActivationFunctionType
AX = mybir.AxisListType

NCORES = 8
D = 2048
SEQ = 8192
NMETA = 16
L = SEQ + NMETA
EPS = 1e-6
NH = 16
DN = 128
DR = 64
DV = 128
QL = 512
KVL = 512
NBLK = 64
OWN = 8
NOWN = OWN * 128
NE = 64
FF = 512
CAP = 128
CAPG = 64
NCONVA = 8
NCONV = 28


class _Op:
    __slots__ = ("eng", "fn", "waits", "milestone", "semval", "dma", "idx")

    def __init__(self, eng, fn, dma=None):
        self.eng = eng
        self.fn = fn
        self.waits = []
        self.milestone = False
        self.semval = None
        self.dma = dma
        self.idx = None


class Sched:
    ENGS = ("pe", "act", "dve", "pool", "sp")

    def __init__(self):
        self.ops = {e: [] for e in self.ENGS}
        self.state = {}
        self.dma_cnt = {}
        self.total_sems = {"const", "constp", "const2", "outs"}

    @staticmethod
    def _overlap(a, b):
        n = min(len(a), len(b))
        return a[:n] == b[:n]

    def _entries(self, key):
        d = self.state.get(key[0])
        if not d:
            return []
        return [(k, v) for k, v in d.items() if self._overlap(k, key)]

    def op(self, eng, fn, reads=(), writes=(), dma_sem=None):
        reads = [r if isinstance(r, tuple) else (r,) for r in reads]
        writes = [w if isinstance(w, tuple) else (w,) for w in writes]
        if ("__mem__",) not in writes:
            reads.append(("__mem__",))
        o = _Op(eng, fn)
        if dma_sem is not None:
            dv = self.dma_cnt.get(dma_sem, 0) + 16
            self.dma_cnt[dma_sem] = dv
            o.dma = (dma_sem, dv)
            ev = ("d", dma_sem, dv)
            rkey = ("d", dma_sem)
        else:
            ev = ("c", o)
            rkey = ("c", eng)
        deps = []
        if dma_sem is not None and dma_sem not in self.total_sems and dv > 16:
            deps.append(("d", dma_sem, dv - 16))
        if dma_sem is not None and eng == "pool":
            hist = self.__dict__.setdefault("_swdge_hist", [])
            if len(hist) >= 4 and hist[-4][1] not in self.total_sems:
                deps.append(hist[-4])
            hist.append(ev)
        for r in reads:
            for k, v in self._entries(r):
                if v[0] is not None:
                    deps.append(v[0])
        for w in writes:
            for k, v in self._entries(w):
                if v[0] is not None:
                    deps.append(v[0])
                deps.extend(v[1].values())
        seen = set()
        for d in deps:
            if d[0] == "c":
                p = d[1]
                if p is o:
                    continue
                if p.eng == eng == "pe" and dma_sem is None:
                    continue
                if id(p) in seen:
                    continue
                seen.add(id(p))
                p.milestone = True
                o.waits.append(d)
            else:
                if d in seen:
                    continue
                seen.add(d)
                o.waits.append(d)
        for r in reads:
            dd = self.state.setdefault(r[0], {})
            ent = dd.get(r)
            if ent is None:
                ent = [None, {}]
                dd[r] = ent
            ent[1][rkey] = ev
        for w in writes:
            dd = self.state.setdefault(w[0], {})
            for k in [k for k in dd if len(k) > len(w) and k[:len(w)] == w]:
                del dd[k]
            dd[w] = [ev, {}]
        o.idx = len(self.ops[eng])
        self.ops[eng].append(o)
        return o

    def emit(self, nc, final_waits=()):
        engobj = {"pe": "tensor", "act": "scalar", "dve": "vector", "pool": "gpsimd", "sp": "sync"}
        for e in self.ENGS:
            c = 0
            for o in self.ops[e]:
                if o.milestone and o.dma is None:
                    c += 1
                    o.semval = c
        sems = {}
        for e in self.ENGS:
            sems[("c", e)] = nc.alloc_semaphore(name=f"s_{e}")
        for name in self.dma_cnt:
            sems[("d", name)] = nc.alloc_semaphore(name=f"d_{name}")
        self.sems = sems
        sched = self

        def run_engine(e, eng):
            waited = {}
            for o in sched.ops[e]:
                for d in o.waits:
                    if d[0] == "c":
                        key = ("c", d[1].eng)
                        val = d[1].semval
                    else:
                        key = ("d", d[1])
                        val = d[2]
                        if d[1] in sched.total_sems:
                            val = sched.dma_cnt[d[1]]
                    if waited.get(key, 0) >= val:
                        continue
                    waited[key] = val
                    eng.wait_ge(sems[key], val)
                inst = o.fn(eng)
                if o.dma is not None:
                    inst.then_inc(sems[("d", o.dma[0])], 16)
                elif o.milestone:
                    inst.then_inc(sems[("c", e)], 1)
            for (fe, name) in final_waits:
                if fe == e:
                    eng.wait_ge(sems[("d", name)], sched.dma_cnt[name])

        with nc.Block() as block:
            @block.tensor
            def _(eng):
                run_engine("pe", eng)

            @block.scalar
            def _(eng):
                run_engine("act", eng)

            @block.vector
            def _(eng):
                run_engine("dve", eng)

            @block.gpsimd
            def _(eng):
                run_engine("pool", eng)

            @block.sync
            def _(eng):
                run_engine("sp", eng)


class Builder:
    def __init__(self, stage="full", ngroups=16):
        self.stage = stage
        self.ngroups = ngroups
        self.nc = bass.Bass("TRN2", target_bir_lowering=False)
        self.S = Sched()
        self.nxt = 3
        self.dram = {}
        self._ctx = []
        self._ctxL = []

    def din(self, name, shape, dt=F32):
        t = self.nc.dram_tensor(name, list(shape), dt, kind="ExternalInput")
        self.dram[name] = t
        return t

    def dout(self, name, shape, dt=F32):
        t = self.nc.dram_tensor(name, list(shape), dt, kind="ExternalOutput")
        self.dram[name] = t
        return t

    def sb(self, name, shape, dt, side="right"):
        self._n = getattr(self, "_n", 0) + 1
        cm = self.nc.sbuf_tensor(f"{name}_{self._n}", list(shape), dt, side=side)
        t = cm.__enter__()
        (self._ctxL if side == "left" else self._ctx).append(cm)
        return t

    def markL(self):
        return len(self._ctxL)

    def releaseL(self, mark):
        self.barrier()
        while len(self._ctxL) > mark:
            self._ctxL.pop().__exit__(None, None, None)

    def ps(self, name, shape, dt=F32):
        self._n = getattr(self, "_n", 0) + 1
        cm = self.nc.psum_tensor(f"{name}_{self._n}", list(shape), dt)
        t = cm.__enter__()
        self._ctx.append(cm)
        return t

    def mark(self):
        return len(self._ctx)

    def release(self, mark):
        self.barrier()
        while len(self._ctx) > mark:
            self._ctx.pop().__exit__(None, None, None)

    def barrier(self):
        if not hasattr(self, "_bar"):
            self._bar = self.nc.alloc_sbuf_tensor("bar_scratch", [128, 8], F32) if False else None
        self.S.op("pool", lambda e: e.memset(self.bar_t[:, 0:1], 0.0), [], [("__mem__",), "bar_t"])

    def close(self):
        for cm in reversed(self._ctx):
            cm.__exit__(None, None, None)
        for cm in reversed(self._ctxL):
            cm.__exit__(None, None, None)
        self._ctx = []
        self._ctxL = []

    def dma(self, q, out, in_, reads, writes, sem):
        return self.S.op(q, lambda e: e.dma_start(out=out, in_=in_), reads, writes, dma_sem=sem)

    def mm(self, out, lhsT, rhs, start, stop, reads, writes):
        return self.S.op("pe", lambda e: e.matmul(out, lhsT, rhs, start=start, stop=stop), reads, writes)

    def tr(self, out, in_, ident, reads, writes):
        return self.S.op("pe", lambda e: e.transpose(out, in_, ident), reads, writes)

    def act(self, out, in_, func, reads, writes, **kw):
        return self.S.op("act", lambda e: e.activation(out, in_, func, **kw), reads, writes)

    def tt(self, eng, out, in0, in1, op, reads, writes):
        return self.S.op(eng, lambda e: e.tensor_tensor(out, in0, in1, op), reads, writes)

    def ts(self, eng, out, in0, s1, s2, op0, op1, reads, writes):
        if op1 is None:
            return self.S.op(eng, lambda e: e.tensor_single_scalar(out, in0, s1, op0), reads, writes)
        return self.S.op(eng, lambda e: e.tensor_scalar(out, in0, s1, s2, op0, op1), reads, writes)

    def stt(self, eng, out, in0, scalar, in1, op0, op1, reads, writes):
        return self.S.op(eng, lambda e: e.scalar_tensor_tensor(out, in0, scalar, in1, op0, op1), reads, writes)

    def cp(self, eng, out, in_, reads, writes):
        if eng == "act":
            return self.S.op("act", lambda e: e.copy(out, in_), reads, writes)
        return self.S.op(eng, lambda e: e.tensor_copy(out, in_), reads, writes)


    def setup_common(self):
        nc = self.nc
        self.x_all = self.din("x_all", [L, D])
        self.w_in = self.din("w_in", [D, 6208])
        self.w_ks = self.din("w_ks", [D, DR])
        self.vec_mix = self.din("norm_mix_w", [1, D])
        self.kvn = self.din("kv_norm_w", [128, 4])
        self.qn = self.din("q_norm_w", [128, 4])
        self.cos_all = self.din("cos_all", [DR, L])
        self.sin_all = self.din("sin_all", [DR, L])
        self.x_own = self.din("x_own", [NOWN + 128, D])
        self.cos_own_d = self.din("cos_own", [DR, NOWN])
        self.sin_own_d = self.din("sin_own", [DR, NOWN])
        self.mask_d = self.din("maskT", [128, 8, 128], BF16)
        self.w_uq = self.din("w_uq", [QL, NH * 192])
        self.w_uqs = self.din("w_uqs", [QL, NH * 64])
        self.w_ukv = self.din("w_ukv", [KVL, NH * 256])
        self.pool_scale_d = self.din("pool_scale", [128, 8])
        self.pool_w_d = self.din("pool_w", [1024, 256])
        self.w_o_d = self.din("w_o_mla", [D, D])
        self.w_po_d = self.din("w_pool_out", [1024, D])
        self.w_out_d = self.din("w_out", [D, D])
        self.ustr_d = self.din("ustr", [128, 128], BF16)
        self.iota_d = self.din("iota", [128, CAP])
        self.ident32_d = self.din("ident32", [128, 128])
        self.vec_ffn = self.din("norm_ffn_w", [1, D])
        self.vec_fin = self.din("final_norm_w", [1, D])
        self.w_r_d = self.din("w_r", [D, 72])
        self.b_r_d = self.din("b_r", [1, 72])
        self.w_eg_d = self.din("w_exp_gate", [NE * D, FF])
        self.w_eu_d = self.din("w_exp_up", [NE * D, FF])
        self.w_ed_d = self.din("w_exp_down", [NE * FF, D])
        self.out_d = self.dout("out", [NOWN, D])
        self.seg_bf = self.nc.dram_tensor("seg_bf", [(NCONV + NCONVA) * D, FF], BF16, kind="Internal")
        self.seu_bf = self.nc.dram_tensor("seu_bf", [(NCONV + NCONVA) * D, FF], BF16, kind="Internal")
        self.sed_bf = self.nc.dram_tensor("sed_bf", [(NCONV + NCONVA) * FF, D], BF16, kind="Internal")
        self.cvi = 0
        self.ident_d = self.din("ident_bf", [128, 128], BF16)
        self.ones_d = self.din("ones_bf", [128, 128], BF16)
        self.ident = self.sb("ident", [128, 128], BF16, side="left")
        self.ones = self.sb("ones", [128, 128], BF16, side="left")
        self.kvn_s = self.sb("kvn_s", [128, 4], F32, side="left")
        self.qn_s = self.sb("qn_s", [128, 4], F32, side="left")
        self.bar_t = self.sb("bar_t", [128, 8], F32, side="left")
        self.mkW = self.markL()
        self.wmix = self.sb("wmix", [128, D], F32, side="left")
        S = self.S
        S.total_sems.add("const")
        self.dma("sp", self.ident[:], self.ident_d[:, :], [], ["ident"], "const")
        self.dma("sp", self.ones[:], self.ones_d[:, :], [], ["ones"], "const")
        self.dma("sp", self.wmix[:], self.vec_mix[0:1, :].partition_broadcast(128), [], ["wmix"], "const")
        self.dma("sp", self.kvn_s[:], self.kvn[:, :], [], ["kvn_s"], "const")
        self.dma("sp", self.qn_s[:], self.qn[:, :], [], ["qn_s"], "const")

    def norm_stage1(self, src_rows_ap, ntok, blk_i, wtile, wkey):
        nb = self.nxt
        s = blk_i % nb
        xt = self.xt[s]
        xs = self.xs[blk_i % 2]
        ss = self.ssx
        kx, kxs = ("xt", s), ("xs", blk_i % 2)
        c = blk_i % 8
        kss = ("ssx", c)
        self.dma("sp", xt[0:ntok, :], src_rows_ap, [], [kx], f"xt{s}")
        self.act(xs[0:ntok, :], xt[0:ntok, :], ACTF.Square, [kx], [kxs, kss],
                 accum_out=ss[0:ntok, c:c + 1])
        self.act(ss[0:ntok, 8 + c:9 + c], ss[0:ntok, c:c + 1], ACTF.Sqrt, [kss], [("rsx", c)],
                 bias=EPS, scale=1.0 / D)
        self.S.op("dve", lambda e: e.reciprocal(ss[0:ntok, 16 + c:17 + c], ss[0:ntok, 8 + c:9 + c]),
                  [("rsx", c)], [("rstdx", c)])
        self.stt("dve", xs[0:ntok, :], xt[0:ntok, :], ss[0:ntok, 16 + c:17 + c], wtile[0:ntok, :],
                 ALU.mult, ALU.mult, [kx, ("rstdx", c), wkey], [kxs])

    def norm_stage2(self, ntok, aT, col0, tag, blk_i):
        xs = self.xs[blk_i % 2]
        kxs = ("xs", blk_i % 2)
        for half, eng in ((0, "act"), (1, "dve")):
            tph = self.tp[half]
            ktp = ("tp", half)
            for k8 in range(8):
                kc = half * 8 + k8
                self.tr(tph[:, k8 * 128: k8 * 128 + ntok], xs[0:ntok, kc * 128:(kc + 1) * 128],
                        self.ident[0:ntok, 0:ntok], [kxs, "ident"], [ktp])
            src = tph[:, :].rearrange("p (k t) -> p k t", t=128)[:, :, 0:ntok]
            dst = aT[:, half * 8:(half + 1) * 8, col0:col0 + ntok]
            self.cp(eng, dst, src, [ktp], [(tag, "aT")])

    def norm_pipeline(self, blocks, after_cb=None):
        base = self.blk_i
        n = len(blocks)
        if n == 0:
            return
        self.norm_stage1(blocks[0][0], blocks[0][1], base, self.wmix, "wmix")
        for k in range(n):
            if k + 1 < n:
                self.norm_stage1(blocks[k + 1][0], blocks[k + 1][1], base + k + 1, self.wmix, "wmix")
            src, ntok, aT, col0, tag = blocks[k]
            self.norm_stage2(ntok, aT, col0, tag, base + k)
            if after_cb is not None:
                after_cb(k)
        self.blk_i = base + n

    def alloc_norm(self):
        self.xt = [self.sb(f"xt{i}", [128, D], F32) for i in range(self.nxt)]
        self.xs = [self.sb(f"xs{i}", [128, D], BF16) for i in range(2)]
        self.tp = [self.ps(f"tp{i}", [128, 1024], BF16) for i in range(2)]
        self.ssx = self.sb("ssx", [128, 24], F32)
        self.blk_i = 0

    def alloc_front(self):
        self.alloc_norm()
        self.aTg = [self.sb(f"aTg{i}", [128, 16, 512], BF16) for i in range(2)]
        self.pc = [self.ps(f"pc{i}", [128, 512], F32) for i in range(2)]
        self.ssb = self.ps("ssb", [128, 512], F32)
        self.craw = self.sb("craw", [128, 4, 512], F32)
        self.sq = self.sb("sq", [128, 4, 512], BF16)
        self.rstdb = self.sb("rstdb", [128, 512], F32)
        self.pci = 0
        self.blk_i = 0

    def latent_group(self, aT, tag, n, wsb, wcol0, wkeyf, nscal, nskey, dstf, dkeyf):
        for mt in range(4):
            p = self.pc[self.pci % 2]
            pk = ("pc", self.pci % 2)
            self.pci += 1
            for kc in range(16):
                self.mm(p[:, 0:n], wsb[:, kc, wcol0 + mt * 128: wcol0 + (mt + 1) * 128], aT[:, kc, 0:n],
                        kc == 0, kc == 15, [wkeyf(kc), (tag, "aT")], [pk])
            self.cp("act", self.craw[:, mt, 0:n], p[:, 0:n], [pk], [("craw", mt)])
            self.act(self.sq[:, mt, 0:n], p[:, 0:n], ACTF.Square, [pk], [("sq", mt)])
        for mt in range(4):
            self.mm(self.ssb[:, 0:n], self.ones[:, :], self.sq[:, mt, 0:n], mt == 0, mt == 3,
                    ["ones", ("sq", mt)], ["ssb"])
        rstdb = self.rstdb
        self.act(rstdb[:, 0:n], self.ssb[:, 0:n], ACTF.Sqrt, ["ssb"], ["rstdb"], bias=EPS, scale=1.0 / 512)
        self.S.op("dve", lambda e, n=n: e.reciprocal(rstdb[:, 0:n], rstdb[:, 0:n]), ["rstdb"], ["rstdb"])
        for mt in range(4):
            self.stt("dve", dstf(mt), self.craw[:, mt, 0:n], nscal[:, mt:mt + 1], rstdb[:, 0:n],
                     ALU.mult, ALU.mult, [("craw", mt), nskey, "rstdb"], [dkeyf(mt)])

    def phase_a(self):
        self.mk0 = self.mark()
        self.ckvT = self.sb("ckvT", [128, 4, L], BF16)
        self.kpeT = self.sb("kpeT", [128, L], BF16)
        self.S.op("pool", lambda e: e.memset(self.kpeT[64:128, :], 0.0), [], [("kpeT", "pad")])
        self.cqT = self.sb("cqT", [128, 4, NOWN], BF16)
        self.mkA = self.mark()
        self.wkv = self.sb("wkv", [128, 16, 576], BF16)
        self.wks = self.sb("wks", [128, 16, 64], BF16)
        wks_tmp = self.sb("wks_tmp", [128, 16, 64], F32)
        for kc in range(0, 16, 4):
            self.dma("pool", self.wkv[:, kc:kc + 4, :],
                     self.w_in[kc * 128:(kc + 4) * 128, 512:1088].rearrange("(k p) c -> p k c", p=128),
                     [], [("wkv", kc)], "constp")
        self.dma("sp", wks_tmp[:], self.w_ks.rearrange("(k p) c -> p k c", p=128), [], ["wks_tmp"], "const")
        self.ts("dve", self.wks[:, :, 0:32], wks_tmp[:, :, 0:32], -1.0, None, ALU.mult, None,
                ["wks_tmp"], [("wks", 0)])
        self.cp("dve", self.wks[:, :, 32:64], wks_tmp[:, :, 32:64], ["wks_tmp"], [("wks", 1)])
        self.alloc_front()
        pkr = self.ps("pkr", [128, 512], F32)
        pks = self.ps("pks", [128, 512], F32)
        cs = self.sb("cs", [64, 2, 512], F32)
        t12 = self.sb("t12", [64, 2, 512], F32)

        segs = [(0, NMETA)] + [(NMETA + 512 * g, 512) for g in range(self.ngroups)]

        def chain_mt(gi, mt):
            row0, n = segs[gi]
            aT = self.aTg[gi % 2]
            tag = f"aTg{gi % 2}"
            p = self.pc[self.pci % 2]
            pk = ("pc", self.pci % 2)
            self.pci += 1
            for kc in range(16):
                self.mm(p[:, 0:n], self.wkv[:, kc, mt * 128:(mt + 1) * 128], aT[:, kc, 0:n],
                        kc == 0, kc == 15, [("wkv", kc - kc % 4), (tag, "aT")], [pk])
            self.cp("act", self.craw[:, mt, 0:n], p[:, 0:n], [pk], [("craw", mt)])
            self.act(self.sq[:, mt, 0:n], p[:, 0:n], ACTF.Square, [pk], [("sq", mt)])

        def chain_kr(gi):
            row0, n = segs[gi]
            aT = self.aTg[gi % 2]
            tag = f"aTg{gi % 2}"
            self.dma("sp", cs[:, 0, 0:n], self.cos_all[:, row0:row0 + n], [], [("cs", 0)], "cs0")
            self.dma("sp", cs[:, 1, 0:n], self.sin_all[:, row0:row0 + n], [], [("cs", 1)], "cs1")
            for kc in range(16):
                self.mm(pkr[0:64, 0:n], self.wkv[:, kc, 512:576], aT[:, kc, 0:n], kc == 0, kc == 15,
                        [("wkv", kc - kc % 4), (tag, "aT")], ["pkr"])
            self.tt("dve", t12[:, 0, 0:n], pkr[0:64, 0:n], cs[:, 0, 0:n], ALU.mult,
                    ["pkr", ("cs", 0)], [("t12", 0)])

        def chain_ks(gi):
            row0, n = segs[gi]
            aT = self.aTg[gi % 2]
            tag = f"aTg{gi % 2}"
            for kc in range(16):
                self.mm(pks[0:64, 0:n], self.wks[:, kc, :], aT[:, kc, 0:n], kc == 0, kc == 15,
                        [("wks",), (tag, "aT")], ["pks"])
            self.tt("dve", t12[:, 1, 0:n], pks[0:64, 0:n], cs[:, 1, 0:n], ALU.mult,
                    ["pks", ("cs", 1)], [("t12", 1)])
            self.tt("dve", self.kpeT[0:64, row0:row0 + n], t12[:, 0, 0:n], t12[:, 1, 0:n], ALU.add,
                    [("t12",)], [("kpeT", gi)])

        def finish(gi):
            row0, n = segs[gi]
            rstdb = self.rstdb
            for mt in range(4):
                self.mm(self.ssb[:, 0:n], self.ones[:, :], self.sq[:, mt, 0:n], mt == 0, mt == 3,
                        ["ones", ("sq", mt)], ["ssb"])
            self.act(rstdb[:, 0:n], self.ssb[:, 0:n], ACTF.Sqrt, ["ssb"], ["rstdb"], bias=EPS, scale=1.0 / 512)
            self.S.op("dve", lambda e, n=n: e.reciprocal(rstdb[:, 0:n], rstdb[:, 0:n]), ["rstdb"], ["rstdb"])
            for mt in range(4):
                self.stt("dve", self.ckvT[:, mt, row0:row0 + n], self.craw[:, mt, 0:n], self.kvn_s[:, mt:mt + 1],
                         rstdb[:, 0:n], ALU.mult, ALU.mult, [("craw", mt), "kvn_s", "rstdb"], [("ckvT", gi, mt)])

        blocks = []
        owner = []
        for gi, (row0, n) in enumerate(segs):
            g2 = gi % 2
            for b in range((n + 127) // 128):
                nt = min(128, n - b * 128)
                blocks.append((self.x_all[row0 + b * 128: row0 + b * 128 + nt, :], nt, self.aTg[g2], b * 128,
                               f"aTg{g2}"))
                owner.append((gi, b, (n + 127) // 128))
        pend_parts = []

        def parts_of(gi):
            return [lambda: (chain_mt(gi, 0), chain_kr(gi)),
                    lambda: (chain_mt(gi, 1), chain_ks(gi)),
                    lambda: chain_mt(gi, 2),
                    lambda: (chain_mt(gi, 3), finish(gi))]

        def after(k):
            gi, b, nb = owner[k]
            if self.stage == "full" and b == 0 and gi >= 1 and gi % 2 == 1 and (gi // 2) < NCONVA:
                self.convert_expert(NCONV + gi // 2)
            if pend_parts:
                pend_parts.pop(0)()
            if b == nb - 1:
                while pend_parts:
                    pend_parts.pop(0)()
                pend_parts.extend(parts_of(gi))

        self.norm_pipeline(blocks, after)
        while pend_parts:
            pend_parts.pop(0)()

    def phase_b1(self):
        for kc in range(0, 16, 4):
            self.dma("pool", self.wkv[:, kc:kc + 4, 0:512],
                     self.w_in[kc * 128:(kc + 4) * 128, 0:512].rearrange("(k p) c -> p k c", p=128),
                     [], [("wkv", kc)], "wq_in")
        for g in range(2):
            aT = self.aTg[g]
            tag = f"aTg{g}"
            self.norm_pipeline([(self.x_own[g * 512 + b * 128: g * 512 + (b + 1) * 128, :], 128, aT, b * 128, tag)
                                for b in range(4)])
            self.latent_group(aT, tag, 512, self.wkv, 0, lambda kc: ("wkv", kc - kc % 4), self.qn_s, "qn_s",
                              lambda mt, g=g: self.cqT[:, mt, g * 512:(g + 1) * 512],
                              lambda mt, g=g: ("cqT", g, mt))
        self.release(self.mkA)

    def convert_expert(self, e):
        for (src, dst, rows, key) in ((self.w_eg_d, self.seg_bf, D, "cvg"), (self.w_eu_d, self.seu_bf, D, "cvu"),
                                      (self.w_ed_d, self.sed_bf, FF, "cvd")):
            j = self.cvi % 6
            self.cvi += 1
            self.dma("pool", dst[e * rows:(e + 1) * rows, :].rearrange("(p r) c -> p r c", p=128),
                     src[e * rows:(e + 1) * rows, :].rearrange("(p r) c -> p r c", p=128),
                     [], [(key, e)], f"cv{j}")

    def phase_attn(self, nheads=NH):
        mk = self.mk0
        self.mkL0 = self.markL()
        self.OT = self.sb("OT", [128, NH, NOWN], BF16, side="left")
        cso = self.sb("cso", [64, 2, NOWN], F32)
        self.dma("sp", cso[:, 0, :], self.cos_own_d[:, :], [], [("cso", 0)], "const2")
        self.dma("sp", cso[:, 1, :], self.sin_own_d[:, :], [], [("cso", 1)], "const2")
        maskT = self.sb("maskT_s", [128, 8, 128], BF16)
        self.dma("sp", maskT[:], self.mask_d[:, :, :], [], ["maskT"], "const2")
        ones32 = self.sb("ones32", [128, 128], F32)
        self.S.op("pool", lambda e: e.memset(ones32[:], 1.0), [], ["ones32"])
        self.S.total_sems.add("const2")
        wq = [self.sb(f"wq{i}", [128, 4, 192], BF16) for i in range(2)]
        wqs_t = [self.sb(f"wqs_t{i}", [128, 4, 64], F32) for i in range(2)]
        wqs = [self.sb(f"wqs{i}", [128, 4, 64], BF16) for i in range(2)]
        wkv_h = [self.sb(f"wkvh{i}", [128, 4, 256], BF16) for i in range(2)]
        KhT = self.sb("KhT", [128, L], BF16)
        Vh = self.sb("Vh", [128, NBLK + 1, 128], BF16)
        Qn = self.sb("Qn", [128, NOWN], BF16)
        Qpe = self.sb("Qpe", [128, NOWN], BF16)
        self.S.op("pool", lambda e: e.memset(Qpe[64:128, :], 0.0), [], [("Qpe", "pad")])
        q12 = self.sb("q12", [64, 2, 512], F32)
        acc = self.sb("acc", [128, NOWN], F32)
        rcp = self.sb("rcp", [128, NOWN], F32)
        NPT = 6
        NST = 4
        pt = [self.sb(f"pt{i}", [128, 512], BF16) for i in range(NPT)]
        st = [self.ps(f"st{i}", [128, 512], F32) for i in range(NST)]
        ot = [self.ps(f"ot{i}", [128, 512], F32) for i in range(2)]
        gen = [self.ps(f"gen{i}", [128, 512], F32) for i in range(2)]
        rs = gen
        geni = 0
        sti = 0
        pti = 0
        scale = float((DN + DR) ** -0.5)

        def load_head_w(h):
            b = h % 2
            with_keys = [("wq", b), ("wqs_t", b), ("wkvh", b)]
            self.dma("pool", wq[b][:], self.w_uq[:, h * 192:(h + 1) * 192].rearrange("(k p) c -> p k c", p=128),
                     [], [("wq", b)], f"whq{b}")
            self.dma("sp", wqs_t[b][:], self.w_uqs[:, h * 64:(h + 1) * 64].rearrange("(k p) c -> p k c", p=128),
                     [], [("wqs_t", b)], f"whs{b}")
            self.dma("pool", wkv_h[b][:], self.w_ukv[:, h * 256:(h + 1) * 256].rearrange("(k p) c -> p k c", p=128),
                     [], [("wkvh", b)], f"whk{b}")

        load_head_w(0)
        for h in range(nheads):
            b = h % 2
            if h + 1 < nheads:
                load_head_w(h + 1)
            if self.stage == "full":
                n0 = min(2 * h, 24 + max(0, h - 12))
                n1 = min(2 * h + 2, 24 + max(0, h + 1 - 12))
                for e_ in range(n0, min(n1, NCONV)):
                    self.convert_expert(e_)
            self.ts("dve", wqs[b][:, :, 0:32], wqs_t[b][:, :, 0:32], -1.0, None, ALU.mult, None,
                    [("wqs_t", b)], [("wqs", b, 0)])
            self.cp("dve", wqs[b][:, :, 32:64], wqs_t[b][:, :, 32:64], [("wqs_t", b)], [("wqs", b, 1)])
            for g in range(2):
                cols = slice(g * 512, (g + 1) * 512)
                p = gen[geni % 2]; pk = ("gen", geni % 2); geni += 1
                for kc in range(4):
                    self.mm(p[:, :], wq[b][:, kc, 0:128], self.cqT[:, kc, cols], kc == 0, kc == 3,
                            [("wq", b), ("cqT",)], [pk])
                self.cp("act", Qn[:, cols], p[:, :], [pk], [("Qn", g)])
                p1 = gen[geni % 2]; pk1 = ("gen", geni % 2); geni += 1
                for kc in range(4):
                    self.mm(p1[0:64, :], wq[b][:, kc, 128:192], self.cqT[:, kc, cols], kc == 0, kc == 3,
                            [("wq", b), ("cqT",)], [pk1])
                self.tt("dve", q12[:, 0, :], p1[0:64, :], cso[:, 0, cols], ALU.mult, [pk1, ("cso", 0)], [("q12", 0)])
                p2 = gen[geni % 2]; pk2 = ("gen", geni % 2); geni += 1
                for kc in range(4):
                    self.mm(p2[0:64, :], wqs[b][:, kc, :], self.cqT[:, kc, cols], kc == 0, kc == 3,
                            [("wqs", b), ("cqT",)], [pk2])
                self.tt("dve", q12[:, 1, :], p2[0:64, :], cso[:, 1, cols], ALU.mult, [pk2, ("cso", 1)], [("q12", 1)])
                self.tt("dve", Qpe[0:64, cols], q12[:, 0, :], q12[:, 1, :], ALU.add, [("q12",)], [("Qpe", g)])
            segs = [(0, NMETA)] + [(NMETA + 512 * g, 512) for g in range(16)]
            for si, (c0, n) in enumerate(segs):
                p = gen[geni % 2]; pk = ("gen", geni % 2); geni += 1
                for kc in range(4):
                    self.mm(p[:, 0:n], wkv_h[b][:, kc, 0:128], self.ckvT[:, kc, c0:c0 + n], kc == 0, kc == 3,
                            [("wkvh", b), ("ckvT",)], [pk])
                self.cp("act" if si % 2 else "dve", KhT[:, c0:c0 + n], p[:, 0:n], [pk], [("KhT", si)])
            p = gen[geni % 2]; pk = ("gen", geni % 2); geni += 1
            for kc in range(4):
                self.mm(p[0:NMETA, 0:128], self.ckvT[:, kc, 0:NMETA], wkv_h[b][:, kc, 128:256], kc == 0, kc == 3,
                        [("wkvh", b), ("ckvT",)], [pk])
            self.cp("act", Vh[0:NMETA, 0, :], p[0:NMETA, 0:128], [pk], [("Vh", 0)])
            for j4 in range(16):
                p = gen[geni % 2]; pk = ("gen", geni % 2); geni += 1
                for jj in range(4):
                    j = j4 * 4 + jj
                    c0 = NMETA + 128 * j
                    for kc in range(4):
                        self.mm(p[:, jj * 128:(jj + 1) * 128], self.ckvT[:, kc, c0:c0 + 128],
                                wkv_h[b][:, kc, 128:256], kc == 0, kc == 3, [("wkvh", b), ("ckvT",)], [pk])
                self.cp("act" if j4 % 2 else "dve", Vh[:, 1 + j4 * 4: 5 + j4 * 4, :],
                        p[:, :].rearrange("p (j d) -> p j d", d=128), [pk], [("Vh", 1 + j4)])
            chunks = [(-1, 0)] + [(sc, r) for sc in range(8) for r in range(8)]
            plist = []
            for ci, (sc, r) in enumerate(chunks):
                if sc < 0:
                    nk, kc0, vch, q0 = NMETA, 0, 0, 0
                else:
                    j = sc * 8 + r
                    nk, kc0, vch, q0 = 128, NMETA + 128 * j, 1 + j, 128 * sc
                if q0 < 512:
                    plist.append((sc, r, nk, kc0, vch, q0, q0, 512, 0, ci))
                    plist.append((sc, r, nk, kc0, vch, q0, 512, 1024, 1, ci))
                else:
                    plist.append((sc, r, nk, kc0, vch, q0, q0, 1024, 1, ci))
            pend = []
            DEPTH = 3
            self.S.op("dve", lambda e: e.memset(acc[:], 0.0), [], [("acc",)])
            started = [False, False]
            lastidx = [max(i for i, p_ in enumerate(plist) if p_[8] == bk) for bk in range(2)]
            for pidx, (sc, r, nk, kc0, vch, q0, qa, qb, bank, ci) in enumerate(plist):
                nq = qb - qa
                sp_ = st[sti % NST]; sk = ("st", sti % NST); sti += 1
                self.mm(sp_[0:nk, 0:nq], KhT[:, kc0:kc0 + nk], Qn[:, qa:qb], True, False,
                        [("KhT",), ("Qn",)], [sk])
                self.mm(sp_[0:nk, 0:nq], self.kpeT[:, kc0:kc0 + nk], Qpe[:, qa:qb], False, True,
                        [("kpeT",), ("Qpe",)], [sk])
                if len(pend) >= DEPTH:
                    pend.pop(0)()
                pp = pt[pti % NPT]; pkey = ("pt", pti % NPT); pti += 1
                self.act(pp[0:nk, 0:nq], sp_[0:nk, 0:nq], ACTF.Exp, [sk], [pkey], scale=scale)
                if sc >= 0 and qa == q0:
                    self.tt("dve", pp[:, 0:128], pp[:, 0:128], maskT[:, r, :], ALU.mult,
                            [pkey, "maskT"], [pkey])
                use_dve = (pidx % 2 == 1)
                if use_dve:
                    self.tt("dve", acc[0:nk, qa:qb], acc[0:nk, qa:qb], pp[0:nk, 0:nq], ALU.add,
                            [pkey, ("acc", bank)], [("acc", bank)])
                    first_pe = False
                else:
                    first_pe = not started[bank]
                    started[bank] = True

                def pv(bank=bank, qa=qa, qb=qb, nk=nk, vch=vch, pp=pp, nq=nq, sc=sc, pkey=pkey,
                       use_dve=use_dve, first_pe=first_pe, lastp=(pidx == lastidx[bank])):
                    self.mm(ot[bank][:, qa - 512 * bank: qb - 512 * bank], Vh[0:nk, vch, :], pp[0:nk, 0:nq],
                            sc < 0, lastp, [("Vh",), pkey], [("ot", bank)])
                    if not use_dve:
                        self.mm(gen[bank][:, qa - 512 * bank: qb - 512 * bank], self.ones[0:nk, :], pp[0:nk, 0:nq],
                                first_pe, False, ["ones", pkey], [("gen", bank)])
                pend.append(pv)
            while pend:
                pend.pop(0)()
            for bank in range(2):
                cols = slice(bank * 512, (bank + 1) * 512)
                self.mm(gen[bank][:, :], ones32[:, :], acc[:, cols], False, True, ["ones32", ("acc", bank)],
                        [("gen", bank)])
            for bank in range(2):
                cols = slice(bank * 512, (bank + 1) * 512)
                self.S.op("dve", lambda e, bank=bank, cols=cols: e.reciprocal(rcp[:, cols], rs[bank][:, :]),
                          [("gen", bank)], [("rcp", bank)])
                self.tt("dve", self.OT[:, h, cols], ot[bank][:, :], rcp[:, cols], ALU.mult,
                        [("ot", bank), ("rcp", bank)], [("OT", h, bank)])
        self.release(mk)

    def debug_dump_attn(self):
        o1 = self.dout("dbg_OT", [128, NH, NOWN], BF16)
        self.dma("sp", o1[:, :, :], self.OT[:], [("OT",)], [], "outs")

    def phase_mix(self):
        mkR = self.mark()
        aTo = self.sb("aTo", [128, 16, NOWN + 128], BF16)
        mixedT = self.sb("mixedT", [128, 8, NOWN], BF16)
        psc = self.sb("psc", [128, 8], F32)
        self.dma("sp", psc[:], self.pool_scale_d[:, :], [], ["psc"], "psc")
        mk2 = self.mark()
        self.alloc_norm()
        self.norm_pipeline([(self.x_own[b * 128:(b + 1) * 128, :], 128, aTo, b * 128, "aTo") for b in range(9)])
        self.release(mk2)
        if getattr(self, "stop", 9) == 1:
            return
        wpool = self.sb("wpool", [128, 16, 1024], BF16)
        for kc in range(0, 16, 4):
            for hh in range(2):
                self.dma("pool", wpool[:, kc:kc + 4, hh * 512:(hh + 1) * 512],
                         self.w_in[kc * 128:(kc + 4) * 128, 1088 + hh * 512:1088 + (hh + 1) * 512].rearrange("(k p) c -> p k c", p=128),
                         [], [("wpool", kc, hh)], f"wpool{kc}_{hh}")
        poolw = self.sb("poolw", [128, 4, 2, 256], BF16)
        for g in range(4):
            self.dma("pool", poolw[:, g, :, :], self.pool_w_d[g * 256:(g + 1) * 256, :].rearrange("(k p) d -> p k d", p=128),
                     [], [("poolw", g)], f"poolw{g}")
        ub = self.sb("ub", [128, 8, 144], F32)
        sA = self.sb("sA", [128, 8, 144], F32)
        sB = self.sb("sB", [128, 8, 144], F32)
        pooledT = self.sb("pooledT", [128, 8, NOWN], BF16)
        up = [self.ps(f"up{i}", [128, 512], F32) for i in range(3)]
        mx = [self.ps(f"mx{i}", [128, 512], F32) for i in range(2)]
        npieces = [(0, 512), (512, 512), (1024, 128)]
        for c in range(8 if getattr(self, "sub", 9) >= 1 else 0):
            for pi, (c0, n) in enumerate(npieces):
                for kc in range(16):
                    self.mm(up[pi][:, 0:n], wpool[:, kc, c * 128:(c + 1) * 128], aTo[:, kc, c0:c0 + n],
                            kc == 0, kc == 15, [("wpool", kc - kc % 4), ("aTo",)], [("up", pi)])
            self.cp("act", ub[:, 0:4, 16:144], up[0][:, :].rearrange("p (m t) -> p m t", t=128), [("up", 0)], [("ub", 0)])
            self.cp("act", ub[:, 4:8, 16:144], up[1][:, :].rearrange("p (m t) -> p m t", t=128), [("up", 1)], [("ub", 1)])
            self.cp("act", ub[:, :, 0:16], up[2][:, 0:128].rearrange("p (m t) -> p m t", t=16), [("up", 2)], [("ub", 2)])
            if getattr(self, "sub", 9) < 2:
                continue
            g = c // 2
            srcs = [ub, sA, sB, sA, sB]
            keys = ["ub", "sA", "sB", "sA", "sB"]
            d = 1
            lo = 0
            for step in range(g + 1):
                src, dst = srcs[step], srcs[step + 1]
                lo2 = lo + d
                self.tt("pool", dst[:, :, lo2:144], src[:, :, lo2:144], src[:, :, lo:144 - d], ALU.add,
                        [(keys[step],)], [(keys[step + 1],)])
                lo = lo2
                d *= 2
            fin = srcs[g + 1]
            self.stt("dve", pooledT[:, c, :].rearrange("p (m t) -> p m t", t=128), fin[:, :, 16:144], 1.0 / d,
                     ub[:, :, 16:144], ALU.mult, ALU.subtract, [(keys[g + 1],), ("ub",)], [("pooledT", c)])
        for g in range(4 if getattr(self, "sub", 9) >= 3 else 0):
            for dl in range(2):
                dc = 2 * g + dl
                for nh in range(2):
                    cols = slice(nh * 512, (nh + 1) * 512)
                    p = mx[(dc * 2 + nh) % 2]; pk = ("mx", (dc * 2 + nh) % 2)
                    for cc in range(2):
                        self.mm(p[:, :], poolw[:, g, cc, dl * 128:(dl + 1) * 128], pooledT[:, 2 * g + cc, cols],
                                cc == 0, cc == 1, [("poolw",), ("pooledT",)], [pk])
                    self.ts("dve", mixedT[:, dc, cols], p[:, :], psc[:, dc:dc + 1], None, ALU.mult, None,
                            [pk, "psc"], [("mixedT", dc, nh)])
        self.release(mk2)
        if getattr(self, "stop", 9) == 2:
            return
        self.mergedT = self.sb("mergedT", [128, 16, NOWN], BF16, side="left")
        CW = 256
        NCG = D // CW
        wgm = [self.sb(f"wgm{i}", [128, 16, CW], BF16) for i in range(2)]
        wgp = [self.sb(f"wgp{i}", [128, 16, CW], BF16) for i in range(2)]
        wo = [self.sb(f"wo{i}", [128, 16, CW], BF16) for i in range(2)]
        wpo = [self.sb(f"wpo{i}", [128, 8, CW], BF16) for i in range(2)]
        sg = [self.sb(f"sg{i}", [128, 2, 512], F32) for i in range(2)]
        tm = [self.sb(f"tm{i}", [128, 2, 512], F32) for i in range(2)]
        pg = [[self.ps(f"pg{i}_{j}", [128, 512], F32) for j in range(4)] for i in range(2)]

        def load_cg(cg):
            wb = cg % 2
            csl = slice(cg * CW, (cg + 1) * CW)
            for kc in range(0, 16, 8):
                self.dma("pool", wgm[wb][:, kc:kc + 8, :],
                         self.w_in[kc * 128:(kc + 8) * 128, 2112 + cg * CW: 2112 + (cg + 1) * CW].rearrange("(k p) c -> p k c", p=128),
                         [], [("wgm", wb, kc)], f"wgm{wb}_{kc}")
                self.dma("pool", wgp[wb][:, kc:kc + 8, :],
                         self.w_in[kc * 128:(kc + 8) * 128, 4160 + cg * CW: 4160 + (cg + 1) * CW].rearrange("(k p) c -> p k c", p=128),
                         [], [("wgp", wb, kc)], f"wgp{wb}_{kc}")
                self.dma("pool", wo[wb][:, kc:kc + 8, :],
                         self.w_o_d[kc * 128:(kc + 8) * 128, csl].rearrange("(k p) c -> p k c", p=128),
                         [], [("wo", wb, kc)], f"wo{wb}_{kc}")
            self.dma("pool", wpo[wb][:, :, :],
                     self.w_po_d[:, csl].rearrange("(k p) c -> p k c", p=128),
                     [], [("wpo", wb)], f"wpo{wb}")

        it = 0
        load_cg(0)
        for cg in range(NCG):
            wb = cg % 2
            if cg + 1 < NCG:
                load_cg(cg + 1)
            for mt in range(CW // 128):
                f = cg * (CW // 128) + mt
                msl = slice(mt * 128, (mt + 1) * 128)
                for nh in range(2):
                    cols = slice(nh * 512, (nh + 1) * 512)
                    i2 = it % 2
                    it += 1
                    P = pg[i2]
                    for kc in range(16):
                        self.mm(P[0][:, :], wgm[wb][:, kc, msl], aTo[:, kc, cols], kc == 0, kc == 15,
                                [("wgm", wb, kc - kc % 8), ("aTo",)], [("pg", i2, 0)])
                    for kc in range(16):
                        self.mm(P[1][:, :], wgp[wb][:, kc, msl], aTo[:, kc, cols], kc == 0, kc == 15,
                                [("wgp", wb, kc - kc % 8), ("aTo",)], [("pg", i2, 1)])
                    for kc in range(16):
                        self.mm(P[2][:, :], wo[wb][:, kc, msl], self.OT[:, kc, cols], kc == 0, kc == 15,
                                [("wo", wb, kc - kc % 8), ("OT",)], [("pg", i2, 2)])
                    for kc in range(8):
                        self.mm(P[3][:, :], wpo[wb][:, kc, msl], mixedT[:, kc, cols], kc == 0, kc == 7,
                                [("wpo", wb), ("mixedT",)], [("pg", i2, 3)])
                    self.act(sg[i2][:, 0, :], P[0][:, :], ACTF.Sigmoid, [("pg", i2, 0)], [("sg", i2, 0)])
                    self.act(sg[i2][:, 1, :], P[1][:, :], ACTF.Sigmoid, [("pg", i2, 1)], [("sg", i2, 1)])
                    self.tt("dve", tm[i2][:, 0, :], P[2][:, :], sg[i2][:, 0, :], ALU.mult,
                            [("pg", i2, 2), ("sg", i2, 0)], [("tm", i2, 0)])
                    self.tt("dve", tm[i2][:, 1, :], P[3][:, :], sg[i2][:, 1, :], ALU.mult,
                            [("pg", i2, 3), ("sg", i2, 1)], [("tm", i2, 1)])
                    self.tt("dve", self.mergedT[:, f, cols], tm[i2][:, 0, :], tm[i2][:, 1, :], ALU.add,
                            [("tm", i2)], [("mergedT", f, nh)])
        self.release(mkR)
        if getattr(self, "stop", 9) == 3:
            return
        self.h2 = self.sb("h2", [128, OWN, D], F32)
        self.mkH = self.mark()
        self.dma("sp", self.h2[:], self.x_own[0:NOWN, :].rearrange("(m p) d -> p m d", p=128), [], [("h2",)], "h2ld")
        wout = [self.sb(f"wout{i}", [128, 16, 512], BF16) for i in range(2)]
        po = [self.ps(f"po{i}", [128, 512], F32) for i in range(4)]
        it = 0
        for cg in range(4):
            c512 = slice(cg * 512, (cg + 1) * 512)
            w = wout[cg % 2]
            for kc in range(0, 16, 4):
                self.dma("pool", w[:, kc:kc + 4, :],
                         self.w_out_d[kc * 128:(kc + 4) * 128, c512].rearrange("(k p) c -> p k c", p=128),
                         [], [("wout", cg % 2, kc)], f"wout{cg % 2}_{kc}")
            for m in range(OWN):
                p = po[it % 4]; pk = ("po", it % 4); it += 1
                for kc in range(16):
                    self.mm(p[:, :], self.mergedT[:, kc, m * 128:(m + 1) * 128], w[:, kc, :], kc == 0, kc == 15,
                            [("mergedT",), ("wout", cg % 2, kc - kc % 4)], [pk])
                self.tt("dve", self.h2[:, m, c512], p[:, :], self.h2[:, m, c512], ALU.add,
                        [pk, ("h2", m, cg)], [("h2", m, cg)])
        self.release(self.mkH)
        self.releaseL(self.mkW)

    def debug_dump_h2(self):
        o1 = self.dout("dbg_h2", [128, OWN, D], F32)
        self.dma("sp", o1[:, :, :], self.h2[:], [("h2",)], [], "outs")

    def phase_moe(self, nexp=NE):
        BIG = 30000.0
        h2 = self.h2
        bn = self.sb("bn", [128, OWN, D], BF16)
        Cw = self.sb("Cw", [128, OWN, NE], F32)
        pos = self.sb("pos", [128, OWN, NE], F32)
        pos64 = self.sb("pos64", [128, OWN, NE], F32)
        iota = self.sb("iota", [128, CAP], F32)
        self.dma("sp", iota[:], self.iota_d[:, :], [], ["iota"], "c4b")
        mkR = self.mark()
        selA = self.sb("selA", [128, OWN, NE], F32)
        selB = self.sb("selB", [128, OWN, NE], BF16)
        ustr = self.sb("ustr", [128, 128], BF16)
        ident32 = self.sb("ident32", [128, 128], F32)
        self.dma("sp", ustr[:], self.ustr_d[:, :], [], ["ustr"], "c4a")
        self.dma("sp", ident32[:], self.ident32_d[:, :], [], ["ident32"], "c4c")
        wffn = self.sb("wffn", [128, D], F32)
        self.dma("sp", wffn[:], self.vec_ffn[0:1, :].partition_broadcast(128), [], ["wffn"], "c4d")
        wr32 = self.sb("wr32", [128, 16, 72], F32)
        self.dma("sp", wr32[:], self.w_r_d.rearrange("(k p) c -> p k c", p=128), [], ["wr32"], "c4e")
        brt = self.sb("brt", [128, 72], F32)
        self.dma("sp", brt[:], self.b_r_d[0:1, :].partition_broadcast(128), [], ["brt"], "c4f")
        bn32s = [self.sb(f"bn32_{i}", [128, D], F32) for i in range(2)]
        bnT32 = self.sb("bnT32", [128, 16, 128], F32)
        rts = [self.sb(f"rt{i}", [128, 64], F32) for i in range(2)]
        lgs = [self.sb(f"lg{i}", [128, 72], F32) for i in range(2)]
        gm = self.sb("gm", [128, 8], F32)
        gex = self.sb("gex", [128, 8], F32)
        pen = self.sb("pen", [128, 8], F32)
        elms = [self.sb(f"elm{i}", [128, NE], F32) for i in range(2)]
        ee = self.sb("ee", [128, NE], F32)
        c0 = self.sb("c0", [128, NE], F32)
        top8 = self.sb("top8", [128, 8], F32)
        tpr = [self.ps(f"tpr{i}", [128, 512], F32) for i in range(2)]
        lgp = [self.ps(f"lgp{i}", [128, 512], F32) for i in range(2)]
        posp = self.ps("posp", [128, 512], F32)

        def stage_a(m):
            i2 = m % 2
            rt = rts[i2]; bn32 = bn32s[i2]
            self.act(bn[:, m, :], h2[:, m, :], ACTF.Square, [("h2", m)], [("bn", m), ("rt", i2, 0)],
                     accum_out=rt[:, 0:1])
            self.act(rt[:, 1:2], rt[:, 0:1], ACTF.Sqrt, [("rt", i2, 0)], [("rt", i2, 1)], bias=EPS, scale=1.0 / D)
            self.S.op("dve", lambda e: e.reciprocal(rt[:, 2:3], rt[:, 1:2]), [("rt", i2, 1)], [("rt", i2, 2)])
            self.stt("dve", bn32[:, :], h2[:, m, :], rt[:, 2:3], wffn[:, :], ALU.mult, ALU.mult,
                     [("h2", m), ("rt", i2, 2), "wffn"], [("bn32", i2)])
            self.cp("act", bn[:, m, :], bn32[:, :], [("bn32", i2)], [("bn", m)])

        def stage_b(m):
            i2 = m % 2
            rt = rts[i2]; bn32 = bn32s[i2]; lg = lgs[i2]; elm = elms[i2]
            for q in range(4):
                t = tpr[q % 2]
                for i in range(4):
                    kc = q * 4 + i
                    self.tr(t[:, i * 128:(i + 1) * 128], bn32[:, kc * 128:(kc + 1) * 128], ident32[:, :],
                            [("bn32", i2), "ident32"], [("tpr", q % 2)])
                self.cp("act" if q % 2 else "dve", bnT32[:, q * 4:(q + 1) * 4, :],
                        t[:, :].rearrange("p (k t) -> p k t", t=128), [("tpr", q % 2)], [("bnT32", q)])
            lp = lgp[i2]
            for kc in range(16):
                self.mm(lp[:, 0:72], bnT32[:, kc, :], wr32[:, kc, :], kc == 0, kc == 15,
                        [("bnT32", kc // 4), "wr32"], [("lgp", i2)])
            self.tt("dve", lg[:, :], lp[:, 0:72], brt[:, :], ALU.add, [("lgp", i2), "brt"], [("lg", i2)])
            self.S.op("dve", lambda e: e.tensor_reduce(rt[:, 3:4], lg[:, 0:8], AX.X, ALU.max), [("lg", i2)],
                      [("rt", i2, 3)])
            self.ts("dve", gm[:, :], lg[:, 0:8], rt[:, 3:4], None, ALU.is_ge, None, [("lg", i2), ("rt", i2, 3)], ["gm"])
            self.ts("dve", rt[:, 4:5], rt[:, 3:4], -1.0, None, ALU.mult, None, [("rt", i2, 3)], [("rt", i2, 4)])
            self.act(gex[:, :], lg[:, 0:8], ACTF.Exp, [("lg", i2), ("rt", i2, 4)], ["gex", ("rt", i2, 5)],
                     bias=rt[:, 4:5], accum_out=rt[:, 5:6])
            self.ts("dve", pen[:, :], gm[:, :], BIG, -BIG, ALU.mult, ALU.add, ["gm"], ["pen"])
            for g in range(8):
                self.ts("dve", elm[:, g * 8:(g + 1) * 8], lg[:, 8 + g * 8: 16 + g * 8], pen[:, g:g + 1], None,
                        ALU.add, None, [("lg", i2), "pen"], [("elm", i2, g)])
            self.S.op("dve", lambda e: e.max(out=top8[:, :], in_=elm[:, :]), [("elm", i2)], ["top8"])
            self.ts("dve", selA[:, m, :], elm[:, :], top8[:, 1:2], None, ALU.is_ge, None, [("elm", i2), "top8"],
                    [("selA", m)])
            self.cp("dve", selB[:, m, :], selA[:, m, :], [("selA", m)], [("selB", m)])
            self.ts("dve", rt[:, 6:7], top8[:, 0:1], -1.0, None, ALU.mult, None, ["top8"], [("rt", i2, 6)])
            self.act(ee[:, :], elm[:, :], ACTF.Exp, [("elm", i2), ("rt", i2, 6)], ["ee"], bias=rt[:, 6:7])
            self.tt("dve", c0[:, :], selA[:, m, :], ee[:, :], ALU.mult, [("selA", m), "ee"], ["c0"])
            self.S.op("dve", lambda e: e.tensor_reduce(rt[:, 7:8], c0[:, :], AX.X, ALU.add), ["c0"], [("rt", i2, 7)])
            self.tt("dve", rt[:, 8:9], rt[:, 7:8], rt[:, 5:6], ALU.mult, [("rt", i2, 7), ("rt", i2, 5)], [("rt", i2, 8)])
            self.S.op("dve", lambda e: e.reciprocal(rt[:, 9:10], rt[:, 8:9]), [("rt", i2, 8)], [("rt", i2, 9)])
            self.ts("dve", Cw[:, m, :], c0[:, :], rt[:, 9:10], None, ALU.mult, None, ["c0", ("rt", i2, 9)], [("Cw", m)])
            self.mm(posp[:, 0:NE], ustr[:, :], selB[:, m, :], True, m == 0, ["ustr", ("selB", m)], ["posp"])
            for mp in range(m):
                self.mm(posp[:, 0:NE], self.ones[:, :], selB[:, mp, :], False, mp == m - 1,
                        ["ones", ("selB", mp)], ["posp"])
            self.stt("dve", pos[:, m, :], posp[:, 0:NE], 1.0, selA[:, m, :], ALU.add, ALU.mult,
                     ["posp", ("selA", m)], [("pos", m)])
            self.ts("dve", pos[:, m, :], pos[:, m, :], -1.0, None, ALU.add, None, [("pos", m)], [("pos", m)])
            self.ts("dve", pos64[:, m, :], pos[:, m, :], 64.0, None, ALU.add, None, [("pos", m)], [("pos64", m)])

        stage_a(0)
        for m in range(OWN):
            if m + 1 < OWN:
                stage_a(m + 1)
            stage_b(m)
        self.release(mkR)
        if self.stage == "route":
            return Cw, pos
        wg = [self.sb(f"wg{i}", [128, 16, FF], BF16) for i in range(2)]
        wu = [self.sb(f"wu{i}", [128, 16, FF], BF16) for i in range(2)]
        wd = self.sb("wd", [128, 4, D], BF16)
        SelE = self.sb("SelE", [128, OWN, CAPG], BF16)
        SelW = self.sb("SelW", [128, 2, CAP], BF16)
        SelWT = [self.sb(f"SelWT{i}", [128, OWN, 128], BF16) for i in range(2)]
        xeT = self.sb("xeT", [128, 16, CAP], BF16)
        self.S.op("dve", lambda e: e.memset(xeT[:], 0.0), [], [("xeT",)])
        hdn = self.sb("hdn", [128, FF], BF16)
        hdnT = self.sb("hdnT", [128, 4, 128], BF16)
        ye = [self.sb(f"ye{i}", [128, D], BF16) for i in range(2)]
        for yt_ in ye:
            self.S.op("dve", lambda e, yt_=yt_: e.memset(yt_[:], 0.0), [], [("ye",)])
        gx = [self.ps(f"gx{i}", [128, 512], F32) for i in range(2)]
        fy = [self.ps(f"fy{i}", [128, 512], F32) for i in range(3)]
        tq = self.ps("tq", [128, 1024], BF16)
        scp = [self.ps(f"scp{i}", [128, 512], F32) for i in range(2)]

        conv = (self.stage == "full")

        def load_gu(i):
            e = order[i]
            b = i % 2
            pre = conv and e < NCONV + NCONVA
            q = "sp" if pre else "pool"
            sg_, su_ = (self.seg_bf, self.seu_bf) if pre else (self.w_eg_d, self.w_eu_d)
            for kc in range(0, 16, 4):
                r0 = e * D + kc * 128
                self.dma(q, wg[b][:, kc:kc + 4, :],
                         sg_[r0:r0 + 512, :].rearrange("(k p) c -> p k c", p=128),
                         [("cvg", e)] if pre else [], [("wg", b, kc)], f"wg{b}_{kc}{q}")
                self.dma(q, wu[b][:, kc:kc + 4, :],
                         su_[r0:r0 + 512, :].rearrange("(k p) c -> p k c", p=128),
                         [("cvu", e)] if pre else [], [("wu", b, kc)], f"wu{b}_{kc}{q}")

        def load_d(i):
            e = order[i]
            pre = conv and e < NCONV + NCONVA
            q = "sp" if pre else "pool"
            sd_ = self.sed_bf if pre else self.w_ed_d
            for hh in range(2):
                self.dma(q, wd[:, :, hh * 1024:(hh + 1) * 1024],
                         sd_[e * FF:(e + 1) * FF, hh * 1024:(hh + 1) * 1024].rearrange("(k p) c -> p k c", p=128),
                         [("cvd", e)] if pre else [], [("wd", hh)], f"wd{hh}{q}")

        if conv:
            la, lb = list(range(NCONV + NCONVA)), list(range(NCONV + NCONVA, NE))
            order = []
            for i in range(max(len(la), len(lb))):
                if i < len(la):
                    order.append(la[i])
                if i < len(lb):
                    order.append(lb[i])
            assert sorted(order) == list(range(NE))
        else:
            order = list(range(nexp))
        pending = []
        state = {"sci": 0}

        def drain(k=1):
            for _ in range(k):
                if pending:
                    pending.pop(0)()

        def make_scatter(pi, m, cgp):
            c512 = slice(cgp * 512, (cgp + 1) * 512)
            rb = pi % 2

            def f():
                i = state["sci"] % 2
                state["sci"] += 1
                p = scp[i]; pk = ("scp", i)
                self.mm(p[:, :], SelWT[rb][:, m, :], ye[rb][:, c512], True, True,
                        [("SelWT", rb), ("ye", rb, cgp)], [pk])
                self.tt("dve", h2[:, m, c512], p[:, :], h2[:, m, c512], ALU.add,
                        [pk, ("h2", m, cgp)], [("h2", m, cgp)])
            return f

        load_gu(0)
        load_d(0)
        gxi = 0
        nexp = len(order)
        for ei in range(nexp):
            e = order[ei]
            b = ei % 2
            if ei + 1 < nexp:
                load_gu(ei + 1)
            half = ei % 2
            pi = ei // 2
            rb = pi % 2
            off = 64 * half
            psel = pos64 if half else pos
            if half == 0:
                eA = e
                eB = order[ei + 1] if ei + 1 < nexp else None
                for m in range(OWN):
                    self.ts("dve", SelW[:, m % 2, 0:64], iota[:, 0:64], pos[:, m, eA:eA + 1], Cw[:, m, eA:eA + 1],
                            ALU.is_equal, ALU.mult, ["iota", ("pos", m), ("Cw", m)], [("SelW", m % 2, 0)])
                    if eB is not None:
                        self.ts("dve", SelW[:, m % 2, 64:128], iota[:, 64:128], pos64[:, m, eB:eB + 1],
                                Cw[:, m, eB:eB + 1], ALU.is_equal, ALU.mult,
                                ["iota", ("pos64", m), ("Cw", m)], [("SelW", m % 2, 1)])
                    else:
                        self.S.op("dve", lambda e_, m=m: e_.memset(SelW[:, m % 2, 64:128], 0.0), [],
                                  [("SelW", m % 2, 1)])
                    self.tr(tq[:, m * 128:(m + 1) * 128], SelW[:, m % 2, :], self.ident[:, :],
                            [("SelW", m % 2), "ident"], ["tq"])
                    if m % 2:
                        drain()
                self.cp("act", SelWT[rb][:, :, :], tq[:, :].rearrange("p (m t) -> p m t", t=128), ["tq"],
                        [("SelWT", rb)])
            for m in range(OWN):
                self.ts("dve", SelE[:, m, :], iota[:, off:off + CAPG], psel[:, m, e:e + 1], None,
                        ALU.is_equal, None, ["iota", ("pos", m), ("pos64", m)], [("SelE", m)])
            for fq in range(4):
                p = gx[gxi % 2]; pk = ("gx", gxi % 2); gxi += 1
                for fi in range(4):
                    f = fq * 4 + fi
                    for m in range(OWN):
                        self.mm(p[:, fi * 128 + off:fi * 128 + off + CAPG], bn[:, m, f * 128:(f + 1) * 128], SelE[:, m, :],
                                m == 0, m == OWN - 1, [("bn", m), ("SelE", m)], [pk])
                    drain()
                self.cp("act" if fq % 2 else "dve", xeT[:, fq * 4:(fq + 1) * 4, off:off + CAPG],
                        p[:, :].rearrange("p (k t) -> p k t", t=128)[:, :, off:off + CAPG], [pk], [("xeT", fq)])
            gp, upp = fy[0], fy[1]
            for kc in range(16):
                self.mm(gp[:, :], xeT[:, kc, :], wg[b][:, kc, :], kc == 0, kc == 15,
                        [("xeT", kc // 4), ("wg", b, kc - kc % 4)], [("fy", 0)])
                if kc % 4 == 3:
                    drain()
            for kc in range(16):
                self.mm(upp[:, :], xeT[:, kc, :], wu[b][:, kc, :], kc == 0, kc == 15,
                        [("xeT", kc // 4), ("wu", b, kc - kc % 4)], [("fy", 1)])
                if kc % 4 == 3:
                    drain()
            self.act(hdn[:, :], gp[:, :], ACTF.Silu, [("fy", 0)], ["hdn"])
            self.tt("dve", hdn[:, :], upp[:, :], hdn[:, :], ALU.mult, [("fy", 1), "hdn"], ["hdn"])
            drain(2)
            for kc in range(4):
                self.tr(tq[:, kc * 128:(kc + 1) * 128], hdn[:, kc * 128:(kc + 1) * 128], self.ident[:, :],
                        ["hdn", "ident"], ["tq"])
            self.cp("act", hdnT[:, :, :], tq[:, 0:512].rearrange("p (k t) -> p k t", t=128), ["tq"], ["hdnT"])
            drain(2)
            for cgp in range(4):
                c512 = slice(cgp * 512, (cgp + 1) * 512)
                fi_ = (2 + cgp) % 3
                yp = fy[fi_]
                for kc in range(4):
                    self.mm(yp[:, :], hdnT[:, kc, :], wd[:, kc, c512], kc == 0, kc == 3,
                            ["hdnT", ("wd", cgp // 2)], [("fy", fi_)])
                self.cp("act" if cgp % 2 else "dve", ye[rb][off:off + 64, c512], yp[off:off + 64, :],
                        [("fy", fi_)], [("ye", rb, cgp, half)])
                drain()
            if half == 1:
                drain(64)
            if ei + 1 < nexp:
                load_d(ei + 1)
            if half == 1 or ei == nexp - 1:
                for m in range(OWN):
                    for cgp in range(4):
                        pending.append(make_scatter(pi, m, cgp))
        drain(64)
        self.release(mkR)

    def phase_final(self):
        h2 = self.h2
        wfin = self.sb("wfin", [128, D], F32)
        self.dma("sp", wfin[:], self.vec_fin[0:1, :].partition_broadcast(128), [], ["wfin"], "c5")
        ot = [self.sb(f"outt{i}", [128, D], F32) for i in range(2)]
        junk = self.sb("junkf", [128, D], BF16)
        rt = self.sb("rtf", [128, 32], F32)
        for m in range(OWN):
            c = m * 3
            self.act(junk[:, :], h2[:, m, :], ACTF.Square, [("h2", m)], ["junkf", ("rtf", c)],
                     accum_out=rt[:, c:c + 1])
            self.act(rt[:, c + 1:c + 2], rt[:, c:c + 1], ACTF.Sqrt, [("rtf", c)], [("rtf", c + 1)], bias=EPS, scale=1.0 / D)
            self.S.op("dve", lambda e, c=c: e.reciprocal(rt[:, c + 2:c + 3], rt[:, c + 1:c + 2]),
                      [("rtf", c + 1)], [("rtf", c + 2)])
            o = ot[m % 2]
            self.stt("dve", o[:, :], h2[:, m, :], rt[:, c + 2:c + 3], wfin[:, :], ALU.mult, ALU.mult,
                     [("h2", m), ("rtf", c + 2), "wfin"], [("outt", m % 2)])
            self.dma("sp", self.out_d[m * 128:(m + 1) * 128, :], o[:, :], [("outt", m % 2)], [], f"outs{m % 2}")

    def debug_dump_phase_a(self):
        o1 = self.dout("dbg_ckvT", [128, 4, L], BF16)
        o2 = self.dout("dbg_kpeT", [64, L], BF16)
        self.dma("sp", o1[:, :, :], self.ckvT[:], [("ckvT",)], [], "outs")
        self.dma("sp", o2[:, :], self.kpeT[0:64, :], [("kpeT",)], [], "outs")


def rope_tables_np():
    inv = 1.0 / (10000.0 ** (np.arange(0, DR, 2, dtype=np.float32) / DR))
    ang = np.arange(L, dtype=np.float32)[:, None] * inv[None, :].astype(np.float32)
    ang = ang.astype(np.float32)
    cos = np.cos(ang).astype(np.float32)
    sin = np.sin(ang).astype(np.float32)
    cosT = np.ascontiguousarray(np.concatenate([cos, cos], axis=1).T)
    sinT = np.ascontiguousarray(np.concatenate([sin, sin], axis=1).T)
    return cosT, sinT


def _uq_swapped(w_uq):
    w = w_uq.reshape(QL, NH, 192)[:, :, 128:192]
    return np.ascontiguousarray(np.concatenate([w[:, :, 32:64], w[:, :, 0:32]], axis=2).reshape(QL, NH * 64))


def own_blocks(core):
    return [8 * m + core for m in range(OWN)]


def prep_core(common, core):
    xa = common["x_all"]
    blks = own_blocks(core)
    main = np.concatenate([xa[NMETA + 128 * j: NMETA + 128 * j + 128] for j in blks], axis=0)
    halo = np.concatenate([xa[128 * j: 128 * j + 16] for j in blks], axis=0)
    pos = np.concatenate([np.arange(NMETA + 128 * j, NMETA + 128 * j + 128) for j in blks])
    k = np.arange(128)[:, None]
    q = np.arange(128)[None, :]
    mask = np.zeros((128, 8, 128), np.float32)
    for r in range(8):
        if r < core:
            mask[:, r, :] = 1.0
        elif r == core:
            mask[:, r, :] = (k <= q).astype(np.float32)
    m = dict(common)
    m["x_own"] = np.ascontiguousarray(np.concatenate([main, halo], axis=0))
    m["cos_own"] = np.ascontiguousarray(common["cos_all"][:, pos])
    m["sin_own"] = np.ascontiguousarray(common["sin_all"][:, pos])
    m["maskT"] = mask.astype(ml_dtypes.bfloat16)
    return m


def prep_common(inp):
    x = np.asarray(inp["x"], np.float32)[0]
    meta = np.asarray(inp["meta_tokens"], np.float32)
    w_in = np.ascontiguousarray(np.asarray(inp["w_in"], np.float32)[0])
    kr = w_in[:, 1024:1088]
    cosT, sinT = rope_tables_np()
    m = {
        "x_all": np.ascontiguousarray(np.concatenate([meta, x], axis=0)),
        "w_in": w_in,
        "w_ks": np.ascontiguousarray(np.concatenate([kr[:, 32:64], kr[:, 0:32]], axis=1)),
        "norm_mix_w": np.ascontiguousarray(np.asarray(inp["norm_mix_w"], np.float32).reshape(1, D)),
        "kv_norm_w": np.ascontiguousarray(np.asarray(inp["kv_norm_w"], np.float32).reshape(4, 128).T),
        "q_norm_w": np.ascontiguousarray(np.asarray(inp["q_norm_w"], np.float32).reshape(4, 128).T),
        "cos_all": cosT,
        "sin_all": sinT,
        "w_uq": np.ascontiguousarray(np.asarray(inp["w_uq"], np.float32)[0]),
        "w_uqs": _uq_swapped(np.asarray(inp["w_uq"], np.float32)[0]),
        "w_ukv": np.ascontiguousarray(np.asarray(inp["w_ukv"], np.float32)[0]),
        "pool_scale": np.ascontiguousarray(np.asarray(inp["pool_scale"], np.float32).reshape(8, 128).T),
        "pool_w": np.ascontiguousarray(np.asarray(inp["pool_w"], np.float32)[0].reshape(1024, 256)),
        "w_o_mla": np.ascontiguousarray(np.asarray(inp["w_o_mla"], np.float32)[0]),
        "w_pool_out": np.ascontiguousarray(np.asarray(inp["w_pool_out"], np.float32)[0]),
        "w_out": np.ascontiguousarray(np.asarray(inp["w_out"], np.float32)[0]),
        "ustr": np.triu(np.ones((128, 128), np.float32), 1).astype(ml_dtypes.bfloat16),
        "iota": np.ascontiguousarray(np.broadcast_to(np.arange(CAP, dtype=np.float32)[None, :], (128, CAP))),
        "ident32": np.eye(128, dtype=np.float32),
        "norm_ffn_w": np.ascontiguousarray(np.asarray(inp["norm_ffn_w"], np.float32).reshape(1, D)),
        "final_norm_w": np.ascontiguousarray(np.asarray(inp["final_norm_w"], np.float32).reshape(1, D)),
        "w_r": np.ascontiguousarray(np.concatenate([np.asarray(inp["w_router_group"], np.float32)[0],
                                                    np.asarray(inp["w_router_expert"], np.float32)[0]], axis=1)),
        "b_r": np.ascontiguousarray(np.concatenate([np.asarray(inp["b_router_group"], np.float32)[0],
                                                    np.asarray(inp["b_router_expert"], np.float32)[0]])[None, :]),
        "w_exp_gate": np.asarray(inp["w_exp_gate"], np.float32).reshape(NE * D, FF),
        "w_exp_up": np.asarray(inp["w_exp_up"], np.float32).reshape(NE * D, FF),
        "w_exp_down": np.asarray(inp["w_exp_down"], np.float32).reshape(NE * FF, D),
        "ident_bf": np.eye(128, dtype=np.float32).astype(ml_dtypes.bfloat16),
        "ones_bf": np.ones((128, 128), np.float32).astype(ml_dtypes.bfloat16),
    }
    return m


def build_full():
    B = Builder(stage="full")
    B.setup_common()
    B.phase_a()
    B.phase_b1()
    B.phase_attn()
    B.phase_mix()
    B.phase_moe()
    B.phase_final()
    B.S.emit(B.nc, final_waits=[("sp", "outs0"), ("sp", "outs1")])
    B.close()
    return B


def kernel(**inputs):
    B = build_full()
    cm = prep_common(inputs)
    names = list(B.dram.keys())
    in_maps = []
    for c in range(NCORES):
        m = prep_core(cm, c)
        in_maps.append({k: m[k] for k in names if k != "out"})
    res = run_bass_kernel_spmd(B.nc, in_maps, core_ids=list(range(NCORES)))
    out = np.zeros((1, SEQ, D), np.float32)
    for c in range(NCORES):
        o = np.asarray(res.results[c]["out"], np.float32)
        for m, j in enumerate(own_blocks(c)):
            out[0, 128 * j:128 * j + 128, :] = o[128 * m:128 * (m + 1), :]
    return out
```

```python
import numpy as np
import ml_dtypes
import concourse.bass as bass
import concourse.mybir as mybir
from concourse.bass_utils import run_bass_kernel_spmd

F32 = mybir.dt.float32
BF16 = mybir.dt.bfloat16
I32 = mybir.dt.int32
ALU = mybir.AluOpType
ACTF = mybir.ActivationFunctionType
AX = mybir.AxisListType

NCORES = 8
D = 2048
SEQ = 8192
NMETA = 16
L = SEQ + NMETA
EPS = 1e-6
NH = 16
DN = 128
DR = 64
DV = 128
QL = 512
KVL = 512
NBLK = 64
OWN = 8
NOWN = OWN * 128
NE = 64
FF = 512
CAP = 128
CAPG = 64
NCONVA = 4
NCONV = 28


class _Op:
    __slots__ = ("eng", "fn", "waits", "milestone", "semval", "dma", "idx")

    def __init__(self, eng, fn, dma=None):
        self.eng = eng
        self.fn = fn
        self.waits = []
        self.milestone = False
        self.semval = None
        self.dma = dma
        self.idx = None


class Sched:
    ENGS = ("pe", "act", "dve", "pool", "sp")

    def __init__(self):
        self.ops = {e: [] for e in self.ENGS}
        self.state = {}
        self.dma_cnt = {}
        self.total_sems = {"const", "constp", "const2", "outs"}

    @staticmethod
    def _overlap(a, b):
        n = min(len(a), len(b))
        return a[:n] == b[:n]

    def _entries(self, key):
        d = self.state.get(key[0])
        if not d:
            return []
        return [(k, v) for k, v in d.items() if self._overlap(k, key)]

    def op(self, eng, fn, reads=(), writes=(), dma_sem=None):
        reads = [r if isinstance(r, tuple) else (r,) for r in reads]
        writes = [w if isinstance(w, tuple) else (w,) for w in writes]
        if ("__mem__",) not in writes:
            reads.append(("__mem__",))
        o = _Op(eng, fn)
        if dma_sem is not None:
            dv = self.dma_cnt.get(dma_sem, 0) + 16
            self.dma_cnt[dma_sem] = dv
            o.dma = (dma_sem, dv)
            ev = ("d", dma_sem, dv)
            rkey = ("d", dma_sem)
        else:
            ev = ("c", o)
            rkey = ("c", eng)
        deps = []
        if dma_sem is not None and dma_sem not in self.total_sems and dv > 16:
            deps.append(("d", dma_sem, dv - 16))
        if dma_sem is not None and eng == "pool":
            hist = self.__dict__.setdefault("_swdge_hist", [])
            if len(hist) >= 4 and hist[-4][1] not in self.total_sems:
                deps.append(hist[-4])
            hist.append(ev)
        for r in reads:
            for k, v in self._entries(r):
                if v[0] is not None:
                    deps.append(v[0])
        for w in writes:
            for k, v in self._entries(w):
                if v[0] is not None:
                    deps.append(v[0])
                deps.extend(v[1].values())
        seen = set()
        for d in deps:
            if d[0] == "c":
                p = d[1]
                if p is o:
                    continue
                if p.eng == eng == "pe" and dma_sem is None:
                    continue
                if id(p) in seen:
                    continue
                seen.add(id(p))
                p.milestone = True
                o.waits.append(d)
            else:
                if d in seen:
                    continue
                seen.add(d)
                o.waits.append(d)
        for r in reads:
            dd = self.state.setdefault(r[0], {})
            ent = dd.get(r)
            if ent is None:
                ent = [None, {}]
                dd[r] = ent
            ent[1][rkey] = ev
        for w in writes:
            dd = self.state.setdefault(w[0], {})
            for k in [k for k in dd if len(k) > len(w) and k[:len(w)] == w]:
                del dd[k]
            dd[w] = [ev, {}]
        o.idx = len(self.ops[eng])
        self.ops[eng].append(o)
        return o

    def emit(self, nc, final_waits=()):
        engobj = {"pe": "tensor", "act": "scalar", "dve": "vector", "pool": "gpsimd", "sp": "sync"}
        for e in self.ENGS:
            c = 0
            for o in self.ops[e]:
                if o.milestone and o.dma is None:
                    c += 1
                    o.semval = c
        sems = {}
        for e in self.ENGS:
            sems[("c", e)] = nc.alloc_semaphore(name=f"s_{e}")
        for name in self.dma_cnt:
            sems[("d", name)] = nc.alloc_semaphore(name=f"d_{name}")
        self.sems = sems
        sched = self

        def run_engine(e, eng):
            waited = {}
            for o in sched.ops[e]:
                for d in o.waits:
                    if d[0] == "c":
                        key = ("c", d[1].eng)
                        val = d[1].semval
                    else:
                        key = ("d", d[1])
                        val = d[2]
                        if d[1] in sched.total_sems:
                            val = sched.dma_cnt[d[1]]
                    if waited.get(key, 0) >= val:
                        continue
                    waited[key] = val
                    eng.wait_ge(sems[key], val)
                inst = o.fn(eng)
                if o.dma is not None:
                    inst.then_inc(sems[("d", o.dma[0])], 16)
                elif o.milestone:
                    inst.then_inc(sems[("c", e)], 1)
            for (fe, name) in final_waits:
                if fe == e:
                    eng.wait_ge(sems[("d", name)], sched.dma_cnt[name])

        with nc.Block() as block:
            @block.tensor
            def _(eng):
                run_engine("pe", eng)

            @block.scalar
            def _(eng):
                run_engine("act", eng)

            @block.vector
            def _(eng):
                run_engine("dve", eng)

            @block.gpsimd
            def _(eng):
                run_engine("pool", eng)

            @block.sync
            def _(eng):
                run_engine("sp", eng)


class Builder:
    def __init__(self, stage="full", ngroups=16):
        self.stage = stage
        self.ngroups = ngroups
        self.nc = bass.Bass("TRN2", target_bir_lowering=False)
        self.S = Sched()
        self.nxt = 3
        self.dram = {}
        self._ctx = []
        self._ctxL = []

    def din(self, name, shape, dt=F32):
        t = self.nc.dram_tensor(name, list(shape), dt, kind="ExternalInput")
        self.dram[name] = t
        return t

    def dout(self, name, shape, dt=F32):
        t = self.nc.dram_tensor(name, list(shape), dt, kind="ExternalOutput")
        self.dram[name] = t
        return t

    def sb(self, name, shape, dt, side="right"):
        self._n = getattr(self, "_n", 0) + 1
        cm = self.nc.sbuf_tensor(f"{name}_{self._n}", list(shape), dt, side=side)
        t = cm.__enter__()
        (self._ctxL if side == "left" else self._ctx).append(cm)
        return t

    def markL(self):
        return len(self._ctxL)

    def releaseL(self, mark):
        self.barrier()
        while len(self._ctxL) > mark:
            self._ctxL.pop().__exit__(None, None, None)

    def ps(self, name, shape, dt=F32):
        self._n = getattr(self, "_n", 0) + 1
        cm = self.nc.psum_tensor(f"{name}_{self._n}", list(shape), dt)
        t = cm.__enter__()
        self._ctx.append(cm)
        return t

    def mark(self):
        return len(self._ctx)

    def release(self, mark):
        self.barrier()
        while len(self._ctx) > mark:
            self._ctx.pop().__exit__(None, None, None)

    def barrier(self):
        if not hasattr(self, "_bar"):
            self._bar = self.nc.alloc_sbuf_tensor("bar_scratch", [128, 8], F32) if False else None
        self.S.op("pool", lambda e: e.memset(self.bar_t[:, 0:1], 0.0), [], [("__mem__",), "bar_t"])

    def close(self):
        for cm in reversed(self._ctx):
            cm.__exit__(None, None, None)
        for cm in reversed(self._ctxL):
            cm.__exit__(None, None, None)
        self._ctx = []
        self._ctxL = []

    def dma(self, q, out, in_, reads, writes, sem):
        return self.S.op(q, lambda e: e.dma_start(out=out, in_=in_), reads, writes, dma_sem=sem)

    def mm(self, out, lhsT, rhs, start, stop, reads, writes):
        return self.S.op("pe", lambda e: e.matmul(out, lhsT, rhs, start=start, stop=stop), reads, writes)

    def tr(self, out, in_, ident, reads, writes):
        return self.S.op("pe", lambda e: e.transpose(out, in_, ident), reads, writes)

    def act(self, out, in_, func, reads, writes, **kw):
        return self.S.op("act", lambda e: e.activation(out, in_, func, **kw), reads, writes)

    def tt(self, eng, out, in0, in1, op, reads, writes):
        return self.S.op(eng, lambda e: e.tensor_tensor(out, in0, in1, op), reads, writes)

    def ts(self, eng, out, in0, s1, s2, op0, op1, reads, writes):
        if op1 is None:
            return self.S.op(eng, lambda e: e.tensor_single_scalar(out, in0, s1, op0), reads, writes)
        return self.S.op(eng, lambda e: e.tensor_scalar(out, in0, s1, s2, op0, op1), reads, writes)

    def stt(self, eng, out, in0, scalar, in1, op0, op1, reads, writes):
        return self.S.op(eng, lambda e: e.scalar_tensor_tensor(out, in0, scalar, in1, op0, op1), reads, writes)

    def cp(self, eng, out, in_, reads, writes):
        if eng == "act":
            return self.S.op("act", lambda e: e.copy(out, in_), reads, writes)
        return self.S.op(eng, lambda e: e.tensor_copy(out, in_), reads, writes)


    def setup_common(self):
        nc = self.nc
        self.x_all = self.din("x_all", [L, D])
        self.w_in = self.din("w_in", [D, 6208])
        self.w_ks = self.din("w_ks", [D, DR])
        self.vec_mix = self.din("norm_mix_w", [1, D])
        self.kvn = self.din("kv_norm_w", [128, 4])
        self.qn = self.din("q_norm_w", [128, 4])
        self.cos_all = self.din("cos_all", [DR, L])
        self.sin_all = self.din("sin_all", [DR, L])
        self.x_own = self.din("x_own", [NOWN + 128, D])
        self.cos_own_d = self.din("cos_own", [DR, NOWN])
        self.sin_own_d = self.din("sin_own", [DR, NOWN])
        self.mask_d = self.din("maskT", [128, 8, 128], BF16)
        self.w_uq = self.din("w_uq", [QL, NH * 192])
        self.w_uqs = self.din("w_uqs", [QL, NH * 64])
        self.w_ukv = self.din("w_ukv", [KVL, NH * 256])
        self.pool_scale_d = self.din("pool_scale", [128, 8])
        self.pool_w_d = self.din("pool_w", [1024, 256])
        self.w_o_d = self.din("w_o_mla", [D, D])
        self.w_po_d = self.din("w_pool_out", [1024, D])
        self.w_out_d = self.din("w_out", [D, D])
        self.ustr_d = self.din("ustr", [128, 128], BF16)
        self.iota_d = self.din("iota", [128, CAP])
        self.ident32_d = self.din("ident32", [128, 128])
        self.vec_ffn = self.din("norm_ffn_w", [1, D])
        self.vec_fin = self.din("final_norm_w", [1, D])
        self.w_r_d = self.din("w_r", [D, 72])
        self.b_r_d = self.din("b_r", [1, 72])
        self.w_eg_d = self.din("w_exp_gate", [NE * D, FF])
        self.w_eu_d = self.din("w_exp_up", [NE * D, FF])
        self.w_ed_d = self.din("w_exp_down", [NE * FF, D])
        self.out_d = self.dout("out", [NOWN, D])
        self.seg_bf = self.nc.dram_tensor("seg_bf", [(NCONV + NCONVA) * D, FF], BF16, kind="Internal")
        self.seu_bf = self.nc.dram_tensor("seu_bf", [(NCONV + NCONVA) * D, FF], BF16, kind="Internal")
        self.sed_bf = self.nc.dram_tensor("sed_bf", [(NCONV + NCONVA) * FF, D], BF16, kind="Internal")
        self.cvi = 0
        self.ident_d = self.din("ident_bf", [128, 128], BF16)
        self.ones_d = self.din("ones_bf", [128, 128], BF16)
        self.ident = self.sb("ident", [128, 128], BF16, side="left")
        self.ones = self.sb("ones", [128, 128], BF16, side="left")
        self.kvn_s = self.sb("kvn_s", [128, 4], F32, side="left")
        self.qn_s = self.sb("qn_s", [128, 4], F32, side="left")
        self.bar_t = self.sb("bar_t", [128, 8], F32, side="left")
        self.mkW = self.markL()
        self.wmix = self.sb("wmix", [128, D], F32, side="left")
        S = self.S
        S.total_sems.add("const")
        self.dma("sp", self.ident[:], self.ident_d[:, :], [], ["ident"], "const")
        self.dma("sp", self.ones[:], self.ones_d[:, :], [], ["ones"], "const")
        self.dma("sp", self.wmix[:], self.vec_mix[0:1, :].partition_broadcast(128), [], ["wmix"], "const")
        self.dma("sp", self.kvn_s[:], self.kvn[:, :], [], ["kvn_s"], "const")
        self.dma("sp", self.qn_s[:], self.qn[:, :], [], ["qn_s"], "const")

    def norm_stage1(self, src_rows_ap, ntok, blk_i, wtile, wkey):
        nb = self.nxt
        s = blk_i % nb
        xt = self.xt[s]
        xs = self.xs[blk_i % 2]
        ss = self.ssx
        kx, kxs = ("xt", s), ("xs", blk_i % 2)
        c = blk_i % 8
        kss = ("ssx", c)
        self.dma("sp", xt[0:ntok, :], src_rows_ap, [], [kx], f"xt{s}")
        self.act(xs[0:ntok, :], xt[0:ntok, :], ACTF.Square, [kx], [kxs, kss],
                 accum_out=ss[0:ntok, c:c + 1])
        self.act(ss[0:ntok, 8 + c:9 + c], ss[0:ntok, c:c + 1], ACTF.Sqrt, [kss], [("rsx", c)],
                 bias=EPS, scale=1.0 / D)
        self.S.op("dve", lambda e: e.reciprocal(ss[0:ntok, 16 + c:17 + c], ss[0:ntok, 8 + c:9 + c]),
                  [("rsx", c)], [("rstdx", c)])
        self.stt("dve", xs[0:ntok, :], xt[0:ntok, :], ss[0:ntok, 16 + c:17 + c], wtile[0:ntok, :],
                 ALU.mult, ALU.mult, [kx, ("rstdx", c), wkey], [kxs])

    def norm_stage2(self, ntok, aT, col0, tag, blk_i):
        xs = self.xs[blk_i % 2]
        kxs = ("xs", blk_i % 2)
        for half, eng in ((0, "act"), (1, "dve")):
            tph = self.tp[half]
            ktp = ("tp", half)
            for k8 in range(8):
                kc = half * 8 + k8
                self.tr(tph[:, k8 * 128: k8 * 128 + ntok], xs[0:ntok, kc * 128:(kc + 1) * 128],
                        self.ident[0:ntok, 0:ntok], [kxs, "ident"], [ktp])
            src = tph[:, :].rearrange("p (k t) -> p k t", t=128)[:, :, 0:ntok]
            dst = aT[:, half * 8:(half + 1) * 8, col0:col0 + ntok]
            self.cp(eng, dst, src, [ktp], [(tag, "aT")])

    def norm_pipeline(self, blocks, after_cb=None):
        base = self.blk_i
        n = len(blocks)
        if n == 0:
            return
        self.norm_stage1(blocks[0][0], blocks[0][1], base, self.wmix, "wmix")
        for k in range(n):
            if k + 1 < n:
                self.norm_stage1(blocks[k + 1][0], blocks[k + 1][1], base + k + 1, self.wmix, "wmix")
            src, ntok, aT, col0, tag = blocks[k]
            self.norm_stage2(ntok, aT, col0, tag, base + k)
            if after_cb is not None:
                after_cb(k)
        self.blk_i = base + n

    def alloc_norm(self):
        self.xt = [self.sb(f"xt{i}", [128, D], F32) for i in range(self.nxt)]
        self.xs = [self.sb(f"xs{i}", [128, D], BF16) for i in range(2)]
        self.tp = [self.ps(f"tp{i}", [128, 1024], BF16) for i in range(2)]
        self.ssx = self.sb("ssx", [128, 24], F32)
        self.blk_i = 0

    def alloc_front(self):
        self.alloc_norm()
        self.aTg = [self.sb(f"aTg{i}", [128, 16, 512], BF16) for i in range(2)]
        self.pc = [self.ps(f"pc{i}", [128, 512], F32) for i in range(2)]
        self.ssb = self.ps("ssb", [128, 512], F32)
        self.craw = self.sb("craw", [128, 4, 512], F32)
        self.sq = self.sb("sq", [128, 4, 512], BF16)
        self.rstdb = self.sb("rstdb", [128, 512], F32)
        self.pci = 0
        self.blk_i = 0

    def latent_group(self, aT, tag, n, wsb, wcol0, wkeyf, nscal, nskey, dstf, dkeyf):
        for mt in range(4):
            p = self.pc[self.pci % 2]
            pk = ("pc", self.pci % 2)
            self.pci += 1
            for kc in range(16):
                self.mm(p[:, 0:n], wsb[:, kc, wcol0 + mt * 128: wcol0 + (mt + 1) * 128], aT[:, kc, 0:n],
                        kc == 0, kc == 15, [wkeyf(kc), (tag, "aT")], [pk])
            self.cp("act", self.craw[:, mt, 0:n], p[:, 0:n], [pk], [("craw", mt)])
            self.act(self.sq[:, mt, 0:n], p[:, 0:n], ACTF.Square, [pk], [("sq", mt)])
        for mt in range(4):
            self.mm(self.ssb[:, 0:n], self.ones[:, :], self.sq[:, mt, 0:n], mt == 0, mt == 3,
                    ["ones", ("sq", mt)], ["ssb"])
        rstdb = self.rstdb
        self.act(rstdb[:, 0:n], self.ssb[:, 0:n], ACTF.Sqrt, ["ssb"], ["rstdb"], bias=EPS, scale=1.0 / 512)
        self.S.op("dve", lambda e, n=n: e.reciprocal(rstdb[:, 0:n], rstdb[:, 0:n]), ["rstdb"], ["rstdb"])
        for mt in range(4):
            self.stt("dve", dstf(mt), self.craw[:, mt, 0:n], nscal[:, mt:mt + 1], rstdb[:, 0:n],
                     ALU.mult, ALU.mult, [("craw", mt), nskey, "rstdb"], [dkeyf(mt)])

    def phase_a(self):
        self.mk0 = self.mark()
        self.ckvT = self.sb("ckvT", [128, 4, L], BF16)
        self.kpeT = self.sb("kpeT", [128, L], BF16)
        self.S.op("pool", lambda e: e.memset(self.kpeT[64:128, :], 0.0), [], [("kpeT", "pad")])
        self.cqT = self.sb("cqT", [128, 4, NOWN], BF16)
        self.mkA = self.mark()
        self.wkv = self.sb("wkv", [128, 16, 576], BF16)
        self.wks = self.sb("wks", [128, 16, 64], BF16)
        wks_tmp = self.sb("wks_tmp", [128, 16, 64], F32)
        for kc in range(0, 16, 4):
            self.dma("pool", self.wkv[:, kc:kc + 4, :],
                     self.w_in[kc * 128:(kc + 4) * 128, 512:1088].rearrange("(k p) c -> p k c", p=128),
                     [], [("wkv", kc)], "constp")
        self.dma("sp", wks_tmp[:], self.w_ks.rearrange("(k p) c -> p k c", p=128), [], ["wks_tmp"], "const")
        self.ts("dve", self.wks[:, :, 0:32], wks_tmp[:, :, 0:32], -1.0, None, ALU.mult, None,
                ["wks_tmp"], [("wks", 0)])
        self.cp("dve", self.wks[:, :, 32:64], wks_tmp[:, :, 32:64], ["wks_tmp"], [("wks", 1)])
        self.alloc_front()
        pkr = self.ps("pkr", [128, 512], F32)
        pks = self.ps("pks", [128, 512], F32)
        cs = self.sb("cs", [64, 2, 512], F32)
        t12 = self.sb("t12", [64, 2, 512], F32)

        segs = [(0, NMETA)] + [(NMETA + 512 * g, 512) for g in range(self.ngroups)]

        def chain_mt(gi, mt):
            row0, n = segs[gi]
            aT = self.aTg[gi % 2]
            tag = f"aTg{gi % 2}"
            p = self.pc[self.pci % 2]
            pk = ("pc", self.pci % 2)
            self.pci += 1
            for kc in range(16):
                self.mm(p[:, 0:n], self.wkv[:, kc, mt * 128:(mt + 1) * 128], aT[:, kc, 0:n],
                        kc == 0, kc == 15, [("wkv", kc - kc % 4), (tag, "aT")], [pk])
            self.cp("act", self.craw[:, mt, 0:n], p[:, 0:n], [pk], [("craw", mt)])
            self.act(self.sq[:, mt, 0:n], p[:, 0:n], ACTF.Square, [pk], [("sq", mt)])

        def chain_kr(gi):
            row0, n = segs[gi]
            aT = self.aTg[gi % 2]
            tag = f"aTg{gi % 2}"
            self.dma("sp", cs[:, 0, 0:n], self.cos_all[:, row0:row0 + n], [], [("cs", 0)], "cs0")
            self.dma("sp", cs[:, 1, 0:n], self.sin_all[:, row0:row0 + n], [], [("cs", 1)], "cs1")
            for kc in range(16):
                self.mm(pkr[0:64, 0:n], self.wkv[:, kc, 512:576], aT[:, kc, 0:n], kc == 0, kc == 15,
                        [("wkv", kc - kc % 4), (tag, "aT")], ["pkr"])
            self.tt("dve", t12[:, 0, 0:n], pkr[0:64, 0:n], cs[:, 0, 0:n], ALU.mult,
                    ["pkr", ("cs", 0)], [("t12", 0)])

        def chain_ks(gi):
            row0, n = segs[gi]
            aT = self.aTg[gi % 2]
            tag = f"aTg{gi % 2}"
            for kc in range(16):
                self.mm(pks[0:64, 0:n], self.wks[:, kc, :], aT[:, kc, 0:n], kc == 0, kc == 15,
                        [("wks",), (tag, "aT")], ["pks"])
            self.tt("dve", t12[:, 1, 0:n], pks[0:64, 0:n], cs[:, 1, 0:n], ALU.mult,
                    ["pks", ("cs", 1)], [("t12", 1)])
            self.tt("dve", self.kpeT[0:64, row0:row0 + n], t12[:, 0, 0:n], t12[:, 1, 0:n], ALU.add,
                    [("t12",)], [("kpeT", gi)])

        def finish(gi):
            row0, n = segs[gi]
            rstdb = self.rstdb
            for mt in range(4):
                self.mm(self.ssb[:, 0:n], self.ones[:, :], self.sq[:, mt, 0:n], mt == 0, mt == 3,
                        ["ones", ("sq", mt)], ["ssb"])
            self.act(rstdb[:, 0:n], self.ssb[:, 0:n], ACTF.Sqrt, ["ssb"], ["rstdb"], bias=EPS, scale=1.0 / 512)
            self.S.op("dve", lambda e, n=n: e.reciprocal(rstdb[:, 0:n], rstdb[:, 0:n]), ["rstdb"], ["rstdb"])
            for mt in range(4):
                self.stt("dve", self.ckvT[:, mt, row0:row0 + n], self.craw[:, mt, 0:n], self.kvn_s[:, mt:mt + 1],
                         rstdb[:, 0:n], ALU.mult, ALU.mult, [("craw", mt), "kvn_s", "rstdb"], [("ckvT", gi, mt)])

        blocks = []
        owner = []
        for gi, (row0, n) in enumerate(segs):
            g2 = gi % 2
            for b in range((n + 127) // 128):
                nt = min(128, n - b * 128)
                blocks.append((self.x_all[row0 + b * 128: row0 + b * 128 + nt, :], nt, self.aTg[g2], b * 128,
                               f"aTg{g2}"))
                owner.append((gi, b, (n + 127) // 128))
        pend_parts = []

        def parts_of(gi):
            return [lambda: (chain_mt(gi, 0), chain_kr(gi)),
                    lambda: (chain_mt(gi, 1), chain_ks(gi)),
                    lambda: chain_mt(gi, 2),
                    lambda: (chain_mt(gi, 3), finish(gi))]

        def after(k):
            gi, b, nb = owner[k]
            if self.stage == "full" and b == 0 and gi % 4 == 3 and (gi // 4) < NCONVA:
                self.convert_expert(NCONV + gi // 4)
            if pend_parts:
                pend_parts.pop(0)()
            if b == nb - 1:
                while pend_parts:
                    pend_parts.pop(0)()
                pend_parts.extend(parts_of(gi))

        self.norm_pipeline(blocks, after)
        while pend_parts:
            pend_parts.pop(0)()

    def phase_b1(self):
        for kc in range(0, 16, 4):
            self.dma("pool", self.wkv[:, kc:kc + 4, 0:512],
                     self.w_in[kc * 128:(kc + 4) * 128, 0:512].rearrange("(k p) c -> p k c", p=128),
                     [], [("wkv", kc)], "wq_in")
        for g in range(2):
            aT = self.aTg[g]
            tag = f"aTg{g}"
            self.norm_pipeline([(self.x_own[g * 512 + b * 128: g * 512 + (b + 1) * 128, :], 128, aT, b * 128, tag)
                                for b in range(4)])
            self.latent_group(aT, tag, 512, self.wkv, 0, lambda kc: ("wkv", kc - kc % 4), self.qn_s, "qn_s",
                              lambda mt, g=g: self.cqT[:, mt, g * 512:(g + 1) * 512],
                              lambda mt, g=g: ("cqT", g, mt))
        self.release(self.mkA)

    def convert_expert(self, e):
        for (src, dst, rows, key) in ((self.w_eg_d, self.seg_bf, D, "cvg"), (self.w_eu_d, self.seu_bf, D, "cvu"),
                                      (self.w_ed_d, self.sed_bf, FF, "cvd")):
            j = self.cvi % 6
            self.cvi += 1
            self.dma("pool", dst[e * rows:(e + 1) * rows, :].rearrange("(p r) c -> p r c", p=128),
                     src[e * rows:(e + 1) * rows, :].rearrange("(p r) c -> p r c", p=128),
                     [], [(key, e)], f"cv{j}")

    def phase_attn(self, nheads=NH):
        mk = self.mk0
        self.mkL0 = self.markL()
        self.OT = self.sb("OT", [128, NH, NOWN], BF16, side="left")
        cso = self.sb("cso", [64, 2, NOWN], F32)
        self.dma("sp", cso[:, 0, :], self.cos_own_d[:, :], [], [("cso", 0)], "const2")
        self.dma("sp", cso[:, 1, :], self.sin_own_d[:, :], [], [("cso", 1)], "const2")
        maskT = self.sb("maskT_s", [128, 8, 128], BF16)
        self.dma("sp", maskT[:], self.mask_d[:, :, :], [], ["maskT"], "const2")
        ones32 = self.sb("ones32", [128, 128], F32)
        self.S.op("pool", lambda e: e.memset(ones32[:], 1.0), [], ["ones32"])
        self.S.total_sems.add("const2")
        wq = [self.sb(f"wq{i}", [128, 4, 192], BF16) for i in range(2)]
        wqs_t = [self.sb(f"wqs_t{i}", [128, 4, 64], F32) for i in range(2)]
        wqs = [self.sb(f"wqs{i}", [128, 4, 64], BF16) for i in range(2)]
        wkv_h = [self.sb(f"wkvh{i}", [128, 4, 256], BF16) for i in range(2)]
        KhT = self.sb("KhT", [128, L], BF16)
        Vh = self.sb("Vh", [128, NBLK + 1, 128], BF16)
        Qn = self.sb("Qn", [128, NOWN], BF16)
        Qpe = self.sb("Qpe", [128, NOWN], BF16)
        self.S.op("pool", lambda e: e.memset(Qpe[64:128, :], 0.0), [], [("Qpe", "pad")])
        q12 = self.sb("q12", [64, 2, 512], F32)
        acc = self.sb("acc", [128, NOWN], F32)
        rcp = self.sb("rcp", [128, NOWN], F32)
        NPT = 6
        NST = 4
        pt = [self.sb(f"pt{i}", [128, 512], BF16) for i in range(NPT)]
        st = [self.ps(f"st{i}", [128, 512], F32) for i in range(NST)]
        ot = [self.ps(f"ot{i}", [128, 512], F32) for i in range(2)]
        gen = [self.ps(f"gen{i}", [128, 512], F32) for i in range(2)]
        rs = gen
        geni = 0
        sti = 0
        pti = 0
        scale = float((DN + DR) ** -0.5)

        def load_head_w(h):
            b = h % 2
            with_keys = [("wq", b), ("wqs_t", b), ("wkvh", b)]
            self.dma("pool", wq[b][:], self.w_uq[:, h * 192:(h + 1) * 192].rearrange("(k p) c -> p k c", p=128),
                     [], [("wq", b)], f"whq{b}")
            self.dma("sp", wqs_t[b][:], self.w_uqs[:, h * 64:(h + 1) * 64].rearrange("(k p) c -> p k c", p=128),
                     [], [("wqs_t", b)], f"whs{b}")
            self.dma("pool", wkv_h[b][:], self.w_ukv[:, h * 256:(h + 1) * 256].rearrange("(k p) c -> p k c", p=128),
                     [], [("wkvh", b)], f"whk{b}")

        load_head_w(0)
        for h in range(nheads):
            b = h % 2
            if h + 1 < nheads:
                load_head_w(h + 1)
            if self.stage == "full":
                n0 = min(2 * h, 24 + max(0, h - 12))
                n1 = min(2 * h + 2, 24 + max(0, h + 1 - 12))
                for e_ in range(n0, min(n1, NCONV)):
                    self.convert_expert(e_)
            self.ts("dve", wqs[b][:, :, 0:32], wqs_t[b][:, :, 0:32], -1.0, None, ALU.mult, None,
                    [("wqs_t", b)], [("wqs", b, 0)])
            self.cp("dve", wqs[b][:, :, 32:64], wqs_t[b][:, :, 32:64], [("wqs_t", b)], [("wqs", b, 1)])
            for g in range(2):
                cols = slice(g * 512, (g + 1) * 512)
                p = gen[geni % 2]; pk = ("gen", geni % 2); geni += 1
                for kc in range(4):
                    self.mm(p[:, :], wq[b][:, kc, 0:128], self.cqT[:, kc, cols], kc == 0, kc == 3,
                            [("wq", b), ("cqT",)], [pk])
                self.cp("act", Qn[:, cols], p[:, :], [pk], [("Qn", g)])
                p1 = gen[geni % 2]; pk1 = ("gen", geni % 2); geni += 1
                for kc in range(4):
                    self.mm(p1[0:64, :], wq[b][:, kc, 128:192], self.cqT[:, kc, cols], kc == 0, kc == 3,
                            [("wq", b), ("cqT",)], [pk1])
                self.tt("dve", q12[:, 0, :], p1[0:64, :], cso[:, 0, cols], ALU.mult, [pk1, ("cso", 0)], [("q12", 0)])
                p2 = gen[geni % 2]; pk2 = ("gen", geni % 2); geni += 1
                for kc in range(4):
                    self.mm(p2[0:64, :], wqs[b][:, kc, :], self.cqT[:, kc, cols], kc == 0, kc == 3,
                            [("wqs", b), ("cqT",)], [pk2])
                self.tt("dve", q12[:, 1, :], p2[0:64, :], cso[:, 1, cols], ALU.mult, [pk2, ("cso", 1)], [("q12", 1)])
                self.tt("dve", Qpe[0:64, cols], q12[:, 0, :], q12[:, 1, :], ALU.add, [("q12",)], [("Qpe", g)])
            segs = [(0, NMETA)] + [(NMETA + 512 * g, 512) for g in range(16)]
            for si, (c0, n) in enumerate(segs):
                p = gen[geni % 2]; pk = ("gen", geni % 2); geni += 1
                for kc in range(4):
                    self.mm(p[:, 0:n], wkv_h[b][:, kc, 0:128], self.ckvT[:, kc, c0:c0 + n], kc == 0, kc == 3,
                            [("wkvh", b), ("ckvT",)], [pk])
                self.cp("act" if si % 2 else "dve", KhT[:, c0:c0 + n], p[:, 0:n], [pk], [("KhT", si)])
            p = gen[geni % 2]; pk = ("gen", geni % 2); geni += 1
            for kc in range(4):
                self.mm(p[0:NMETA, 0:128], self.ckvT[:, kc, 0:NMETA], wkv_h[b][:, kc, 128:256], kc == 0, kc == 3,
                        [("wkvh", b), ("ckvT",)], [pk])
            self.cp("act", Vh[0:NMETA, 0, :], p[0:NMETA, 0:128], [pk], [("Vh", 0)])
            for j4 in range(16):
                p = gen[geni % 2]; pk = ("gen", geni % 2); geni += 1
                for jj in range(4):
                    j = j4 * 4 + jj
                    c0 = NMETA + 128 * j
                    for kc in range(4):
                        self.mm(p[:, jj * 128:(jj + 1) * 128], self.ckvT[:, kc, c0:c0 + 128],
                                wkv_h[b][:, kc, 128:256], kc == 0, kc == 3, [("wkvh", b), ("ckvT",)], [pk])
                self.cp("act" if j4 % 2 else "dve", Vh[:, 1 + j4 * 4: 5 + j4 * 4, :],
                        p[:, :].rearrange("p (j d) -> p j d", d=128), [pk], [("Vh", 1 + j4)])
            chunks = [(-1, 0)] + [(sc, r) for sc in range(8) for r in range(8)]
            plist = []
            for ci, (sc, r) in enumerate(chunks):
                if sc < 0:
                    nk, kc0, vch, q0 = NMETA, 0, 0, 0
                else:
                    j = sc * 8 + r
                    nk, kc0, vch, q0 = 128, NMETA + 128 * j, 1 + j, 128 * sc
                if q0 < 512:
                    plist.append((sc, r, nk, kc0, vch, q0, q0, 512, 0, ci))
                    plist.append((sc, r, nk, kc0, vch, q0, 512, 1024, 1, ci))
                else:
                    plist.append((sc, r, nk, kc0, vch, q0, q0, 1024, 1, ci))
            pend = []
            DEPTH = 3
            self.S.op("dve", lambda e: e.memset(acc[:], 0.0), [], [("acc",)])
            started = [False, False]
            lastidx = [max(i for i, p_ in enumerate(plist) if p_[8] == bk) for bk in range(2)]
            for pidx, (sc, r, nk, kc0, vch, q0, qa, qb, bank, ci) in enumerate(plist):
                nq = qb - qa
                sp_ = st[sti % NST]; sk = ("st", sti % NST); sti += 1
                self.mm(sp_[0:nk, 0:nq], KhT[:, kc0:kc0 + nk], Qn[:, qa:qb], True, False,
                        [("KhT",), ("Qn",)], [sk])
                self.mm(sp_[0:nk, 0:nq], self.kpeT[:, kc0:kc0 + nk], Qpe[:, qa:qb], False, True,
                        [("kpeT",), ("Qpe",)], [sk])
                if len(pend) >= DEPTH:
                    pend.pop(0)()
                pp = pt[pti % NPT]; pkey = ("pt", pti % NPT); pti += 1
                self.act(pp[0:nk, 0:nq], sp_[0:nk, 0:nq], ACTF.Exp, [sk], [pkey], scale=scale)
                if sc >= 0 and qa == q0:
                    self.tt("dve", pp[:, 0:128], pp[:, 0:128], maskT[:, r, :], ALU.mult,
                            [pkey, "maskT"], [pkey])
                use_dve = (pidx % 2 == 1)
                if use_dve:
                    self.tt("dve", acc[0:nk, qa:qb], acc[0:nk, qa:qb], pp[0:nk, 0:nq], ALU.add,
                            [pkey, ("acc", bank)], [("acc", bank)])
                    first_pe = False
                else:
                    first_pe = not started[bank]
                    started[bank] = True

                def pv(bank=bank, qa=qa, qb=qb, nk=nk, vch=vch, pp=pp, nq=nq, sc=sc, pkey=pkey,
                       use_dve=use_dve, first_pe=first_pe, lastp=(pidx == lastidx[bank])):
                    self.mm(ot[bank][:, qa - 512 * bank: qb - 512 * bank], Vh[0:nk, vch, :], pp[0:nk, 0:nq],
                            sc < 0, lastp, [("Vh",), pkey], [("ot", bank)])
                    if not use_dve:
                        self.mm(gen[bank][:, qa - 512 * bank: qb - 512 * bank], self.ones[0:nk, :], pp[0:nk, 0:nq],
                                first_pe, False, ["ones", pkey], [("gen", bank)])
                pend.append(pv)
            while pend:
                pend.pop(0)()
            for bank in range(2):
                cols = slice(bank * 512, (bank + 1) * 512)
                self.mm(gen[bank][:, :], ones32[:, :], acc[:, cols], False, True, ["ones32", ("acc", bank)],
                        [("gen", bank)])
            for bank in range(2):
                cols = slice(bank * 512, (bank + 1) * 512)
                self.S.op("dve", lambda e, bank=bank, cols=cols: e.reciprocal(rcp[:, cols], rs[bank][:, :]),
                          [("gen", bank)], [("rcp", bank)])
                self.tt("dve", self.OT[:, h, cols], ot[bank][:, :], rcp[:, cols], ALU.mult,
                        [("ot", bank), ("rcp", bank)], [("OT", h, bank)])
        self.release(mk)

    def debug_dump_attn(self):
        o1 = self.dout("dbg_OT", [128, NH, NOWN], BF16)
        self.dma("sp", o1[:, :, :], self.OT[:], [("OT",)], [], "outs")

    def phase_mix(self):
        mkR = self.mark()
        aTo = self.sb("aTo", [128, 16, NOWN + 128], BF16)
        mixedT = self.sb("mixedT", [128, 8, NOWN], BF16)
        psc = self.sb("psc", [128, 8], F32)
        self.dma("sp", psc[:], self.pool_scale_d[:, :], [], ["psc"], "psc")
        mk2 = self.mark()
        self.alloc_norm()
        self.norm_pipeline([(self.x_own[b * 128:(b + 1) * 128, :], 128, aTo, b * 128, "aTo") for b in range(9)])
        self.release(mk2)
        if getattr(self, "stop", 9) == 1:
            return
        wpool = self.sb("wpool", [128, 16, 1024], BF16)
        for kc in range(0, 16, 4):
            for hh in range(2):
                self.dma("pool", wpool[:, kc:kc + 4, hh * 512:(hh + 1) * 512],
                         self.w_in[kc * 128:(kc + 4) * 128, 1088 + hh * 512:1088 + (hh + 1) * 512].rearrange("(k p) c -> p k c", p=128),
                         [], [("wpool", kc, hh)], f"wpool{kc}_{hh}")
        poolw = self.sb("poolw", [128, 4, 2, 256], BF16)
        for g in range(4):
            self.dma("pool", poolw[:, g, :, :], self.pool_w_d[g * 256:(g + 1) * 256, :].rearrange("(k p) d -> p k d", p=128),
                     [], [("poolw", g)], f"poolw{g}")
        ub = self.sb("ub", [128, 8, 144], F32)
        sA = self.sb("sA", [128, 8, 144], F32)
        sB = self.sb("sB", [128, 8, 144], F32)
        pooledT = self.sb("pooledT", [128, 8, NOWN], BF16)
        up = [self.ps(f"up{i}", [128, 512], F32) for i in range(3)]
        mx = [self.ps(f"mx{i}", [128, 512], F32) for i in range(2)]
        npieces = [(0, 512), (512, 512), (1024, 128)]
        for c in range(8 if getattr(self, "sub", 9) >= 1 else 0):
            for pi, (c0, n) in enumerate(npieces):
                for kc in range(16):
                    self.mm(up[pi][:, 0:n], wpool[:, kc, c * 128:(c + 1) * 128], aTo[:, kc, c0:c0 + n],
                            kc == 0, kc == 15, [("wpool", kc - kc % 4), ("aTo",)], [("up", pi)])
            self.cp("act", ub[:, 0:4, 16:144], up[0][:, :].rearrange("p (m t) -> p m t", t=128), [("up", 0)], [("ub", 0)])
            self.cp("act", ub[:, 4:8, 16:144], up[1][:, :].rearrange("p (m t) -> p m t", t=128), [("up", 1)], [("ub", 1)])
            self.cp("act", ub[:, :, 0:16], up[2][:, 0:128].rearrange("p (m t) -> p m t", t=16), [("up", 2)], [("ub", 2)])
            if getattr(self, "sub", 9) < 2:
                continue
            g = c // 2
            srcs = [ub, sA, sB, sA, sB]
            keys = ["ub", "sA", "sB", "sA", "sB"]
            d = 1
            lo = 0
            for step in range(g + 1):
                src, dst = srcs[step], srcs[step + 1]
                lo2 = lo + d
                self.tt("pool", dst[:, :, lo2:144], src[:, :, lo2:144], src[:, :, lo:144 - d], ALU.add,
                        [(keys[step],)], [(keys[step + 1],)])
                lo = lo2
                d *= 2
            fin = srcs[g + 1]
            self.stt("dve", pooledT[:, c, :].rearrange("p (m t) -> p m t", t=128), fin[:, :, 16:144], 1.0 / d,
                     ub[:, :, 16:144], ALU.mult, ALU.subtract, [(keys[g + 1],), ("ub",)], [("pooledT", c)])
        for g in range(4 if getattr(self, "sub", 9) >= 3 else 0):
            for dl in range(2):
                dc = 2 * g + dl
                for nh in range(2):
                    cols = slice(nh * 512, (nh + 1) * 512)
                    p = mx[(dc * 2 + nh) % 2]; pk = ("mx", (dc * 2 + nh) % 2)
                    for cc in range(2):
                        self.mm(p[:, :], poolw[:, g, cc, dl * 128:(dl + 1) * 128], pooledT[:, 2 * g + cc, cols],
                                cc == 0, cc == 1, [("poolw",), ("pooledT",)], [pk])
                    self.ts("dve", mixedT[:, dc, cols], p[:, :], psc[:, dc:dc + 1], None, ALU.mult, None,
                            [pk, "psc"], [("mixedT", dc, nh)])
        self.release(mk2)
        if getattr(self, "stop", 9) == 2:
            return
        self.mergedT = self.sb("mergedT", [128, 16, NOWN], BF16, side="left")
        CW = 256
        NCG = D // CW
        wgm = [self.sb(f"wgm{i}", [128, 16, CW], BF16) for i in range(2)]
        wgp = [self.sb(f"wgp{i}", [128, 16, CW], BF16) for i in range(2)]
        wo = [self.sb(f"wo{i}", [128, 16, CW], BF16) for i in range(2)]
        wpo = [self.sb(f"wpo{i}", [128, 8, CW], BF16) for i in range(2)]
        sg = [self.sb(f"sg{i}", [128, 2, 512], F32) for i in range(2)]
        tm = [self.sb(f"tm{i}", [128, 2, 512], F32) for i in range(2)]
        pg = [[self.ps(f"pg{i}_{j}", [128, 512], F32) for j in range(4)] for i in range(2)]

        def load_cg(cg):
            wb = cg % 2
            csl = slice(cg * CW, (cg + 1) * CW)
            for kc in range(0, 16, 8):
                self.dma("pool", wgm[wb][:, kc:kc + 8, :],
                         self.w_in[kc * 128:(kc + 8) * 128, 2112 + cg * CW: 2112 + (cg + 1) * CW].rearrange("(k p) c -> p k c", p=128),
                         [], [("wgm", wb, kc)], f"wgm{wb}_{kc}")
                self.dma("pool", wgp[wb][:, kc:kc + 8, :],
                         self.w_in[kc * 128:(kc + 8) * 128, 4160 + cg * CW: 4160 + (cg + 1) * CW].rearrange("(k p) c -> p k c", p=128),
                         [], [("wgp", wb, kc)], f"wgp{wb}_{kc}")
                self.dma("pool", wo[wb][:, kc:kc + 8, :],
                         self.w_o_d[kc * 128:(kc + 8) * 128, csl].rearrange("(k p) c -> p k c", p=128),
                         [], [("wo", wb, kc)], f"wo{wb}_{kc}")
            self.dma("pool", wpo[wb][:, :, :],
                     self.w_po_d[:, csl].rearrange("(k p) c -> p k c", p=128),
                     [], [("wpo", wb)], f"wpo{wb}")

        it = 0
        load_cg(0)
        for cg in range(NCG):
            wb = cg % 2
            if cg + 1 < NCG:
                load_cg(cg + 1)
            for mt in range(CW // 128):
                f = cg * (CW // 128) + mt
                msl = slice(mt * 128, (mt + 1) * 128)
                for nh in range(2):
                    cols = slice(nh * 512, (nh + 1) * 512)
                    i2 = it % 2
                    it += 1
                    P = pg[i2]
                    for kc in range(16):
                        self.mm(P[0][:, :], wgm[wb][:, kc, msl], aTo[:, kc, cols], kc == 0, kc == 15,
                                [("wgm", wb, kc - kc % 8), ("aTo",)], [("pg", i2, 0)])
                    for kc in range(16):
                        self.mm(P[1][:, :], wgp[wb][:, kc, msl], aTo[:, kc, cols], kc == 0, kc == 15,
                                [("wgp", wb, kc - kc % 8), ("aTo",)], [("pg", i2, 1)])
                    for kc in range(16):
                        self.mm(P[2][:, :], wo[wb][:, kc, msl], self.OT[:, kc, cols], kc == 0, kc == 15,
                                [("wo", wb, kc - kc % 8), ("OT",)], [("pg", i2, 2)])
                    for kc in range(8):
                        self.mm(P[3][:, :], wpo[wb][:, kc, msl], mixedT[:, kc, cols], kc == 0, kc == 7,
                                [("wpo", wb), ("mixedT",)], [("pg", i2, 3)])
                    self.act(sg[i2][:, 0, :], P[0][:, :], ACTF.Sigmoid, [("pg", i2, 0)], [("sg", i2, 0)])
                    self.act(sg[i2][:, 1, :], P[1][:, :], ACTF.Sigmoid, [("pg", i2, 1)], [("sg", i2, 1)])
                    self.tt("dve", tm[i2][:, 0, :], P[2][:, :], sg[i2][:, 0, :], ALU.mult,
                            [("pg", i2, 2), ("sg", i2, 0)], [("tm", i2, 0)])
                    self.tt("dve", tm[i2][:, 1, :], P[3][:, :], sg[i2][:, 1, :], ALU.mult,
                            [("pg", i2, 3), ("sg", i2, 1)], [("tm", i2, 1)])
                    self.tt("dve", self.mergedT[:, f, cols], tm[i2][:, 0, :], tm[i2][:, 1, :], ALU.add,
                            [("tm", i2)], [("mergedT", f, nh)])
        self.release(mkR)
        if getattr(self, "stop", 9) == 3:
            return
        self.h2 = self.sb("h2", [128, OWN, D], F32)
        self.mkH = self.mark()
        self.dma("sp", self.h2[:], self.x_own[0:NOWN, :].rearrange("(m p) d -> p m d", p=128), [], [("h2",)], "h2ld")
        wout = [self.sb(f"wout{i}", [128, 16, 512], BF16) for i in range(2)]
        po = [self.ps(f"po{i}", [128, 512], F32) for i in range(4)]
        it = 0
        for cg in range(4):
            c512 = slice(cg * 512, (cg + 1) * 512)
            w = wout[cg % 2]
            for kc in range(0, 16, 4):
                self.dma("pool", w[:, kc:kc + 4, :],
                         self.w_out_d[kc * 128:(kc + 4) * 128, c512].rearrange("(k p) c -> p k c", p=128),
                         [], [("wout", cg % 2, kc)], f"wout{cg % 2}_{kc}")
            for m in range(OWN):
                p = po[it % 4]; pk = ("po", it % 4); it += 1
                for kc in range(16):
                    self.mm(p[:, :], self.mergedT[:, kc, m * 128:(m + 1) * 128], w[:, kc, :], kc == 0, kc == 15,
                            [("mergedT",), ("wout", cg % 2, kc - kc % 4)], [pk])
                self.tt("dve", self.h2[:, m, c512], p[:, :], self.h2[:, m, c512], ALU.add,
                        [pk, ("h2", m, cg)], [("h2", m, cg)])
        self.release(self.mkH)
        self.releaseL(self.mkW)

    def debug_dump_h2(self):
        o1 = self.dout("dbg_h2", [128, OWN, D], F32)
        self.dma("sp", o1[:, :, :], self.h2[:], [("h2",)], [], "outs")

    def phase_moe(self, nexp=NE):
        BIG = 30000.0
        h2 = self.h2
        bn = self.sb("bn", [128, OWN, D], BF16)
        Cw = self.sb("Cw", [128, OWN, NE], F32)
        pos = self.sb("pos", [128, OWN, NE], F32)
        pos64 = self.sb("pos64", [128, OWN, NE], F32)
        iota = self.sb("iota", [128, CAP], F32)
        self.dma("sp", iota[:], self.iota_d[:, :], [], ["iota"], "c4b")
        mkR = self.mark()
        selA = self.sb("selA", [128, OWN, NE], F32)
        selB = self.sb("selB", [128, OWN, NE], BF16)
        ustr = self.sb("ustr", [128, 128], BF16)
        ident32 = self.sb("ident32", [128, 128], F32)
        self.dma("sp", ustr[:], self.ustr_d[:, :], [], ["ustr"], "c4a")
        self.dma("sp", ident32[:], self.ident32_d[:, :], [], ["ident32"], "c4c")
        wffn = self.sb("wffn", [128, D], F32)
        self.dma("sp", wffn[:], self.vec_ffn[0:1, :].partition_broadcast(128), [], ["wffn"], "c4d")
        wr32 = self.sb("wr32", [128, 16, 72], F32)
        self.dma("sp", wr32[:], self.w_r_d.rearrange("(k p) c -> p k c", p=128), [], ["wr32"], "c4e")
        brt = self.sb("brt", [128, 72], F32)
        self.dma("sp", brt[:], self.b_r_d[0:1, :].partition_broadcast(128), [], ["brt"], "c4f")
        bn32s = [self.sb(f"bn32_{i}", [128, D], F32) for i in range(2)]
        bnT32 = self.sb("bnT32", [128, 16, 128], F32)
        rts = [self.sb(f"rt{i}", [128, 64], F32) for i in range(2)]
        lgs = [self.sb(f"lg{i}", [128, 72], F32) for i in range(2)]
        gm = self.sb("gm", [128, 8], F32)
        gex = self.sb("gex", [128, 8], F32)
        pen = self.sb("pen", [128, 8], F32)
        elms = [self.sb(f"elm{i}", [128, NE], F32) for i in range(2)]
        ee = self.sb("ee", [128, NE], F32)
        c0 = self.sb("c0", [128, NE], F32)
        top8 = self.sb("top8", [128, 8], F32)
        tpr = [self.ps(f"tpr{i}", [128, 512], F32) for i in range(2)]
        lgp = [self.ps(f"lgp{i}", [128, 512], F32) for i in range(2)]
        posp = self.ps("posp", [128, 512], F32)

        def stage_a(m):
            i2 = m % 2
            rt = rts[i2]; bn32 = bn32s[i2]
            self.act(bn[:, m, :], h2[:, m, :], ACTF.Square, [("h2", m)], [("bn", m), ("rt", i2, 0)],
                     accum_out=rt[:, 0:1])
            self.act(rt[:, 1:2], rt[:, 0:1], ACTF.Sqrt, [("rt", i2, 0)], [("rt", i2, 1)], bias=EPS, scale=1.0 / D)
            self.S.op("dve", lambda e: e.reciprocal(rt[:, 2:3], rt[:, 1:2]), [("rt", i2, 1)], [("rt", i2, 2)])
            self.stt("dve", bn32[:, :], h2[:, m, :], rt[:, 2:3], wffn[:, :], ALU.mult, ALU.mult,
                     [("h2", m), ("rt", i2, 2), "wffn"], [("bn32", i2)])
            self.cp("act", bn[:, m, :], bn32[:, :], [("bn32", i2)], [("bn", m)])

        def stage_b(m):
            i2 = m % 2
            rt = rts[i2]; bn32 = bn32s[i2]; lg = lgs[i2]; elm = elms[i2]
            for q in range(4):
                t = tpr[q % 2]
                for i in range(4):
                    kc = q * 4 + i
                    self.tr(t[:, i * 128:(i + 1) * 128], bn32[:, kc * 128:(kc + 1) * 128], ident32[:, :],
                            [("bn32", i2), "ident32"], [("tpr", q % 2)])
                self.cp("act" if q % 2 else "dve", bnT32[:, q * 4:(q + 1) * 4, :],
                        t[:, :].rearrange("p (k t) -> p k t", t=128), [("tpr", q % 2)], [("bnT32", q)])
            lp = lgp[i2]
            for kc in range(16):
                self.mm(lp[:, 0:72], bnT32[:, kc, :], wr32[:, kc, :], kc == 0, kc == 15,
                        [("bnT32", kc // 4), "wr32"], [("lgp", i2)])
            self.tt("dve", lg[:, :], lp[:, 0:72], brt[:, :], ALU.add, [("lgp", i2), "brt"], [("lg", i2)])
            self.S.op("dve", lambda e: e.tensor_reduce(rt[:, 3:4], lg[:, 0:8], AX.X, ALU.max), [("lg", i2)],
                      [("rt", i2, 3)])
            self.ts("dve", gm[:, :], lg[:, 0:8], rt[:, 3:4], None, ALU.is_ge, None, [("lg", i2), ("rt", i2, 3)], ["gm"])
            self.ts("dve", rt[:, 4:5], rt[:, 3:4], -1.0, None, ALU.mult, None, [("rt", i2, 3)], [("rt", i2, 4)])
            self.act(gex[:, :], lg[:, 0:8], ACTF.Exp, [("lg", i2), ("rt", i2, 4)], ["gex", ("rt", i2, 5)],
                     bias=rt[:, 4:5], accum_out=rt[:, 5:6])
            self.ts("dve", pen[:, :], gm[:, :], BIG, -BIG, ALU.mult, ALU.add, ["gm"], ["pen"])
            for g in range(8):
                self.ts("dve", elm[:, g * 8:(g + 1) * 8], lg[:, 8 + g * 8: 16 + g * 8], pen[:, g:g + 1], None,
                        ALU.add, None, [("lg", i2), "pen"], [("elm", i2, g)])
            self.S.op("dve", lambda e: e.max(out=top8[:, :], in_=elm[:, :]), [("elm", i2)], ["top8"])
            self.ts("dve", selA[:, m, :], elm[:, :], top8[:, 1:2], None, ALU.is_ge, None, [("elm", i2), "top8"],
                    [("selA", m)])
            self.cp("dve", selB[:, m, :], selA[:, m, :], [("selA", m)], [("selB", m)])
            self.ts("dve", rt[:, 6:7], top8[:, 0:1], -1.0, None, ALU.mult, None, ["top8"], [("rt", i2, 6)])
            self.act(ee[:, :], elm[:, :], ACTF.Exp, [("elm", i2), ("rt", i2, 6)], ["ee"], bias=rt[:, 6:7])
            self.tt("dve", c0[:, :], selA[:, m, :], ee[:, :], ALU.mult, [("selA", m), "ee"], ["c0"])
            self.S.op("dve", lambda e: e.tensor_reduce(rt[:, 7:8], c0[:, :], AX.X, ALU.add), ["c0"], [("rt", i2, 7)])
            self.tt("dve", rt[:, 8:9], rt[:, 7:8], rt[:, 5:6], ALU.mult, [("rt", i2, 7), ("rt", i2, 5)], [("rt", i2, 8)])
            self.S.op("dve", lambda e: e.reciprocal(rt[:, 9:10], rt[:, 8:9]), [("rt", i2, 8)], [("rt", i2, 9)])
            self.ts("dve", Cw[:, m, :], c0[:, :], rt[:, 9:10], None, ALU.mult, None, ["c0", ("rt", i2, 9)], [("Cw", m)])
            self.mm(posp[:, 0:NE], ustr[:, :], selB[:, m, :], True, m == 0, ["ustr", ("selB", m)], ["posp"])
            for mp in range(m):
                self.mm(posp[:, 0:NE], self.ones[:, :], selB[:, mp, :], False, mp == m - 1,
                        ["ones", ("selB", mp)], ["posp"])
            self.stt("dve", pos[:, m, :], posp[:, 0:NE], 1.0, selA[:, m, :], ALU.add, ALU.mult,
                     ["posp", ("selA", m)], [("pos", m)])
            self.ts("dve", pos[:, m, :], pos[:, m, :], -1.0, None, ALU.add, None, [("pos", m)], [("pos", m)])
            self.ts("dve", pos64[:, m, :], pos[:, m, :], 64.0, None, ALU.add, None, [("pos", m)], [("pos64", m)])

        stage_a(0)
        for m in range(OWN):
            if m + 1 < OWN:
                stage_a(m + 1)
            stage_b(m)
        self.release(mkR)
        if self.stage == "route":
            return Cw, pos
        wg = [self.sb(f"wg{i}", [128, 16, FF], BF16) for i in range(2)]
        wu = [self.sb(f"wu{i}", [128, 16, FF], BF16) for i in range(2)]
        wd = self.sb("wd", [128, 4, D], BF16)
        SelE = self.sb("SelE", [128, OWN, CAPG], BF16)
        SelW = self.sb("SelW", [128, 2, CAP], BF16)
        SelWT = [self.sb(f"SelWT{i}", [128, OWN, 128], BF16) for i in range(2)]
        xeT = self.sb("xeT", [128, 16, CAP], BF16)
        self.S.op("dve", lambda e: e.memset(xeT[:], 0.0), [], [("xeT",)])
        hdn = self.sb("hdn", [128, FF], BF16)
        hdnT = self.sb("hdnT", [128, 4, 128], BF16)
        ye = [self.sb(f"ye{i}", [128, D], BF16) for i in range(2)]
        for yt_ in ye:
            self.S.op("dve", lambda e, yt_=yt_: e.memset(yt_[:], 0.0), [], [("ye",)])
        gx = [self.ps(f"gx{i}", [128, 512], F32) for i in range(2)]
        fy = [self.ps(f"fy{i}", [128, 512], F32) for i in range(3)]
        tq = self.ps("tq", [128, 1024], BF16)
        scp = [self.ps(f"scp{i}", [128, 512], F32) for i in range(2)]

        conv = (self.stage == "full")

        def load_gu(i):
            e = order[i]
            b = i % 2
            pre = conv and e < NCONV + NCONVA
            q = "sp" if pre else "pool"
            sg_, su_ = (self.seg_bf, self.seu_bf) if pre else (self.w_eg_d, self.w_eu_d)
            for kc in range(0, 16, 4):
                r0 = e * D + kc * 128
                self.dma(q, wg[b][:, kc:kc + 4, :],
                         sg_[r0:r0 + 512, :].rearrange("(k p) c -> p k c", p=128),
                         [("cvg", e)] if pre else [], [("wg", b, kc)], f"wg{b}_{kc}{q}")
                self.dma(q, wu[b][:, kc:kc + 4, :],
                         su_[r0:r0 + 512, :].rearrange("(k p) c -> p k c", p=128),
                         [("cvu", e)] if pre else [], [("wu", b, kc)], f"wu{b}_{kc}{q}")

        def load_d(i):
            e = order[i]
            pre = conv and e < NCONV + NCONVA
            q = "sp" if pre else "pool"
            sd_ = self.sed_bf if pre else self.w_ed_d
            for hh in range(2):
                self.dma(q, wd[:, :, hh * 1024:(hh + 1) * 1024],
                         sd_[e * FF:(e + 1) * FF, hh * 1024:(hh + 1) * 1024].rearrange("(k p) c -> p k c", p=128),
                         [("cvd", e)] if pre else [], [("wd", hh)], f"wd{hh}{q}")

        if conv:
            la, lb = list(range(NCONV + NCONVA)), list(range(NCONV + NCONVA, NE))
            order = []
            for i in range(max(len(la), len(lb))):
                if i < len(la):
                    order.append(la[i])
                if i < len(lb):
                    order.append(lb[i])
            assert sorted(order) == list(range(NE))
        else:
            order = list(range(nexp))
        pending = []
        state = {"sci": 0}

        def drain(k=1):
            for _ in range(k):
                if pending:
                    pending.pop(0)()

        def make_scatter(pi, m, cgp):
            c512 = slice(cgp * 512, (cgp + 1) * 512)
            rb = pi % 2

            def f():
                i = state["sci"] % 2
                state["sci"] += 1
                p = scp[i]; pk = ("scp", i)
                self.mm(p[:, :], SelWT[rb][:, m, :], ye[rb][:, c512], True, True,
                        [("SelWT", rb), ("ye", rb, cgp)], [pk])
                self.tt("dve", h2[:, m, c512], p[:, :], h2[:, m, c512], ALU.add,
                        [pk, ("h2", m, cgp)], [("h2", m, cgp)])
            return f

        load_gu(0)
        load_d(0)
        gxi = 0
        nexp = len(order)
        for ei in range(nexp):
            e = order[ei]
            b = ei % 2
            if ei + 1 < nexp:
                load_gu(ei + 1)
            half = ei % 2
            pi = ei // 2
            rb = pi % 2
            off = 64 * half
            psel = pos64 if half else pos
            if half == 0:
                eA = e
                eB = order[ei + 1] if ei + 1 < nexp else None
                for m in range(OWN):
                    self.ts("dve", SelW[:, m % 2, 0:64], iota[:, 0:64], pos[:, m, eA:eA + 1], Cw[:, m, eA:eA + 1],
                            ALU.is_equal, ALU.mult, ["iota", ("pos", m), ("Cw", m)], [("SelW", m % 2, 0)])
                    if eB is not None:
                        self.ts("dve", SelW[:, m % 2, 64:128], iota[:, 64:128], pos64[:, m, eB:eB + 1],
                                Cw[:, m, eB:eB + 1], ALU.is_equal, ALU.mult,
                                ["iota", ("pos64", m), ("Cw", m)], [("SelW", m % 2, 1)])
                    else:
                        self.S.op("dve", lambda e_, m=m: e_.memset(SelW[:, m % 2, 64:128], 0.0), [],
                                  [("SelW", m % 2, 1)])
                    self.tr(tq[:, m * 128:(m + 1) * 128], SelW[:, m % 2, :], self.ident[:, :],
                            [("SelW", m % 2), "ident"], ["tq"])
                    if m % 2:
                        drain()
                self.cp("act", SelWT[rb][:, :, :], tq[:, :].rearrange("p (m t) -> p m t", t=128), ["tq"],
                        [("SelWT", rb)])
            for m in range(OWN):
                self.ts("dve", SelE[:, m, :], iota[:, off:off + CAPG], psel[:, m, e:e + 1], None,
                        ALU.is_equal, None, ["iota", ("pos", m), ("pos64", m)], [("SelE", m)])
            for fq in range(4):
                p = gx[gxi % 2]; pk = ("gx", gxi % 2); gxi += 1
                for fi in range(4):
                    f = fq * 4 + fi
                    for m in range(OWN):
                        self.mm(p[:, fi * 128 + off:fi * 128 + off + CAPG], bn[:, m, f * 128:(f + 1) * 128], SelE[:, m, :],
                                m == 0, m == OWN - 1, [("bn", m), ("SelE", m)], [pk])
                    drain()
                self.cp("act" if fq % 2 else "dve", xeT[:, fq * 4:(fq + 1) * 4, off:off + CAPG],
                        p[:, :].rearrange("p (k t) -> p k t", t=128)[:, :, off:off + CAPG], [pk], [("xeT", fq)])
            gp, upp = fy[0], fy[1]
            for kc in range(16):
                self.mm(gp[:, :], xeT[:, kc, :], wg[b][:, kc, :], kc == 0, kc == 15,
                        [("xeT", kc // 4), ("wg", b, kc - kc % 4)], [("fy", 0)])
                if kc % 4 == 3:
                    drain()
            for kc in range(16):
                self.mm(upp[:, :], xeT[:, kc, :], wu[b][:, kc, :], kc == 0, kc == 15,
                        [("xeT", kc // 4), ("wu", b, kc - kc % 4)], [("fy", 1)])
                if kc % 4 == 3:
                    drain()
            self.act(hdn[:, :], gp[:, :], ACTF.Silu, [("fy", 0)], ["hdn"])
            self.tt("dve", hdn[:, :], upp[:, :], hdn[:, :], ALU.mult, [("fy", 1), "hdn"], ["hdn"])
            drain(2)
            for kc in range(4):
                self.tr(tq[:, kc * 128:(kc + 1) * 128], hdn[:, kc * 128:(kc + 1) * 128], self.ident[:, :],
                        ["hdn", "ident"], ["tq"])
            self.cp("act", hdnT[:, :, :], tq[:, 0:512].rearrange("p (k t) -> p k t", t=128), ["tq"], ["hdnT"])
            drain(2)
            for cgp in range(4):
                c512 = slice(cgp * 512, (cgp + 1) * 512)
                fi_ = (2 + cgp) % 3
                yp = fy[fi_]
                for kc in range(4):
                    self.mm(yp[:, :], hdnT[:, kc, :], wd[:, kc, c512], kc == 0, kc == 3,
                            ["hdnT", ("wd", cgp // 2)], [("fy", fi_)])
                self.cp("act" if cgp % 2 else "dve", ye[rb][off:off + 64, c512], yp[off:off + 64, :],
                        [("fy", fi_)], [("ye", rb, cgp, half)])
                drain()
            if half == 1:
                drain(64)
            if ei + 1 < nexp:
                load_d(ei + 1)
            if half == 1 or ei == nexp - 1:
                for m in range(OWN):
                    for cgp in range(4):
                        pending.append(make_scatter(pi, m, cgp))
        drain(64)
        self.release(mkR)

    def phase_final(self):
        h2 = self.h2
        wfin = self.sb("wfin", [128, D], F32)
        self.dma("sp", wfin[:], self.vec_fin[0:1, :].partition_broadcast(128), [], ["wfin"], "c5")
        ot = [self.sb(f"outt{i}", [128, D], F32) for i in range(2)]
        junk = self.sb("junkf", [128, D], BF16)
        rt = self.sb("rtf", [128, 32], F32)
        for m in range(OWN):
            c = m * 3
            self.act(junk[:, :], h2[:, m, :], ACTF.Square, [("h2", m)], ["junkf", ("rtf", c)],
                     accum_out=rt[:, c:c + 1])
            self.act(rt[:, c + 1:c + 2], rt[:, c:c + 1], ACTF.Sqrt, [("rtf", c)], [("rtf", c + 1)], bias=EPS, scale=1.0 / D)
            self.S.op("dve", lambda e, c=c: e.reciprocal(rt[:, c + 2:c + 3], rt[:, c + 1:c + 2]),
                      [("rtf", c + 1)], [("rtf", c + 2)])
            o = ot[m % 2]
            self.stt("dve", o[:, :], h2[:, m, :], rt[:, c + 2:c + 3], wfin[:, :], ALU.mult, ALU.mult,
                     [("h2", m), ("rtf", c + 2), "wfin"], [("outt", m % 2)])
            self.dma("sp", self.out_d[m * 128:(m + 1) * 128, :], o[:, :], [("outt", m % 2)], [], f"outs{m % 2}")

    def debug_dump_phase_a(self):
        o1 = self.dout("dbg_ckvT", [128, 4, L], BF16)
        o2 = self.dout("dbg_kpeT", [64, L], BF16)
        self.dma("sp", o1[:, :, :], self.ckvT[:], [("ckvT",)], [], "outs")
        self.dma("sp", o2[:, :], self.kpeT[0:64, :], [("kpeT",)], [], "outs")


def rope_tables_np():
    inv = 1.0 / (10000.0 ** (np.arange(0, DR, 2, dtype=np.float32) / DR))
    ang = np.arange(L, dtype=np.float32)[:, None] * inv[None, :].astype(np.float32)
    ang = ang.astype(np.float32)
    cos = np.cos(ang).astype(np.float32)
    sin = np.sin(ang).astype(np.float32)
    cosT = np.ascontiguousarray(np.concatenate([cos, cos], axis=1).T)
    sinT = np.ascontiguousarray(np.concatenate([sin, sin], axis=1).T)
    return cosT, sinT


def _uq_swapped(w_uq):
    w = w_uq.reshape(QL, NH, 192)[:, :, 128:192]
    return np.ascontiguousarray(np.concatenate([w[:, :, 32:64], w[:, :, 0:32]], axis=2).reshape(QL, NH * 64))


def own_blocks(core):
    return [8 * m + core for m in range(OWN)]


def prep_core(common, core):
    xa = common["x_all"]
    blks = own_blocks(core)
    main = np.concatenate([xa[NMETA + 128 * j: NMETA + 128 * j + 128] for j in blks], axis=0)
    halo = np.concatenate([xa[128 * j: 128 * j + 16] for j in blks], axis=0)
    pos = np.concatenate([np.arange(NMETA + 128 * j, NMETA + 128 * j + 128) for j in blks])
    k = np.arange(128)[:, None]
    q = np.arange(128)[None, :]
    mask = np.zeros((128, 8, 128), np.float32)
    for r in range(8):
        if r < core:
            mask[:, r, :] = 1.0
        elif r == core:
            mask[:, r, :] = (k <= q).astype(np.float32)
    m = dict(common)
    m["x_own"] = np.ascontiguousarray(np.concatenate([main, halo], axis=0))
    m["cos_own"] = np.ascontiguousarray(common["cos_all"][:, pos])
    m["sin_own"] = np.ascontiguousarray(common["sin_all"][:, pos])
    m["maskT"] = mask.astype(ml_dtypes.bfloat16)
    return m


def prep_common(inp):
    x = np.asarray(inp["x"], np.float32)[0]
    meta = np.asarray(inp["meta_tokens"], np.float32)
    w_in = np.ascontiguousarray(np.asarray(inp["w_in"], np.float32)[0])
    kr = w_in[:, 1024:1088]
    cosT, sinT = rope_tables_np()
    m = {
        "x_all": np.ascontiguousarray(np.concatenate([meta, x], axis=0)),
        "w_in": w_in,
        "w_ks": np.ascontiguousarray(np.concatenate([kr[:, 32:64], kr[:, 0:32]], axis=1)),
        "norm_mix_w": np.ascontiguousarray(np.asarray(inp["norm_mix_w"], np.float32).reshape(1, D)),
        "kv_norm_w": np.ascontiguousarray(np.asarray(inp["kv_norm_w"], np.float32).reshape(4, 128).T),
        "q_norm_w": np.ascontiguousarray(np.asarray(inp["q_norm_w"], np.float32).reshape(4, 128).T),
        "cos_all": cosT,
        "sin_all": sinT,
        "w_uq": np.ascontiguousarray(np.asarray(inp["w_uq"], np.float32)[0]),
        "w_uqs": _uq_swapped(np.asarray(inp["w_uq"], np.float32)[0]),
        "w_ukv": np.ascontiguousarray(np.asarray(inp["w_ukv"], np.float32)[0]),
        "pool_scale": np.ascontiguousarray(np.asarray(inp["pool_scale"], np.float32).reshape(8, 128).T),
        "pool_w": np.ascontiguousarray(np.asarray(inp["pool_w"], np.float32)[0].reshape(1024, 256)),
        "w_o_mla": np.ascontiguousarray(np.asarray(inp["w_o_mla"], np.float32)[0]),
        "w_pool_out": np.ascontiguousarray(np.asarray(inp["w_pool_out"], np.float32)[0]),
        "w_out": np.ascontiguousarray(np.asarray(inp["w_out"], np.float32)[0]),
        "ustr": np.triu(np.ones((128, 128), np.float32), 1).astype(ml_dtypes.bfloat16),
        "iota": np.ascontiguousarray(np.broadcast_to(np.arange(CAP, dtype=np.float32)[None, :], (128, CAP))),
        "ident32": np.eye(128, dtype=np.float32),
        "norm_ffn_w": np.ascontiguousarray(np.asarray(inp["norm_ffn_w"], np.float32).reshape(1, D)),
        "final_norm_w": np.ascontiguousarray(np.asarray(inp["final_norm_w"], np.float32).reshape(1, D)),
        "w_r": np.ascontiguousarray(np.concatenate([np.asarray(inp["w_router_group"], np.float32)[0],
                                                    np.asarray(inp["w_router_expert"], np.float32)[0]], axis=1)),
        "b_r": np.ascontiguousarray(np.concatenate([np.asarray(inp["b_router_group"], np.float32)[0],
                                                    np.asarray(inp["b_router_expert"], np.float32)[0]])[None, :]),
        "w_exp_gate": np.asarray(inp["w_exp_gate"], np.float32).reshape(NE * D, FF),
        "w_exp_up": np.asarray(inp["w_exp_up"], np.float32).reshape(NE * D, FF),
        "w_exp_down": np.asarray(inp["w_exp_down"], np.float32).reshape(NE * FF, D),
        "ident_bf": np.eye(128, dtype=np.float32).astype(ml_dtypes.bfloat16),
        "ones_bf": np.ones((128, 128), np.float32).astype(ml_dtypes.bfloat16),
    }
    return m


def build_full():
    B = Builder(stage="full")
    B.setup_common()
    B.phase_a()
    B.phase_b1()
    B.phase_attn()
    B.phase_mix()
    B.phase_moe()
    B.phase_final()
    B.S.emit(B.nc, final_waits=[("sp", "outs0"), ("sp", "outs1")])
    B.close()
    return B


def kernel(**inputs):
    B = build_full()
    cm = prep_common(inputs)
    names = list(B.dram.keys())
    in_maps = []
    for c in range(NCORES):
        m = prep_core(cm, c)
        in_maps.append({k: m[k] for k in names if k != "out"})
    res = run_bass_kernel_spmd(B.nc, in_maps, core_ids=list(range(NCORES)))
    out = np.zeros((1, SEQ, D), np.float32)
    for c in range(NCORES):
        o = np.asarray(res.results[c]["out"], np.float32)
        for m, j in enumerate(own_blocks(c)):
            out[0, 128 * j:128 * j + 128, :] = o[128 * m:128 * (m + 1), :]
    return out
```

```python
import numpy as np
import ml_dtypes
import concourse.bass as bass
import concourse.mybir as mybir
from concourse.bass_utils import run_bass_kernel_spmd

F32 = mybir.dt.float32
BF16 = mybir.dt.bfloat16
I32 = mybir.dt.int32
ALU = mybir.AluOpType
ACTF = mybir.ActivationFunctionType
AX = mybir.AxisListType

NCORES = 8
D = 2048
SEQ = 8192
NMETA = 16
L = SEQ + NMETA
EPS = 1e-6
NH = 16
DN = 128
DR = 64
DV = 128
QL = 512
KVL = 512
NBLK = 64
OWN = 8
NOWN = OWN * 128
NE = 64
FF = 512
CAP = 128
CAPG = 64
NCONVA = 0
NCONV = 28


class _Op:
    __slots__ = ("eng", "fn", "waits", "milestone", "semval", "dma", "idx")

    def __init__(self, eng, fn, dma=None):
        self.eng = eng
        self.fn = fn
        self.waits = []
        self.milestone = False
        self.semval = None
        self.dma = dma
        self.idx = None


class Sched:
    ENGS = ("pe", "act", "dve", "pool", "sp")

    def __init__(self):
        self.ops = {e: [] for e in self.ENGS}
        self.state = {}
        self.dma_cnt = {}
        self.total_sems = {"const", "constp", "const2", "outs"}

    @staticmethod
    def _overlap(a, b):
        n = min(len(a), len(b))
        return a[:n] == b[:n]

    def _entries(self, key):
        d = self.state.get(key[0])
        if not d:
            return []
        return [(k, v) for k, v in d.items() if self._overlap(k, key)]

    def op(self, eng, fn, reads=(), writes=(), dma_sem=None):
        reads = [r if isinstance(r, tuple) else (r,) for r in reads]
        writes = [w if isinstance(w, tuple) else (w,) for w in writes]
        if ("__mem__",) not in writes:
            reads.append(("__mem__",))
        o = _Op(eng, fn)
        if dma_sem is not None:
            dv = self.dma_cnt.get(dma_sem, 0) + 16
            self.dma_cnt[dma_sem] = dv
            o.dma = (dma_sem, dv)
            ev = ("d", dma_sem, dv)
            rkey = ("d", dma_sem)
        else:
            ev = ("c", o)
            rkey = ("c", eng)
        deps = []
        if dma_sem is not None and dma_sem not in self.total_sems and dv > 16:
            deps.append(("d", dma_sem, dv - 16))
        if dma_sem is not None and eng == "pool":
            hist = self.__dict__.setdefault("_swdge_hist", [])
            if len(hist) >= 4 and hist[-4][1] not in self.total_sems:
                deps.append(hist[-4])
            hist.append(ev)
        for r in reads:
            for k, v in self._entries(r):
                if v[0] is not None:
                    deps.append(v[0])
        for w in writes:
            for k, v in self._entries(w):
                if v[0] is not None:
                    deps.append(v[0])
                deps.extend(v[1].values())
        seen = set()
        for d in deps:
            if d[0] == "c":
                p = d[1]
                if p is o:
                    continue
                if p.eng == eng == "pe" and dma_sem is None:
                    continue
                if id(p) in seen:
                    continue
                seen.add(id(p))
                p.milestone = True
                o.waits.append(d)
            else:
                if d in seen:
                    continue
                seen.add(d)
                o.waits.append(d)
        for r in reads:
            dd = self.state.setdefault(r[0], {})
            ent = dd.get(r)
            if ent is None:
                ent = [None, {}]
                dd[r] = ent
            ent[1][rkey] = ev
        for w in writes:
            dd = self.state.setdefault(w[0], {})
            for k in [k for k in dd if len(k) > len(w) and k[:len(w)] == w]:
                del dd[k]
            dd[w] = [ev, {}]
        o.idx = len(self.ops[eng])
        self.ops[eng].append(o)
        return o

    def emit(self, nc, final_waits=()):
        engobj = {"pe": "tensor", "act": "scalar", "dve": "vector", "pool": "gpsimd", "sp": "sync"}
        for e in self.ENGS:
            c = 0
            for o in self.ops[e]:
                if o.milestone and o.dma is None:
                    c += 1
                    o.semval = c
        sems = {}
        for e in self.ENGS:
            sems[("c", e)] = nc.alloc_semaphore(name=f"s_{e}")
        for name in self.dma_cnt:
            sems[("d", name)] = nc.alloc_semaphore(name=f"d_{name}")
        self.sems = sems
        sched = self

        def run_engine(e, eng):
            waited = {}
            for o in sched.ops[e]:
                for d in o.waits:
                    if d[0] == "c":
                        key = ("c", d[1].eng)
                        val = d[1].semval
                    else:
                        key = ("d", d[1])
                        val = d[2]
                        if d[1] in sched.total_sems:
                            val = sched.dma_cnt[d[1]]
                    if waited.get(key, 0) >= val:
                        continue
                    waited[key] = val
                    eng.wait_ge(sems[key], val)
                inst = o.fn(eng)
                if o.dma is not None:
                    inst.then_inc(sems[("d", o.dma[0])], 16)
                elif o.milestone:
                    inst.then_inc(sems[("c", e)], 1)
            for (fe, name) in final_waits:
                if fe == e:
                    eng.wait_ge(sems[("d", name)], sched.dma_cnt[name])

        with nc.Block() as block:
            @block.tensor
            def _(eng):
                run_engine("pe", eng)

            @block.scalar
            def _(eng):
                run_engine("act", eng)

            @block.vector
            def _(eng):
                run_engine("dve", eng)

            @block.gpsimd
            def _(eng):
                run_engine("pool", eng)

            @block.sync
            def _(eng):
                run_engine("sp", eng)


class Builder:
    def __init__(self, stage="full", ngroups=16):
        self.stage = stage
        self.ngroups = ngroups
        self.nc = bass.Bass("TRN2", target_bir_lowering=False)
        self.S = Sched()
        self.nxt = 3
        self.dram = {}
        self._ctx = []
        self._ctxL = []

    def din(self, name, shape, dt=F32):
        t = self.nc.dram_tensor(name, list(shape), dt, kind="ExternalInput")
        self.dram[name] = t
        return t

    def dout(self, name, shape, dt=F32):
        t = self.nc.dram_tensor(name, list(shape), dt, kind="ExternalOutput")
        self.dram[name] = t
        return t

    def sb(self, name, shape, dt, side="right"):
        self._n = getattr(self, "_n", 0) + 1
        cm = self.nc.sbuf_tensor(f"{name}_{self._n}", list(shape), dt, side=side)
        t = cm.__enter__()
        (self._ctxL if side == "left" else self._ctx).append(cm)
        return t

    def markL(self):
        return len(self._ctxL)

    def releaseL(self, mark):
        self.barrier()
        while len(self._ctxL) > mark:
            self._ctxL.pop().__exit__(None, None, None)

    def ps(self, name, shape, dt=F32):
        self._n = getattr(self, "_n", 0) + 1
        cm = self.nc.psum_tensor(f"{name}_{self._n}", list(shape), dt)
        t = cm.__enter__()
        self._ctx.append(cm)
        return t

    def mark(self):
        return len(self._ctx)

    def release(self, mark):
        self.barrier()
        while len(self._ctx) > mark:
            self._ctx.pop().__exit__(None, None, None)

    def barrier(self):
        if not hasattr(self, "_bar"):
            self._bar = self.nc.alloc_sbuf_tensor("bar_scratch", [128, 8], F32) if False else None
        self.S.op("pool", lambda e: e.memset(self.bar_t[:, 0:1], 0.0), [], [("__mem__",), "bar_t"])

    def close(self):
        for cm in reversed(self._ctx):
            cm.__exit__(None, None, None)
        for cm in reversed(self._ctxL):
            cm.__exit__(None, None, None)
        self._ctx = []
        self._ctxL = []

    def dma(self, q, out, in_, reads, writes, sem):
        return self.S.op(q, lambda e: e.dma_start(out=out, in_=in_), reads, writes, dma_sem=sem)

    def mm(self, out, lhsT, rhs, start, stop, reads, writes):
        return self.S.op("pe", lambda e: e.matmul(out, lhsT, rhs, start=start, stop=stop), reads, writes)

    def tr(self, out, in_, ident, reads, writes):
        return self.S.op("pe", lambda e: e.transpose(out, in_, ident), reads, writes)

    def act(self, out, in_, func, reads, writes, **kw):
        return self.S.op("act", lambda e: e.activation(out, in_, func, **kw), reads, writes)

    def tt(self, eng, out, in0, in1, op, reads, writes):
        return self.S.op(eng, lambda e: e.tensor_tensor(out, in0, in1, op), reads, writes)

    def ts(self, eng, out, in0, s1, s2, op0, op1, reads, writes):
        if op1 is None:
            return self.S.op(eng, lambda e: e.tensor_single_scalar(out, in0, s1, op0), reads, writes)
        return self.S.op(eng, lambda e: e.tensor_scalar(out, in0, s1, s2, op0, op1), reads, writes)

    def stt(self, eng, out, in0, scalar, in1, op0, op1, reads, writes):
        return self.S.op(eng, lambda e: e.scalar_tensor_tensor(out, in0, scalar, in1, op0, op1), reads, writes)

    def cp(self, eng, out, in_, reads, writes):
        if eng == "act":
            return self.S.op("act", lambda e: e.copy(out, in_), reads, writes)
        return self.S.op(eng, lambda e: e.tensor_copy(out, in_), reads, writes)


    def setup_common(self):
        nc = self.nc
        self.x_all = self.din("x_all", [L, D])
        self.w_in = self.din("w_in", [D, 6208])
        self.w_ks = self.din("w_ks", [D, DR])
        self.vec_mix = self.din("norm_mix_w", [1, D])
        self.kvn = self.din("kv_norm_w", [128, 4])
        self.qn = self.din("q_norm_w", [128, 4])
        self.cos_all = self.din("cos_all", [DR, L])
        self.sin_all = self.din("sin_all", [DR, L])
        self.x_own = self.din("x_own", [NOWN + 128, D])
        self.cos_own_d = self.din("cos_own", [DR, NOWN])
        self.sin_own_d = self.din("sin_own", [DR, NOWN])
        self.mask_d = self.din("maskT", [128, 8, 128], BF16)
        self.w_uq = self.din("w_uq", [QL, NH * 192])
        self.w_uqs = self.din("w_uqs", [QL, NH * 64])
        self.w_ukv = self.din("w_ukv", [KVL, NH * 256])
        self.pool_scale_d = self.din("pool_scale", [128, 8])
        self.pool_w_d = self.din("pool_w", [1024, 256])
        self.w_o_d = self.din("w_o_mla", [D, D])
        self.w_po_d = self.din("w_pool_out", [1024, D])
        self.w_out_d = self.din("w_out", [D, D])
        self.ustr_d = self.din("ustr", [128, 128], BF16)
        self.iota_d = self.din("iota", [128, CAP])
        self.ident32_d = self.din("ident32", [128, 128])
        self.vec_ffn = self.din("norm_ffn_w", [1, D])
        self.vec_fin = self.din("final_norm_w", [1, D])
        self.w_r_d = self.din("w_r", [D, 72])
        self.b_r_d = self.din("b_r", [1, 72])
        self.w_eg_d = self.din("w_exp_gate", [NE * D, FF])
        self.w_eu_d = self.din("w_exp_up", [NE * D, FF])
        self.w_ed_d = self.din("w_exp_down", [NE * FF, D])
        self.out_d = self.dout("out", [NOWN, D])
        self.seg_bf = self.nc.dram_tensor("seg_bf", [(NCONV + NCONVA) * D, FF], BF16, kind="Internal")
        self.seu_bf = self.nc.dram_tensor("seu_bf", [(NCONV + NCONVA) * D, FF], BF16, kind="Internal")
        self.sed_bf = self.nc.dram_tensor("sed_bf", [(NCONV + NCONVA) * FF, D], BF16, kind="Internal")
        self.cvi = 0
        self.ident_d = self.din("ident_bf", [128, 128], BF16)
        self.ones_d = self.din("ones_bf", [128, 128], BF16)
        self.ident = self.sb("ident", [128, 128], BF16, side="left")
        self.ones = self.sb("ones", [128, 128], BF16, side="left")
        self.kvn_s = self.sb("kvn_s", [128, 4], F32, side="left")
        self.qn_s = self.sb("qn_s", [128, 4], F32, side="left")
        self.bar_t = self.sb("bar_t", [128, 8], F32, side="left")
        self.mkW = self.markL()
        self.wmix = self.sb("wmix", [128, D], F32, side="left")
        S = self.S
        S.total_sems.add("const")
        self.dma("sp", self.ident[:], self.ident_d[:, :], [], ["ident"], "const")
        self.dma("sp", self.ones[:], self.ones_d[:, :], [], ["ones"], "const")
        self.dma("sp", self.wmix[:], self.vec_mix[0:1, :].partition_broadcast(128), [], ["wmix"], "const")
        self.dma("sp", self.kvn_s[:], self.kvn[:, :], [], ["kvn_s"], "const")
        self.dma("sp", self.qn_s[:], self.qn[:, :], [], ["qn_s"], "const")

    def norm_stage1(self, src_rows_ap, ntok, blk_i, wtile, wkey):
        nb = self.nxt
        s = blk_i % nb
        xt = self.xt[s]
        xs = self.xs[blk_i % 2]
        ss = self.ssx
        kx, kxs = ("xt", s), ("xs", blk_i % 2)
        c = blk_i % 8
        kss = ("ssx", c)
        self.dma("sp", xt[0:ntok, :], src_rows_ap, [], [kx], f"xt{s}")
        self.act(xs[0:ntok, :], xt[0:ntok, :], ACTF.Square, [kx], [kxs, kss],
                 accum_out=ss[0:ntok, c:c + 1])
        self.act(ss[0:ntok, 8 + c:9 + c], ss[0:ntok, c:c + 1], ACTF.Sqrt, [kss], [("rsx", c)],
                 bias=EPS, scale=1.0 / D)
        self.S.op("dve", lambda e: e.reciprocal(ss[0:ntok, 16 + c:17 + c], ss[0:ntok, 8 + c:9 + c]),
                  [("rsx", c)], [("rstdx", c)])
        self.stt("dve", xs[0:ntok, :], xt[0:ntok, :], ss[0:ntok, 16 + c:17 + c], wtile[0:ntok, :],
                 ALU.mult, ALU.mult, [kx, ("rstdx", c), wkey], [kxs])

    def norm_stage2(self, ntok, aT, col0, tag, blk_i):
        xs = self.xs[blk_i % 2]
        kxs = ("xs", blk_i % 2)
        for half, eng in ((0, "act"), (1, "dve")):
            tph = self.tp[half]
            ktp = ("tp", half)
            for k8 in range(8):
                kc = half * 8 + k8
                self.tr(tph[:, k8 * 128: k8 * 128 + ntok], xs[0:ntok, kc * 128:(kc + 1) * 128],
                        self.ident[0:ntok, 0:ntok], [kxs, "ident"], [ktp])
            src = tph[:, :].rearrange("p (k t) -> p k t", t=128)[:, :, 0:ntok]
            dst = aT[:, half * 8:(half + 1) * 8, col0:col0 + ntok]
            self.cp(eng, dst, src, [ktp], [(tag, "aT")])

    def norm_pipeline(self, blocks, after_cb=None):
        base = self.blk_i
        n = len(blocks)
        if n == 0:
            return
        self.norm_stage1(blocks[0][0], blocks[0][1], base, self.wmix, "wmix")
        for k in range(n):
            if k + 1 < n:
                self.norm_stage1(blocks[k + 1][0], blocks[k + 1][1], base + k + 1, self.wmix, "wmix")
            src, ntok, aT, col0, tag = blocks[k]
            self.norm_stage2(ntok, aT, col0, tag, base + k)
            if after_cb is not None:
                after_cb(k)
        self.blk_i = base + n

    def alloc_norm(self):
        self.xt = [self.sb(f"xt{i}", [128, D], F32) for i in range(self.nxt)]
        self.xs = [self.sb(f"xs{i}", [128, D], BF16) for i in range(2)]
        self.tp = [self.ps(f"tp{i}", [128, 1024], BF16) for i in range(2)]
        self.ssx = self.sb("ssx", [128, 24], F32)
        self.blk_i = 0

    def alloc_front(self):
        self.alloc_norm()
        self.aTg = [self.sb(f"aTg{i}", [128, 16, 512], BF16) for i in range(2)]
        self.pc = [self.ps(f"pc{i}", [128, 512], F32) for i in range(2)]
        self.ssb = self.ps("ssb", [128, 512], F32)
        self.craw = self.sb("craw", [128, 4, 512], F32)
        self.sq = self.sb("sq", [128, 4, 512], BF16)
        self.rstdb = self.sb("rstdb", [128, 512], F32)
        self.pci = 0
        self.blk_i = 0

    def latent_group(self, aT, tag, n, wsb, wcol0, wkeyf, nscal, nskey, dstf, dkeyf):
        for mt in range(4):
            p = self.pc[self.pci % 2]
            pk = ("pc", self.pci % 2)
            self.pci += 1
            for kc in range(16):
                self.mm(p[:, 0:n], wsb[:, kc, wcol0 + mt * 128: wcol0 + (mt + 1) * 128], aT[:, kc, 0:n],
                        kc == 0, kc == 15, [wkeyf(kc), (tag, "aT")], [pk])
            self.cp("act", self.craw[:, mt, 0:n], p[:, 0:n], [pk], [("craw", mt)])
            self.act(self.sq[:, mt, 0:n], p[:, 0:n], ACTF.Square, [pk], [("sq", mt)])
        for mt in range(4):
            self.mm(self.ssb[:, 0:n], self.ones[:, :], self.sq[:, mt, 0:n], mt == 0, mt == 3,
                    ["ones", ("sq", mt)], ["ssb"])
        rstdb = self.rstdb
        self.act(rstdb[:, 0:n], self.ssb[:, 0:n], ACTF.Sqrt, ["ssb"], ["rstdb"], bias=EPS, scale=1.0 / 512)
        self.S.op("dve", lambda e, n=n: e.reciprocal(rstdb[:, 0:n], rstdb[:, 0:n]), ["rstdb"], ["rstdb"])
        for mt in range(4):
            self.stt("dve", dstf(mt), self.craw[:, mt, 0:n], nscal[:, mt:mt + 1], rstdb[:, 0:n],
                     ALU.mult, ALU.mult, [("craw", mt), nskey, "rstdb"], [dkeyf(mt)])

    def phase_a(self):
        self.mk0 = self.mark()
        self.ckvT = self.sb("ckvT", [128, 4, L], BF16)
        self.kpeT = self.sb("kpeT", [128, L], BF16)
        self.S.op("pool", lambda e: e.memset(self.kpeT[64:128, :], 0.0), [], [("kpeT", "pad")])
        self.cqT = self.sb("cqT", [128, 4, NOWN], BF16)
        self.mkA = self.mark()
        self.wkv = self.sb("wkv", [128, 16, 576], BF16)
        self.wks = self.sb("wks", [128, 16, 64], BF16)
        wks_tmp = self.sb("wks_tmp", [128, 16, 64], F32)
        for kc in range(0, 16, 4):
            self.dma("pool", self.wkv[:, kc:kc + 4, :],
                     self.w_in[kc * 128:(kc + 4) * 128, 512:1088].rearrange("(k p) c -> p k c", p=128),
                     [], [("wkv", kc)], "constp")
        self.dma("sp", wks_tmp[:], self.w_ks.rearrange("(k p) c -> p k c", p=128), [], ["wks_tmp"], "const")
        self.ts("dve", self.wks[:, :, 0:32], wks_tmp[:, :, 0:32], -1.0, None, ALU.mult, None,
                ["wks_tmp"], [("wks", 0)])
        self.cp("dve", self.wks[:, :, 32:64], wks_tmp[:, :, 32:64], ["wks_tmp"], [("wks", 1)])
        self.alloc_front()
        pkr = self.ps("pkr", [128, 512], F32)
        pks = self.ps("pks", [128, 512], F32)
        cs = self.sb("cs", [64, 2, 512], F32)
        t12 = self.sb("t12", [64, 2, 512], F32)

        segs = [(0, NMETA)] + [(NMETA + 512 * g, 512) for g in range(self.ngroups)]

        def chain_mt(gi, mt):
            row0, n = segs[gi]
            aT = self.aTg[gi % 2]
            tag = f"aTg{gi % 2}"
            p = self.pc[self.pci % 2]
            pk = ("pc", self.pci % 2)
            self.pci += 1
            for kc in range(16):
                self.mm(p[:, 0:n], self.wkv[:, kc, mt * 128:(mt + 1) * 128], aT[:, kc, 0:n],
                        kc == 0, kc == 15, [("wkv", kc - kc % 4), (tag, "aT")], [pk])
            self.cp("act", self.craw[:, mt, 0:n], p[:, 0:n], [pk], [("craw", mt)])
            self.act(self.sq[:, mt, 0:n], p[:, 0:n], ACTF.Square, [pk], [("sq", mt)])

        def chain_kr(gi):
            row0, n = segs[gi]
            aT = self.aTg[gi % 2]
            tag = f"aTg{gi % 2}"
            self.dma("sp", cs[:, 0, 0:n], self.cos_all[:, row0:row0 + n], [], [("cs", 0)], "cs0")
            self.dma("sp", cs[:, 1, 0:n], self.sin_all[:, row0:row0 + n], [], [("cs", 1)], "cs1")
            for kc in range(16):
                self.mm(pkr[0:64, 0:n], self.wkv[:, kc, 512:576], aT[:, kc, 0:n], kc == 0, kc == 15,
                        [("wkv", kc - kc % 4), (tag, "aT")], ["pkr"])
            self.tt("dve", t12[:, 0, 0:n], pkr[0:64, 0:n], cs[:, 0, 0:n], ALU.mult,
                    ["pkr", ("cs", 0)], [("t12", 0)])

        def chain_ks(gi):
            row0, n = segs[gi]
            aT = self.aTg[gi % 2]
            tag = f"aTg{gi % 2}"
            for kc in range(16):
                self.mm(pks[0:64, 0:n], self.wks[:, kc, :], aT[:, kc, 0:n], kc == 0, kc == 15,
                        [("wks",), (tag, "aT")], ["pks"])
            self.tt("dve", t12[:, 1, 0:n], pks[0:64, 0:n], cs[:, 1, 0:n], ALU.mult,
                    ["pks", ("cs", 1)], [("t12", 1)])
            self.tt("dve", self.kpeT[0:64, row0:row0 + n], t12[:, 0, 0:n], t12[:, 1, 0:n], ALU.add,
                    [("t12",)], [("kpeT", gi)])

        def finish(gi):
            row0, n = segs[gi]
            rstdb = self.rstdb
            for mt in range(4):
                self.mm(self.ssb[:, 0:n], self.ones[:, :], self.sq[:, mt, 0:n], mt == 0, mt == 3,
                        ["ones", ("sq", mt)], ["ssb"])
            self.act(rstdb[:, 0:n], self.ssb[:, 0:n], ACTF.Sqrt, ["ssb"], ["rstdb"], bias=EPS, scale=1.0 / 512)
            self.S.op("dve", lambda e, n=n: e.reciprocal(rstdb[:, 0:n], rstdb[:, 0:n]), ["rstdb"], ["rstdb"])
            for mt in range(4):
                self.stt("dve", self.ckvT[:, mt, row0:row0 + n], self.craw[:, mt, 0:n], self.kvn_s[:, mt:mt + 1],
                         rstdb[:, 0:n], ALU.mult, ALU.mult, [("craw", mt), "kvn_s", "rstdb"], [("ckvT", gi, mt)])

        blocks = []
        owner = []
        for gi, (row0, n) in enumerate(segs):
            g2 = gi % 2
            for b in range((n + 127) // 128):
                nt = min(128, n - b * 128)
                blocks.append((self.x_all[row0 + b * 128: row0 + b * 128 + nt, :], nt, self.aTg[g2], b * 128,
                               f"aTg{g2}"))
                owner.append((gi, b, (n + 127) // 128))
        pend_parts = []

        def parts_of(gi):
            return [lambda: (chain_mt(gi, 0), chain_kr(gi)),
                    lambda: (chain_mt(gi, 1), chain_ks(gi)),
                    lambda: chain_mt(gi, 2),
                    lambda: (chain_mt(gi, 3), finish(gi))]

        def after(k):
            gi, b, nb = owner[k]
            if self.stage == "full" and b == 0 and gi % 4 == 3 and (gi // 4) < NCONVA:
                self.convert_expert(NCONV + gi // 4)
            if pend_parts:
                pend_parts.pop(0)()
            if b == nb - 1:
                while pend_parts:
                    pend_parts.pop(0)()
                pend_parts.extend(parts_of(gi))

        self.norm_pipeline(blocks, after)
        while pend_parts:
            pend_parts.pop(0)()

    def phase_b1(self):
        for kc in range(0, 16, 4):
            self.dma("pool", self.wkv[:, kc:kc + 4, 0:512],
                     self.w_in[kc * 128:(kc + 4) * 128, 0:512].rearrange("(k p) c -> p k c", p=128),
                     [], [("wkv", kc)], "wq_in")
        for g in range(2):
            aT = self.aTg[g]
            tag = f"aTg{g}"
            self.norm_pipeline([(self.x_own[g * 512 + b * 128: g * 512 + (b + 1) * 128, :], 128, aT, b * 128, tag)
                                for b in range(4)])
            self.latent_group(aT, tag, 512, self.wkv, 0, lambda kc: ("wkv", kc - kc % 4), self.qn_s, "qn_s",
                              lambda mt, g=g: self.cqT[:, mt, g * 512:(g + 1) * 512],
                              lambda mt, g=g: ("cqT", g, mt))
        self.release(self.mkA)

    def convert_expert(self, e):
        for (src, dst, rows, key) in ((self.w_eg_d, self.seg_bf, D, "cvg"), (self.w_eu_d, self.seu_bf, D, "cvu"),
                                      (self.w_ed_d, self.sed_bf, FF, "cvd")):
            j = self.cvi % 6
            self.cvi += 1
            self.dma("pool", dst[e * rows:(e + 1) * rows, :].rearrange("(p r) c -> p r c", p=128),
                     src[e * rows:(e + 1) * rows, :].rearrange("(p r) c -> p r c", p=128),
                     [], [(key, e)], f"cv{j}")

    def phase_attn(self, nheads=NH):
        mk = self.mk0
        self.mkL0 = self.markL()
        self.OT = self.sb("OT", [128, NH, NOWN], BF16, side="left")
        cso = self.sb("cso", [64, 2, NOWN], F32)
        self.dma("sp", cso[:, 0, :], self.cos_own_d[:, :], [], [("cso", 0)], "const2")
        self.dma("sp", cso[:, 1, :], self.sin_own_d[:, :], [], [("cso", 1)], "const2")
        maskT = self.sb("maskT_s", [128, 8, 128], BF16)
        self.dma("sp", maskT[:], self.mask_d[:, :, :], [], ["maskT"], "const2")
        ones32 = self.sb("ones32", [128, 128], F32)
        self.S.op("pool", lambda e: e.memset(ones32[:], 1.0), [], ["ones32"])
        self.S.total_sems.add("const2")
        wq = [self.sb(f"wq{i}", [128, 4, 192], BF16) for i in range(2)]
        wqs_t = [self.sb(f"wqs_t{i}", [128, 4, 64], F32) for i in range(2)]
        wqs = [self.sb(f"wqs{i}", [128, 4, 64], BF16) for i in range(2)]
        wkv_h = [self.sb(f"wkvh{i}", [128, 4, 256], BF16) for i in range(2)]
        KhT = self.sb("KhT", [128, L], BF16)
        Vh = self.sb("Vh", [128, NBLK + 1, 128], BF16)
        Qn = self.sb("Qn", [128, NOWN], BF16)
        Qpe = self.sb("Qpe", [128, NOWN], BF16)
        self.S.op("pool", lambda e: e.memset(Qpe[64:128, :], 0.0), [], [("Qpe", "pad")])
        q12 = self.sb("q12", [64, 2, 512], F32)
        acc = self.sb("acc", [128, NOWN], F32)
        rcp = self.sb("rcp", [128, NOWN], F32)
        NPT = 6
        NST = 4
        pt = [self.sb(f"pt{i}", [128, 512], BF16) for i in range(NPT)]
        st = [self.ps(f"st{i}", [128, 512], F32) for i in range(NST)]
        ot = [self.ps(f"ot{i}", [128, 512], F32) for i in range(2)]
        gen = [self.ps(f"gen{i}", [128, 512], F32) for i in range(2)]
        rs = gen
        geni = 0
        sti = 0
        pti = 0
        scale = float((DN + DR) ** -0.5)

        def load_head_w(h):
            b = h % 2
            with_keys = [("wq", b), ("wqs_t", b), ("wkvh", b)]
            self.dma("pool", wq[b][:], self.w_uq[:, h * 192:(h + 1) * 192].rearrange("(k p) c -> p k c", p=128),
                     [], [("wq", b)], f"whq{b}")
            self.dma("sp", wqs_t[b][:], self.w_uqs[:, h * 64:(h + 1) * 64].rearrange("(k p) c -> p k c", p=128),
                     [], [("wqs_t", b)], f"whs{b}")
            self.dma("pool", wkv_h[b][:], self.w_ukv[:, h * 256:(h + 1) * 256].rearrange("(k p) c -> p k c", p=128),
                     [], [("wkvh", b)], f"whk{b}")

        load_head_w(0)
        for h in range(nheads):
            b = h % 2
            if h + 1 < nheads:
                load_head_w(h + 1)
            if self.stage == "full":
                n0 = min(2 * h, 24 + max(0, h - 12))
                n1 = min(2 * h + 2, 24 + max(0, h + 1 - 12))
                for e_ in range(n0, min(n1, NCONV)):
                    self.convert_expert(e_)
            self.ts("dve", wqs[b][:, :, 0:32], wqs_t[b][:, :, 0:32], -1.0, None, ALU.mult, None,
                    [("wqs_t", b)], [("wqs", b, 0)])
            self.cp("dve", wqs[b][:, :, 32:64], wqs_t[b][:, :, 32:64], [("wqs_t", b)], [("wqs", b, 1)])
            for g in range(2):
                cols = slice(g * 512, (g + 1) * 512)
                p = gen[geni % 2]; pk = ("gen", geni % 2); geni += 1
                for kc in range(4):
                    self.mm(p[:, :], wq[b][:, kc, 0:128], self.cqT[:, kc, cols], kc == 0, kc == 3,
                            [("wq", b), ("cqT",)], [pk])
                self.cp("act", Qn[:, cols], p[:, :], [pk], [("Qn", g)])
                p1 = gen[geni % 2]; pk1 = ("gen", geni % 2); geni += 1
                for kc in range(4):
                    self.mm(p1[0:64, :], wq[b][:, kc, 128:192], self.cqT[:, kc, cols], kc == 0, kc == 3,
                            [("wq", b), ("cqT",)], [pk1])
                self.tt("dve", q12[:, 0, :], p1[0:64, :], cso[:, 0, cols], ALU.mult, [pk1, ("cso", 0)], [("q12", 0)])
                p2 = gen[geni % 2]; pk2 = ("gen", geni % 2); geni += 1
                for kc in range(4):
                    self.mm(p2[0:64, :], wqs[b][:, kc, :], self.cqT[:, kc, cols], kc == 0, kc == 3,
                            [("wqs", b), ("cqT",)], [pk2])
                self.tt("dve", q12[:, 1, :], p2[0:64, :], cso[:, 1, cols], ALU.mult, [pk2, ("cso", 1)], [("q12", 1)])
                self.tt("dve", Qpe[0:64, cols], q12[:, 0, :], q12[:, 1, :], ALU.add, [("q12",)], [("Qpe", g)])
            segs = [(0, NMETA)] + [(NMETA + 512 * g, 512) for g in range(16)]
            for si, (c0, n) in enumerate(segs):
                p = gen[geni % 2]; pk = ("gen", geni % 2); geni += 1
                for kc in range(4):
                    self.mm(p[:, 0:n], wkv_h[b][:, kc, 0:128], self.ckvT[:, kc, c0:c0 + n], kc == 0, kc == 3,
                            [("wkvh", b), ("ckvT",)], [pk])
                self.cp("act" if si % 2 else "dve", KhT[:, c0:c0 + n], p[:, 0:n], [pk], [("KhT", si)])
            p = gen[geni % 2]; pk = ("gen", geni % 2); geni += 1
            for kc in range(4):
                self.mm(p[0:NMETA, 0:128], self.ckvT[:, kc, 0:NMETA], wkv_h[b][:, kc, 128:256], kc == 0, kc == 3,
                        [("wkvh", b), ("ckvT",)], [pk])
            self.cp("act", Vh[0:NMETA, 0, :], p[0:NMETA, 0:128], [pk], [("Vh", 0)])
            for j4 in range(16):
                p = gen[geni % 2]; pk = ("gen", geni % 2); geni += 1
                for jj in range(4):
                    j = j4 * 4 + jj
                    c0 = NMETA + 128 * j
                    for kc in range(4):
                        self.mm(p[:, jj * 128:(jj + 1) * 128], self.ckvT[:, kc, c0:c0 + 128],
                                wkv_h[b][:, kc, 128:256], kc == 0, kc == 3, [("wkvh", b), ("ckvT",)], [pk])
                self.cp("act" if j4 % 2 else "dve", Vh[:, 1 + j4 * 4: 5 + j4 * 4, :],
                        p[:, :].rearrange("p (j d) -> p j d", d=128), [pk], [("Vh", 1 + j4)])
            chunks = [(-1, 0)] + [(sc, r) for sc in range(8) for r in range(8)]
            plist = []
            for ci, (sc, r) in enumerate(chunks):
                if sc < 0:
                    nk, kc0, vch, q0 = NMETA, 0, 0, 0
                else:
                    j = sc * 8 + r
                    nk, kc0, vch, q0 = 128, NMETA + 128 * j, 1 + j, 128 * sc
                if q0 < 512:
                    plist.append((sc, r, nk, kc0, vch, q0, q0, 512, 0, ci))
                    plist.append((sc, r, nk, kc0, vch, q0, 512, 1024, 1, ci))
                else:
                    plist.append((sc, r, nk, kc0, vch, q0, q0, 1024, 1, ci))
            pend = []
            DEPTH = 3
            self.S.op("dve", lambda e: e.memset(acc[:], 0.0), [], [("acc",)])
            started = [False, False]
            lastidx = [max(i for i, p_ in enumerate(plist) if p_[8] == bk) for bk in range(2)]
            for pidx, (sc, r, nk, kc0, vch, q0, qa, qb, bank, ci) in enumerate(plist):
                nq = qb - qa
                sp_ = st[sti % NST]; sk = ("st", sti % NST); sti += 1
                self.mm(sp_[0:nk, 0:nq], KhT[:, kc0:kc0 + nk], Qn[:, qa:qb], True, False,
                        [("KhT",), ("Qn",)], [sk])
                self.mm(sp_[0:nk, 0:nq], self.kpeT[:, kc0:kc0 + nk], Qpe[:, qa:qb], False, True,
                        [("kpeT",), ("Qpe",)], [sk])
                if len(pend) >= DEPTH:
                    pend.pop(0)()
                pp = pt[pti % NPT]; pkey = ("pt", pti % NPT); pti += 1
                self.act(pp[0:nk, 0:nq], sp_[0:nk, 0:nq], ACTF.Exp, [sk], [pkey], scale=scale)
                if sc >= 0 and qa == q0:
                    self.tt("dve", pp[:, 0:128], pp[:, 0:128], maskT[:, r, :], ALU.mult,
                            [pkey, "maskT"], [pkey])
                use_dve = (pidx % 2 == 1)
                if use_dve:
                    self.tt("dve", acc[0:nk, qa:qb], acc[0:nk, qa:qb], pp[0:nk, 0:nq], ALU.add,
                            [pkey, ("acc", bank)], [("acc", bank)])
                    first_pe = False
                else:
                    first_pe = not started[bank]
                    started[bank] = True

                def pv(bank=bank, qa=qa, qb=qb, nk=nk, vch=vch, pp=pp, nq=nq, sc=sc, pkey=pkey,
                       use_dve=use_dve, first_pe=first_pe, lastp=(pidx == lastidx[bank])):
                    self.mm(ot[bank][:, qa - 512 * bank: qb - 512 * bank], Vh[0:nk, vch, :], pp[0:nk, 0:nq],
                            sc < 0, lastp, [("Vh",), pkey], [("ot", bank)])
                    if not use_dve:
                        self.mm(gen[bank][:, qa - 512 * bank: qb - 512 * bank], self.ones[0:nk, :], pp[0:nk, 0:nq],
                                first_pe, False, ["ones", pkey], [("gen", bank)])
                pend.append(pv)
            while pend:
                pend.pop(0)()
            for bank in range(2):
                cols = slice(bank * 512, (bank + 1) * 512)
                self.mm(gen[bank][:, :], ones32[:, :], acc[:, cols], False, True, ["ones32", ("acc", bank)],
                        [("gen", bank)])
            for bank in range(2):
                cols = slice(bank * 512, (bank + 1) * 512)
                self.S.op("dve", lambda e, bank=bank, cols=cols: e.reciprocal(rcp[:, cols], rs[bank][:, :]),
                          [("gen", bank)], [("rcp", bank)])
                self.tt("dve", self.OT[:, h, cols], ot[bank][:, :], rcp[:, cols], ALU.mult,
                        [("ot", bank), ("rcp", bank)], [("OT", h, bank)])
        self.release(mk)

    def debug_dump_attn(self):
        o1 = self.dout("dbg_OT", [128, NH, NOWN], BF16)
        self.dma("sp", o1[:, :, :], self.OT[:], [("OT",)], [], "outs")

    def phase_mix(self):
        mkR = self.mark()
        aTo = self.sb("aTo", [128, 16, NOWN + 128], BF16)
        mixedT = self.sb("mixedT", [128, 8, NOWN], BF16)
        psc = self.sb("psc", [128, 8], F32)
        self.dma("sp", psc[:], self.pool_scale_d[:, :], [], ["psc"], "psc")
        mk2 = self.mark()
        self.alloc_norm()
        self.norm_pipeline([(self.x_own[b * 128:(b + 1) * 128, :], 128, aTo, b * 128, "aTo") for b in range(9)])
        self.release(mk2)
        if getattr(self, "stop", 9) == 1:
            return
        wpool = self.sb("wpool", [128, 16, 1024], BF16)
        for kc in range(0, 16, 4):
            for hh in range(2):
                self.dma("pool", wpool[:, kc:kc + 4, hh * 512:(hh + 1) * 512],
                         self.w_in[kc * 128:(kc + 4) * 128, 1088 + hh * 512:1088 + (hh + 1) * 512].rearrange("(k p) c -> p k c", p=128),
                         [], [("wpool", kc, hh)], f"wpool{kc}_{hh}")
        poolw = self.sb("poolw", [128, 4, 2, 256], BF16)
        for g in range(4):
            self.dma("pool", poolw[:, g, :, :], self.pool_w_d[g * 256:(g + 1) * 256, :].rearrange("(k p) d -> p k d", p=128),
                     [], [("poolw", g)], f"poolw{g}")
        ub = self.sb("ub", [128, 8, 144], F32)
        sA = self.sb("sA", [128, 8, 144], F32)
        sB = self.sb("sB", [128, 8, 144], F32)
        pooledT = self.sb("pooledT", [128, 8, NOWN], BF16)
        up = [self.ps(f"up{i}", [128, 512], F32) for i in range(3)]
        mx = [self.ps(f"mx{i}", [128, 512], F32) for i in range(2)]
        npieces = [(0, 512), (512, 512), (1024, 128)]
        for c in range(8 if getattr(self, "sub", 9) >= 1 else 0):
            for pi, (c0, n) in enumerate(npieces):
                for kc in range(16):
                    self.mm(up[pi][:, 0:n], wpool[:, kc, c * 128:(c + 1) * 128], aTo[:, kc, c0:c0 + n],
                            kc == 0, kc == 15, [("wpool", kc - kc % 4), ("aTo",)], [("up", pi)])
            self.cp("act", ub[:, 0:4, 16:144], up[0][:, :].rearrange("p (m t) -> p m t", t=128), [("up", 0)], [("ub", 0)])
            self.cp("act", ub[:, 4:8, 16:144], up[1][:, :].rearrange("p (m t) -> p m t", t=128), [("up", 1)], [("ub", 1)])
            self.cp("act", ub[:, :, 0:16], up[2][:, 0:128].rearrange("p (m t) -> p m t", t=16), [("up", 2)], [("ub", 2)])
            if getattr(self, "sub", 9) < 2:
                continue
            g = c // 2
            srcs = [ub, sA, sB, sA, sB]
            keys = ["ub", "sA", "sB", "sA", "sB"]
            d = 1
            lo = 0
            for step in range(g + 1):
                src, dst = srcs[step], srcs[step + 1]
                lo2 = lo + d
                self.tt("pool", dst[:, :, lo2:144], src[:, :, lo2:144], src[:, :, lo:144 - d], ALU.add,
                        [(keys[step],)], [(keys[step + 1],)])
                lo = lo2
                d *= 2
            fin = srcs[g + 1]
            self.stt("dve", pooledT[:, c, :].rearrange("p (m t) -> p m t", t=128), fin[:, :, 16:144], 1.0 / d,
                     ub[:, :, 16:144], ALU.mult, ALU.subtract, [(keys[g + 1],), ("ub",)], [("pooledT", c)])
        for g in range(4 if getattr(self, "sub", 9) >= 3 else 0):
            for dl in range(2):
                dc = 2 * g + dl
                for nh in range(2):
                    cols = slice(nh * 512, (nh + 1) * 512)
                    p = mx[(dc * 2 + nh) % 2]; pk = ("mx", (dc * 2 + nh) % 2)
                    for cc in range(2):
                        self.mm(p[:, :], poolw[:, g, cc, dl * 128:(dl + 1) * 128], pooledT[:, 2 * g + cc, cols],
                                cc == 0, cc == 1, [("poolw",), ("pooledT",)], [pk])
                    self.ts("dve", mixedT[:, dc, cols], p[:, :], psc[:, dc:dc + 1], None, ALU.mult, None,
                            [pk, "psc"], [("mixedT", dc, nh)])
        self.release(mk2)
        if getattr(self, "stop", 9) == 2:
            return
        self.mergedT = self.sb("mergedT", [128, 16, NOWN], BF16, side="left")
        CW = 256
        NCG = D // CW
        wgm = [self.sb(f"wgm{i}", [128, 16, CW], BF16) for i in range(2)]
        wgp = [self.sb(f"wgp{i}", [128, 16, CW], BF16) for i in range(2)]
        wo = [self.sb(f"wo{i}", [128, 16, CW], BF16) for i in range(2)]
        wpo = [self.sb(f"wpo{i}", [128, 8, CW], BF16) for i in range(2)]
        sg = [self.sb(f"sg{i}", [128, 2, 512], F32) for i in range(2)]
        tm = [self.sb(f"tm{i}", [128, 2, 512], F32) for i in range(2)]
        pg = [[self.ps(f"pg{i}_{j}", [128, 512], F32) for j in range(4)] for i in range(2)]

        def load_cg(cg):
            wb = cg % 2
            csl = slice(cg * CW, (cg + 1) * CW)
            for kc in range(0, 16, 8):
                self.dma("pool", wgm[wb][:, kc:kc + 8, :],
                         self.w_in[kc * 128:(kc + 8) * 128, 2112 + cg * CW: 2112 + (cg + 1) * CW].rearrange("(k p) c -> p k c", p=128),
                         [], [("wgm", wb, kc)], f"wgm{wb}_{kc}")
                self.dma("pool", wgp[wb][:, kc:kc + 8, :],
                         self.w_in[kc * 128:(kc + 8) * 128, 4160 + cg * CW: 4160 + (cg + 1) * CW].rearrange("(k p) c -> p k c", p=128),
                         [], [("wgp", wb, kc)], f"wgp{wb}_{kc}")
                self.dma("pool", wo[wb][:, kc:kc + 8, :],
                         self.w_o_d[kc * 128:(kc + 8) * 128, csl].rearrange("(k p) c -> p k c", p=128),
                         [], [("wo", wb, kc)], f"wo{wb}_{kc}")
            self.dma("pool", wpo[wb][:, :, :],
                     self.w_po_d[:, csl].rearrange("(k p) c -> p k c", p=128),
                     [], [("wpo", wb)], f"wpo{wb}")

        it = 0
        load_cg(0)
        for cg in range(NCG):
            wb = cg % 2
            if cg + 1 < NCG:
                load_cg(cg + 1)
            for mt in range(CW // 128):
                f = cg * (CW // 128) + mt
                msl = slice(mt * 128, (mt + 1) * 128)
                for nh in range(2):
                    cols = slice(nh * 512, (nh + 1) * 512)
                    i2 = it % 2
                    it += 1
                    P = pg[i2]
                    for kc in range(16):
                        self.mm(P[0][:, :], wgm[wb][:, kc, msl], aTo[:, kc, cols], kc == 0, kc == 15,
                                [("wgm", wb, kc - kc % 8), ("aTo",)], [("pg", i2, 0)])
                    for kc in range(16):
                        self.mm(P[1][:, :], wgp[wb][:, kc, msl], aTo[:, kc, cols], kc == 0, kc == 15,
                                [("wgp", wb, kc - kc % 8), ("aTo",)], [("pg", i2, 1)])
                    for kc in range(16):
                        self.mm(P[2][:, :], wo[wb][:, kc, msl], self.OT[:, kc, cols], kc == 0, kc == 15,
                                [("wo", wb, kc - kc % 8), ("OT",)], [("pg", i2, 2)])
                    for kc in range(8):
                        self.mm(P[3][:, :], wpo[wb][:, kc, msl], mixedT[:, kc, cols], kc == 0, kc == 7,
                                [("wpo", wb), ("mixedT",)], [("pg", i2, 3)])
                    self.act(sg[i2][:, 0, :], P[0][:, :], ACTF.Sigmoid, [("pg", i2, 0)], [("sg", i2, 0)])
                    self.act(sg[i2][:, 1, :], P[1][:, :], ACTF.Sigmoid, [("pg", i2, 1)], [("sg", i2, 1)])
                    self.tt("dve", tm[i2][:, 0, :], P[2][:, :], sg[i2][:, 0, :], ALU.mult,
                            [("pg", i2, 2), ("sg", i2, 0)], [("tm", i2, 0)])
                    self.tt("dve", tm[i2][:, 1, :], P[3][:, :], sg[i2][:, 1, :], ALU.mult,
                            [("pg", i2, 3), ("sg", i2, 1)], [("tm", i2, 1)])
                    self.tt("dve", self.mergedT[:, f, cols], tm[i2][:, 0, :], tm[i2][:, 1, :], ALU.add,
                            [("tm", i2)], [("mergedT", f, nh)])
        self.release(mkR)
        if getattr(self, "stop", 9) == 3:
            return
        self.h2 = self.sb("h2", [128, OWN, D], F32)
        self.mkH = self.mark()
        self.dma("sp", self.h2[:], self.x_own[0:NOWN, :].rearrange("(m p) d -> p m d", p=128), [], [("h2",)], "h2ld")
        wout = [self.sb(f"wout{i}", [128, 16, 512], BF16) for i in range(2)]
        po = [self.ps(f"po{i}", [128, 512], F32) for i in range(4)]
        it = 0
        for cg in range(4):
            c512 = slice(cg * 512, (cg + 1) * 512)
            w = wout[cg % 2]
            for kc in range(0, 16, 4):
                self.dma("pool", w[:, kc:kc + 4, :],
                         self.w_out_d[kc * 128:(kc + 4) * 128, c512].rearrange("(k p) c -> p k c", p=128),
                         [], [("wout", cg % 2, kc)], f"wout{cg % 2}_{kc}")
            for m in range(OWN):
                p = po[it % 4]; pk = ("po", it % 4); it += 1
                for kc in range(16):
                    self.mm(p[:, :], self.mergedT[:, kc, m * 128:(m + 1) * 128], w[:, kc, :], kc == 0, kc == 15,
                            [("mergedT",), ("wout", cg % 2, kc - kc % 4)], [pk])
                self.tt("dve", self.h2[:, m, c512], p[:, :], self.h2[:, m, c512], ALU.add,
                        [pk, ("h2", m, cg)], [("h2", m, cg)])
        self.release(self.mkH)
        self.releaseL(self.mkW)

    def debug_dump_h2(self):
        o1 = self.dout("dbg_h2", [128, OWN, D], F32)
        self.dma("sp", o1[:, :, :], self.h2[:], [("h2",)], [], "outs")

    def phase_moe(self, nexp=NE):
        BIG = 30000.0
        h2 = self.h2
        bn = self.sb("bn", [128, OWN, D], BF16)
        Cw = self.sb("Cw", [128, OWN, NE], F32)
        pos = self.sb("pos", [128, OWN, NE], F32)
        pos64 = self.sb("pos64", [128, OWN, NE], F32)
        iota = self.sb("iota", [128, CAP], F32)
        self.dma("sp", iota[:], self.iota_d[:, :], [], ["iota"], "c4b")
        mkR = self.mark()
        selA = self.sb("selA", [128, OWN, NE], F32)
        selB = self.sb("selB", [128, OWN, NE], BF16)
        ustr = self.sb("ustr", [128, 128], BF16)
        ident32 = self.sb("ident32", [128, 128], F32)
        self.dma("sp", ustr[:], self.ustr_d[:, :], [], ["ustr"], "c4a")
        self.dma("sp", ident32[:], self.ident32_d[:, :], [], ["ident32"], "c4c")
        wffn = self.sb("wffn", [128, D], F32)
        self.dma("sp", wffn[:], self.vec_ffn[0:1, :].partition_broadcast(128), [], ["wffn"], "c4d")
        wr32 = self.sb("wr32", [128, 16, 72], F32)
        self.dma("sp", wr32[:], self.w_r_d.rearrange("(k p) c -> p k c", p=128), [], ["wr32"], "c4e")
        brt = self.sb("brt", [128, 72], F32)
        self.dma("sp", brt[:], self.b_r_d[0:1, :].partition_broadcast(128), [], ["brt"], "c4f")
        bn32s = [self.sb(f"bn32_{i}", [128, D], F32) for i in range(2)]
        bnT32 = self.sb("bnT32", [128, 16, 128], F32)
        rts = [self.sb(f"rt{i}", [128, 64], F32) for i in range(2)]
        lgs = [self.sb(f"lg{i}", [128, 72], F32) for i in range(2)]
        gm = self.sb("gm", [128, 8], F32)
        gex = self.sb("gex", [128, 8], F32)
        pen = self.sb("pen", [128, 8], F32)
        elms = [self.sb(f"elm{i}", [128, NE], F32) for i in range(2)]
        ee = self.sb("ee", [128, NE], F32)
        c0 = self.sb("c0", [128, NE], F32)
        top8 = self.sb("top8", [128, 8], F32)
        tpr = [self.ps(f"tpr{i}", [128, 512], F32) for i in range(2)]
        lgp = [self.ps(f"lgp{i}", [128, 512], F32) for i in range(2)]
        posp = self.ps("posp", [128, 512], F32)

        def stage_a(m):
            i2 = m % 2
            rt = rts[i2]; bn32 = bn32s[i2]
            self.act(bn[:, m, :], h2[:, m, :], ACTF.Square, [("h2", m)], [("bn", m), ("rt", i2, 0)],
                     accum_out=rt[:, 0:1])
            self.act(rt[:, 1:2], rt[:, 0:1], ACTF.Sqrt, [("rt", i2, 0)], [("rt", i2, 1)], bias=EPS, scale=1.0 / D)
            self.S.op("dve", lambda e: e.reciprocal(rt[:, 2:3], rt[:, 1:2]), [("rt", i2, 1)], [("rt", i2, 2)])
            self.stt("dve", bn32[:, :], h2[:, m, :], rt[:, 2:3], wffn[:, :], ALU.mult, ALU.mult,
                     [("h2", m), ("rt", i2, 2), "wffn"], [("bn32", i2)])
            self.cp("act", bn[:, m, :], bn32[:, :], [("bn32", i2)], [("bn", m)])

        def stage_b(m):
            i2 = m % 2
            rt = rts[i2]; bn32 = bn32s[i2]; lg = lgs[i2]; elm = elms[i2]
            for q in range(4):
                t = tpr[q % 2]
                for i in range(4):
                    kc = q * 4 + i
                    self.tr(t[:, i * 128:(i + 1) * 128], bn32[:, kc * 128:(kc + 1) * 128], ident32[:, :],
                            [("bn32", i2), "ident32"], [("tpr", q % 2)])
                self.cp("act" if q % 2 else "dve", bnT32[:, q * 4:(q + 1) * 4, :],
                        t[:, :].rearrange("p (k t) -> p k t", t=128), [("tpr", q % 2)], [("bnT32", q)])
            lp = lgp[i2]
            for kc in range(16):
                self.mm(lp[:, 0:72], bnT32[:, kc, :], wr32[:, kc, :], kc == 0, kc == 15,
                        [("bnT32", kc // 4), "wr32"], [("lgp", i2)])
            self.tt("dve", lg[:, :], lp[:, 0:72], brt[:, :], ALU.add, [("lgp", i2), "brt"], [("lg", i2)])
            self.S.op("dve", lambda e: e.tensor_reduce(rt[:, 3:4], lg[:, 0:8], AX.X, ALU.max), [("lg", i2)],
                      [("rt", i2, 3)])
            self.ts("dve", gm[:, :], lg[:, 0:8], rt[:, 3:4], None, ALU.is_ge, None, [("lg", i2), ("rt", i2, 3)], ["gm"])
            self.ts("dve", rt[:, 4:5], rt[:, 3:4], -1.0, None, ALU.mult, None, [("rt", i2, 3)], [("rt", i2, 4)])
            self.act(gex[:, :], lg[:, 0:8], ACTF.Exp, [("lg", i2), ("rt", i2, 4)], ["gex", ("rt", i2, 5)],
                     bias=rt[:, 4:5], accum_out=rt[:, 5:6])
            self.ts("dve", pen[:, :], gm[:, :], BIG, -BIG, ALU.mult, ALU.add, ["gm"], ["pen"])
            for g in range(8):
                self.ts("dve", elm[:, g * 8:(g + 1) * 8], lg[:, 8 + g * 8: 16 + g * 8], pen[:, g:g + 1], None,
                        ALU.add, None, [("lg", i2), "pen"], [("elm", i2, g)])
            self.S.op("dve", lambda e: e.max(out=top8[:, :], in_=elm[:, :]), [("elm", i2)], ["top8"])
            self.ts("dve", selA[:, m, :], elm[:, :], top8[:, 1:2], None, ALU.is_ge, None, [("elm", i2), "top8"],
                    [("selA", m)])
            self.cp("dve", selB[:, m, :], selA[:, m, :], [("selA", m)], [("selB", m)])
            self.ts("dve", rt[:, 6:7], top8[:, 0:1], -1.0, None, ALU.mult, None, ["top8"], [("rt", i2, 6)])
            self.act(ee[:, :], elm[:, :], ACTF.Exp, [("elm", i2), ("rt", i2, 6)], ["ee"], bias=rt[:, 6:7])
            self.tt("dve", c0[:, :], selA[:, m, :], ee[:, :], ALU.mult, [("selA", m), "ee"], ["c0"])
            self.S.op("dve", lambda e: e.tensor_reduce(rt[:, 7:8], c0[:, :], AX.X, ALU.add), ["c0"], [("rt", i2, 7)])
            self.tt("dve", rt[:, 8:9], rt[:, 7:8], rt[:, 5:6], ALU.mult, [("rt", i2, 7), ("rt", i2, 5)], [("rt", i2, 8)])
            self.S.op("dve", lambda e: e.reciprocal(rt[:, 9:10], rt[:, 8:9]), [("rt", i2, 8)], [("rt", i2, 9)])
            self.ts("dve", Cw[:, m, :], c0[:, :], rt[:, 9:10], None, ALU.mult, None, ["c0", ("rt", i2, 9)], [("Cw", m)])
            self.mm(posp[:, 0:NE], ustr[:, :], selB[:, m, :], True, m == 0, ["ustr", ("selB", m)], ["posp"])
            for mp in range(m):
                self.mm(posp[:, 0:NE], self.ones[:, :], selB[:, mp, :], False, mp == m - 1,
                        ["ones", ("selB", mp)], ["posp"])
            self.stt("dve", pos[:, m, :], posp[:, 0:NE], 1.0, selA[:, m, :], ALU.add, ALU.mult,
                     ["posp", ("selA", m)], [("pos", m)])
            self.ts("dve", pos[:, m, :], pos[:, m, :], -1.0, None, ALU.add, None, [("pos", m)], [("pos", m)])
            self.ts("dve", pos64[:, m, :], pos[:, m, :], 64.0, None, ALU.add, None, [("pos", m)], [("pos64", m)])

        stage_a(0)
        for m in range(OWN):
            if m + 1 < OWN:
                stage_a(m + 1)
            stage_b(m)
        self.release(mkR)
        if self.stage == "route":
            return Cw, pos
        wg = [self.sb(f"wg{i}", [128, 16, FF], BF16) for i in range(2)]
        wu = [self.sb(f"wu{i}", [128, 16, FF], BF16) for i in range(2)]
        wd = self.sb("wd", [128, 4, D], BF16)
        SelE = self.sb("SelE", [128, OWN, CAPG], BF16)
        SelW = self.sb("SelW", [128, 2, CAP], BF16)
        SelWT = [self.sb(f"SelWT{i}", [128, OWN, 128], BF16) for i in range(2)]
        xeT = self.sb("xeT", [128, 16, CAP], BF16)
        self.S.op("dve", lambda e: e.memset(xeT[:], 0.0), [], [("xeT",)])
        hdn = self.sb("hdn", [128, FF], BF16)
        hdnT = self.sb("hdnT", [128, 4, 128], BF16)
        ye = [self.sb(f"ye{i}", [128, D], BF16) for i in range(2)]
        for yt_ in ye:
            self.S.op("dve", lambda e, yt_=yt_: e.memset(yt_[:], 0.0), [], [("ye",)])
        gx = [self.ps(f"gx{i}", [128, 512], F32) for i in range(2)]
        fy = [self.ps(f"fy{i}", [128, 512], F32) for i in range(3)]
        tq = self.ps("tq", [128, 1024], BF16)
        scp = [self.ps(f"scp{i}", [128, 512], F32) for i in range(2)]

        conv = (self.stage == "full")

        def load_gu(i):
            e = order[i]
            b = i % 2
            pre = conv and e < NCONV + NCONVA
            q = "sp" if pre else "pool"
            sg_, su_ = (self.seg_bf, self.seu_bf) if pre else (self.w_eg_d, self.w_eu_d)
            for kc in range(0, 16, 4):
                r0 = e * D + kc * 128
                self.dma(q, wg[b][:, kc:kc + 4, :],
                         sg_[r0:r0 + 512, :].rearrange("(k p) c -> p k c", p=128),
                         [("cvg", e)] if pre else [], [("wg", b, kc)], f"wg{b}_{kc}{q}")
                self.dma(q, wu[b][:, kc:kc + 4, :],
                         su_[r0:r0 + 512, :].rearrange("(k p) c -> p k c", p=128),
                         [("cvu", e)] if pre else [], [("wu", b, kc)], f"wu{b}_{kc}{q}")

        def load_d(i):
            e = order[i]
            pre = conv and e < NCONV + NCONVA
            q = "sp" if pre else "pool"
            sd_ = self.sed_bf if pre else self.w_ed_d
            for hh in range(2):
                self.dma(q, wd[:, :, hh * 1024:(hh + 1) * 1024],
                         sd_[e * FF:(e + 1) * FF, hh * 1024:(hh + 1) * 1024].rearrange("(k p) c -> p k c", p=128),
                         [("cvd", e)] if pre else [], [("wd", hh)], f"wd{hh}{q}")

        if conv:
            la, lb = list(range(NCONV + NCONVA)), list(range(NCONV + NCONVA, NE))
            order = []
            for i in range(max(len(la), len(lb))):
                if i < len(la):
                    order.append(la[i])
                if i < len(lb):
                    order.append(lb[i])
            assert sorted(order) == list(range(NE))
        else:
            order = list(range(nexp))
        pending = []
        state = {"sci": 0}

        def drain(k=1):
            for _ in range(k):
                if pending:
                    pending.pop(0)()

        def make_scatter(pi, m, cgp):
            c512 = slice(cgp * 512, (cgp + 1) * 512)
            rb = pi % 2

            def f():
                i = state["sci"] % 2
                state["sci"] += 1
                p = scp[i]; pk = ("scp", i)
                self.mm(p[:, :], SelWT[rb][:, m, :], ye[rb][:, c512], True, True,
                        [("SelWT", rb), ("ye", rb, cgp)], [pk])
                self.tt("dve", h2[:, m, c512], p[:, :], h2[:, m, c512], ALU.add,
                        [pk, ("h2", m, cgp)], [("h2", m, cgp)])
            return f

        load_gu(0)
        load_d(0)
        gxi = 0
        nexp = len(order)
        for ei in range(nexp):
            e = order[ei]
            b = ei % 2
            if ei + 1 < nexp:
                load_gu(ei + 1)
            half = ei % 2
            pi = ei // 2
            rb = pi % 2
            off = 64 * half
            psel = pos64 if half else pos
            if half == 0:
                eA = e
                eB = order[ei + 1] if ei + 1 < nexp else None
                for m in range(OWN):
                    self.ts("dve", SelW[:, m % 2, 0:64], iota[:, 0:64], pos[:, m, eA:eA + 1], Cw[:, m, eA:eA + 1],
                            ALU.is_equal, ALU.mult, ["iota", ("pos", m), ("Cw", m)], [("SelW", m % 2, 0)])
                    if eB is not None:
                        self.ts("dve", SelW[:, m % 2, 64:128], iota[:, 64:128], pos64[:, m, eB:eB + 1],
                                Cw[:, m, eB:eB + 1], ALU.is_equal, ALU.mult,
                                ["iota", ("pos64", m), ("Cw", m)], [("SelW", m % 2, 1)])
                    else:
                        self.S.op("dve", lambda e_, m=m: e_.memset(SelW[:, m % 2, 64:128], 0.0), [],
                                  [("SelW", m % 2, 1)])
                    self.tr(tq[:, m * 128:(m + 1) * 128], SelW[:, m % 2, :], self.ident[:, :],
                            [("SelW", m % 2), "ident"], ["tq"])
                    if m % 2:
                        drain()
                self.cp("act", SelWT[rb][:, :, :], tq[:, :].rearrange("p (m t) -> p m t", t=128), ["tq"],
                        [("SelWT", rb)])
            for m in range(OWN):
                self.ts("dve", SelE[:, m, :], iota[:, off:off + CAPG], psel[:, m, e:e + 1], None,
                        ALU.is_equal, None, ["iota", ("pos", m), ("pos64", m)], [("SelE", m)])
            for fq in range(4):
                p = gx[gxi % 2]; pk = ("gx", gxi % 2); gxi += 1
                for fi in range(4):
                    f = fq * 4 + fi
                    for m in range(OWN):
                        self.mm(p[:, fi * 128 + off:fi * 128 + off + CAPG], bn[:, m, f * 128:(f + 1) * 128], SelE[:, m, :],
                                m == 0, m == OWN - 1, [("bn", m), ("SelE", m)], [pk])
                    drain()
                self.cp("act" if fq % 2 else "dve", xeT[:, fq * 4:(fq + 1) * 4, off:off + CAPG],
                        p[:, :].rearrange("p (k t) -> p k t", t=128)[:, :, off:off + CAPG], [pk], [("xeT", fq)])
            gp, upp = fy[0], fy[1]
            for kc in range(16):
                self.mm(gp[:, :], xeT[:, kc, :], wg[b][:, kc, :], kc == 0, kc == 15,
                        [("xeT", kc // 4), ("wg", b, kc - kc % 4)], [("fy", 0)])
                if kc % 4 == 3:
                    drain()
            for kc in range(16):
                self.mm(upp[:, :], xeT[:, kc, :], wu[b][:, kc, :], kc == 0, kc == 15,
                        [("xeT", kc // 4), ("wu", b, kc - kc % 4)], [("fy", 1)])
                if kc % 4 == 3:
                    drain()
            self.act(hdn[:, :], gp[:, :], ACTF.Silu, [("fy", 0)], ["hdn"])
            self.tt("dve", hdn[:, :], upp[:, :], hdn[:, :], ALU.mult, [("fy", 1), "hdn"], ["hdn"])
            drain(2)
            for kc in range(4):
                self.tr(tq[:, kc * 128:(kc + 1) * 128], hdn[:, kc * 128:(kc + 1) * 128], self.ident[:, :],
                        ["hdn", "ident"], ["tq"])
            self.cp("act", hdnT[:, :, :], tq[:, 0:512].rearrange("p (k t) -> p k t", t=128), ["tq"], ["hdnT"])
            drain(2)
            for cgp in range(4):
                c512 = slice(cgp * 512, (cgp + 1) * 512)
                fi_ = (2 + cgp) % 3
                yp = fy[fi_]
                for kc in range(4):
                    self.mm(yp[:, :], hdnT[:, kc, :], wd[:, kc, c512], kc == 0, kc == 3,
                            ["hdnT", ("wd", cgp // 2)], [("fy", fi_)])
                self.cp("act" if cgp % 2 else "dve", ye[rb][off:off + 64, c512], yp[off:off + 64, :],
                        [("fy", fi_)], [("ye", rb, cgp, half)])
                drain()
            if half == 1:
                drain(64)
            if ei + 1 < nexp:
                load_d(ei + 1)
            if half == 1 or ei == nexp - 1:
                for m in range(OWN):
                    for cgp in range(4):
                        pending.append(make_scatter(pi, m, cgp))
        drain(64)
        self.release(mkR)

    def phase_final(self):
        h2 = self.h2
        wfin = self.sb("wfin", [128, D], F32)
        self.dma("sp", wfin[:], self.vec_fin[0:1, :].partition_broadcast(128), [], ["wfin"], "c5")
        ot = [self.sb(f"outt{i}", [128, D], F32) for i in range(2)]
        junk = self.sb("junkf", [128, D], BF16)
        rt = self.sb("rtf", [128, 32], F32)
        for m in range(OWN):
            c = m * 3
            self.act(junk[:, :], h2[:, m, :], ACTF.Square, [("h2", m)], ["junkf", ("rtf", c)],
                     accum_out=rt[:, c:c + 1])
            self.act(rt[:, c + 1:c + 2], rt[:, c:c + 1], ACTF.Sqrt, [("rtf", c)], [("rtf", c + 1)], bias=EPS, scale=1.0 / D)
            self.S.op("dve", lambda e, c=c: e.reciprocal(rt[:, c + 2:c + 3], rt[:, c + 1:c + 2]),
                      [("rtf", c + 1)], [("rtf", c + 2)])
            o = ot[m % 2]
            self.stt("dve", o[:, :], h2[:, m, :], rt[:, c + 2:c + 3], wfin[:, :], ALU.mult, ALU.mult,
                     [("h2", m), ("rtf", c + 2), "wfin"], [("outt", m % 2)])
            self.dma("sp", self.out_d[m * 128:(m + 1) * 128, :], o[:, :], [("outt", m % 2)], [], f"outs{m % 2}")

    def debug_dump_phase_a(self):
        o1 = self.dout("dbg_ckvT", [128, 4, L], BF16)
        o2 = self.dout("dbg_kpeT", [64, L], BF16)
        self.dma("sp", o1[:, :, :], self.ckvT[:], [("ckvT",)], [], "outs")
        self.dma("sp", o2[:, :], self.kpeT[0:64, :], [("kpeT",)], [], "outs")


def rope_tables_np():
    inv = 1.0 / (10000.0 ** (np.arange(0, DR, 2, dtype=np.float32) / DR))
    ang = np.arange(L, dtype=np.float32)[:, None] * inv[None, :].astype(np.float32)
    ang = ang.astype(np.float32)
    cos = np.cos(ang).astype(np.float32)
    sin = np.sin(ang).astype(np.float32)
    cosT = np.ascontiguousarray(np.concatenate([cos, cos], axis=1).T)
    sinT = np.ascontiguousarray(np.concatenate([sin, sin], axis=1).T)
    return cosT, sinT


def _uq_swapped(w_uq):
    w = w_uq.reshape(QL, NH, 192)[:, :, 128:192]
    return np.ascontiguousarray(np.concatenate([w[:, :, 32:64], w[:, :, 0:32]], axis=2).reshape(QL, NH * 64))


def own_blocks(core):
    return [8 * m + core for m in range(OWN)]


def prep_core(common, core):
    xa = common["x_all"]
    blks = own_blocks(core)
    main = np.concatenate([xa[NMETA + 128 * j: NMETA + 128 * j + 128] for j in blks], axis=0)
    halo = np.concatenate([xa[128 * j: 128 * j + 16] for j in blks], axis=0)
    pos = np.concatenate([np.arange(NMETA + 128 * j, NMETA + 128 * j + 128) for j in blks])
    k = np.arange(128)[:, None]
    q = np.arange(128)[None, :]
    mask = np.zeros((128, 8, 128), np.float32)
    for r in range(8):
        if r < core:
            mask[:, r, :] = 1.0
        elif r == core:
            mask[:, r, :] = (k <= q).astype(np.float32)
    m = dict(common)
    m["x_own"] = np.ascontiguousarray(np.concatenate([main, halo], axis=0))
    m["cos_own"] = np.ascontiguousarray(common["cos_all"][:, pos])
    m["sin_own"] = np.ascontiguousarray(common["sin_all"][:, pos])
    m["maskT"] = mask.astype(ml_dtypes.bfloat16)
    return m


def prep_common(inp):
    x = np.asarray(inp["x"], np.float32)[0]
    meta = np.asarray(inp["meta_tokens"], np.float32)
    w_in = np.ascontiguousarray(np.asarray(inp["w_in"], np.float32)[0])
    kr = w_in[:, 1024:1088]
    cosT, sinT = rope_tables_np()
    m = {
        "x_all": np.ascontiguousarray(np.concatenate([meta, x], axis=0)),
        "w_in": w_in,
        "w_ks": np.ascontiguousarray(np.concatenate([kr[:, 32:64], kr[:, 0:32]], axis=1)),
        "norm_mix_w": np.ascontiguousarray(np.asarray(inp["norm_mix_w"], np.float32).reshape(1, D)),
        "kv_norm_w": np.ascontiguousarray(np.asarray(inp["kv_norm_w"], np.float32).reshape(4, 128).T),
        "q_norm_w": np.ascontiguousarray(np.asarray(inp["q_norm_w"], np.float32).reshape(4, 128).T),
        "cos_all": cosT,
        "sin_all": sinT,
        "w_uq": np.ascontiguousarray(np.asarray(inp["w_uq"], np.float32)[0]),
        "w_uqs": _uq_swapped(np.asarray(inp["w_uq"], np.float32)[0]),
        "w_ukv": np.ascontiguousarray(np.asarray(inp["w_ukv"], np.float32)[0]),
        "pool_scale": np.ascontiguousarray(np.asarray(inp["pool_scale"], np.float32).reshape(8, 128).T),
        "pool_w": np.ascontiguousarray(np.asarray(inp["pool_w"], np.float32)[0].reshape(1024, 256)),
        "w_o_mla": np.ascontiguousarray(np.asarray(inp["w_o_mla"], np.float32)[0]),
        "w_pool_out": np.ascontiguousarray(np.asarray(inp["w_pool_out"], np.float32)[0]),
        "w_out": np.ascontiguousarray(np.asarray(inp["w_out"], np.float32)[0]),
        "ustr": np.triu(np.ones((128, 128), np.float32), 1).astype(ml_dtypes.bfloat16),
        "iota": np.ascontiguousarray(np.broadcast_to(np.arange(CAP, dtype=np.float32)[None, :], (128, CAP))),
        "ident32": np.eye(128, dtype=np.float32),
        "norm_ffn_w": np.ascontiguousarray(np.asarray(inp["norm_ffn_w"], np.float32).reshape(1, D)),
        "final_norm_w": np.ascontiguousarray(np.asarray(inp["final_norm_w"], np.float32).reshape(1, D)),
        "w_r": np.ascontiguousarray(np.concatenate([np.asarray(inp["w_router_group"], np.float32)[0],
                                                    np.asarray(inp["w_router_expert"], np.float32)[0]], axis=1)),
        "b_r": np.ascontiguousarray(np.concatenate([np.asarray(inp["b_router_group"], np.float32)[0],
                                                    np.asarray(inp["b_router_expert"], np.float32)[0]])[None, :]),
        "w_exp_gate": np.asarray(inp["w_exp_gate"], np.float32).reshape(NE * D, FF),
        "w_exp_up": np.asarray(inp["w_exp_up"], np.float32).reshape(NE * D, FF),
        "w_exp_down": np.asarray(inp["w_exp_down"], np.float32).reshape(NE * FF, D),
        "ident_bf": np.eye(128, dtype=np.float32).astype(ml_dtypes.bfloat16),
        "ones_bf": np.ones((128, 128), np.float32).astype(ml_dtypes.bfloat16),
    }
    return m


def build_full():
    B = Builder(stage="full")
    B.setup_common()
    B.phase_a()
    B.phase_b1()
    B.phase_attn()
    B.phase_mix()
    B.phase_moe()
    B.phase_final()
    B.S.emit(B.nc, final_waits=[("sp", "outs0"), ("sp", "outs1")])
    B.close()
    return B


def kernel(**inputs):
    B = build_full()
    cm = prep_common(inputs)
    names = list(B.dram.keys())
    in_maps = []
    for c in range(NCORES):
        m = prep_core(cm, c)
        in_maps.append({k: m[k] for k in names if k != "out"})
    res = run_bass_kernel_spmd(B.nc, in_maps, core_ids=list(range(NCORES)))
    out = np.zeros((1, SEQ, D), np.float32)
    for c in range(NCORES):
        o = np.asarray(res.results[c]["out"], np.float32)
        for m, j in enumerate(own_blocks(c)):
            out[0, 128 * j:128 * j + 128, :] = o[128 * m:128 * (m + 1), :]
    return out
```
